# Optimizing a Trainium2 kernel written in Bass

```python
import jax, jax.numpy as jnp
from jax import lax
import numpy as np

D_MODEL = 1024
BATCH = 8
SEQ = 8192
DEPTH = 1

HEAD_DIM = 64
N_HEADS_DIL = 8
N_HEADS_RET = 8
W_DIL = N_HEADS_DIL * HEAD_DIM
W_RET = N_HEADS_RET * HEAD_DIM
MIX_WIDTH = W_DIL + W_RET
N_IN = 3 * W_DIL + 4 * W_RET
IN_SPLITS = (W_DIL, 2 * W_DIL, 3 * W_DIL, 3 * W_DIL + W_RET, 3 * W_DIL + 2 * W_RET, 3 * W_DIL + 3 * W_RET)
DILATED_PATTERNS = ((128, 1), (512, 4), (2048, 16))
BAND_BLOCK = 128
ROPE_THETA = 500000.0
ROT_DIM = HEAD_DIM // 4
RET_THETA = 10000.0
RET_CHUNK = 128
N_MEM = 256
N_HEADS_MEM = 4
MEM_HEAD_DIM = D_MODEL // N_HEADS_MEM
N_EXPERTS = 32
TOP_K = 4
D_FF_EXPERT = D_MODEL
SWIGLU_LIMIT = 7.0
SWIGLU_ALPHA = 1.702
MOE_BLOCK = 256
LN_EPS = 1e-5
DEEPNORM_ALPHA = (2 * DEPTH) ** 0.25
DEEPNORM_BETA = (8 * DEPTH) ** -0.25

kernel_name = "hybrid_dilated_retention_moe_block"


def layer_norm(x, g, b):
    xf = x.astype(jnp.float32)
    mu = jnp.mean(xf, axis=-1, keepdims=True)
    var = jnp.mean(jnp.square(xf - mu), axis=-1, keepdims=True)
    y = (xf - mu) * lax.rsqrt(var + LN_EPS)
    return (y * g.astype(jnp.float32) + b.astype(jnp.float32)).astype(x.dtype)


def rotary(x, theta, rot_dim):
    S = x.shape[-2]
    half = rot_dim // 2
    inv = 1.0 / (theta ** (jnp.arange(half, dtype=jnp.float32) / half))
    ang = jnp.arange(S, dtype=jnp.float32)[:, None] * inv[None, :]
    cos, sin = jnp.cos(ang), jnp.sin(ang)
    xr = x[..., :rot_dim].astype(jnp.float32)
    x1, x2 = xr[..., :half], xr[..., half:]
    rot = jnp.concatenate([x1 * cos - x2 * sin, x1 * sin + x2 * cos], axis=-1).astype(x.dtype)
    return jnp.concatenate([rot, x[..., rot_dim:]], axis=-1)


def to_heads(t, n_heads):
    B, S, _ = t.shape
    return t.reshape(B, S, n_heads, -1).transpose(0, 2, 1, 3)


def from_heads(t):
    B, H, S, hd = t.shape
    return t.transpose(0, 2, 1, 3).reshape(B, S, H * hd)


def band_attention(q, k, v, window):
    L, hd = q.shape[-2], q.shape[-1]
    nb = -(-L // BAND_BLOCK)
    Lp = nb * BAND_BLOCK
    pad = [(0, 0)] * (q.ndim - 2) + [(0, Lp - L), (0, 0)]
    q, k, v = jnp.pad(q, pad), jnp.pad(k, pad), jnp.pad(v, pad)
    lead = q.shape[:-2]
    blk = lambda t: t.reshape(*lead, nb, BAND_BLOCK, hd)
    qb, kb, vb = blk(q), blk(k), blk(v)

    def with_prev(t):
        prev = jnp.concatenate([jnp.zeros_like(t[..., :1, :, :]), t[..., :-1, :, :]], axis=-3)
        return jnp.concatenate([prev, t], axis=-2)

    kk, vv = with_prev(kb), with_prev(vb)
    s = jnp.einsum('...nqd,...nkd->...nqk', qb, kk).astype(jnp.float32)
    qi = jnp.arange(BAND_BLOCK)[:, None]
    kj = jnp.arange(2 * BAND_BLOCK)[None, :]
    dist = qi + BAND_BLOCK - kj
    bidx = jnp.arange(nb)[:, None, None]
    valid = (dist >= 0) & (dist <= window) & (bidx * BAND_BLOCK + kj - BAND_BLOCK >= 0)
    s = jnp.where(valid, s, -jnp.inf)
    m = jnp.max(s, axis=-1, keepdims=True)
    p = jnp.exp(s - m)
    l = jnp.sum(p, axis=-1, keepdims=True)
    o = jnp.einsum('...nqk,...nkd->...nqd', p, vv.astype(jnp.float32)) / l
    lse = (m + jnp.log(l))[..., 0]
    o = o.reshape(*lead, Lp, hd)[..., :L, :]
    lse = lse.reshape(*lead, Lp)[..., :L]
    return o, lse


def dilated_attention(q, k, v):
    B, H, S, hd = q.shape
    outs, lses = [], []
    for w, d in DILATED_PATTERNS:
        split = lambda t: t.reshape(B, H, S // d, d, hd).swapaxes(2, 3)
        o, lse = band_attention(split(q), split(k), split(v), w // d)
        outs.append(o.swapaxes(2, 3).reshape(B, H, S, hd))
        lses.append(lse.swapaxes(2, 3).reshape(B, H, S))
    wts = jax.nn.softmax(jnp.stack(lses), axis=0)
    return jnp.einsum('gbhs,gbhsd->bhsd', wts, jnp.stack(outs))


def retention(q, k, v):
    B, H, S, hd = q.shape
    C = RET_CHUNK
    n = S // C
    log_g = jnp.log1p(-(2.0 ** (-5.0 - jnp.arange(H, dtype=jnp.float32))))
    qc = q.reshape(B, H, n, C, hd).astype(jnp.float32)
    kc = k.reshape(B, H, n, C, hd).astype(jnp.float32)
    vc = v.reshape(B, H, n, C, hd).astype(jnp.float32)
    idx = jnp.arange(C, dtype=jnp.float32)
    diff = idx[:, None] - idx[None, :]
    inner_decay = jnp.exp(log_g[:, None, None] * jnp.maximum(diff, 0.0)) * (diff >= 0)
    s = jnp.einsum('bhnqd,bhnkd->bhnqk', qc, kc) * inner_decay[None, :, None]
    inner = jnp.einsum('bhnqk,bhnkd->bhnqd', s, vc)
    k_decay = jnp.exp(log_g[:, None] * (C - 1 - idx))
    kv = jnp.einsum('bhnkd,bhnke->nbhde', kc * k_decay[None, :, None, :, None], vc)
    chunk_decay = jnp.exp(log_g * C)[None, :, None, None]

    def step(state, kv_i):
        return state * chunk_decay + kv_i, state

    _, prev = lax.scan(step, jnp.zeros((B, H, hd, hd), jnp.float32), kv)
    q_decay = jnp.exp(log_g[:, None] * (idx + 1.0))
    cross = jnp.einsum('bhnqd,nbhde->bhnqe', qc * q_decay[None, :, None, :, None], prev)
    return (inner + cross).reshape(B, H, S, hd)


def head_norm(y, g):
    mu = jnp.mean(y, axis=-1, keepdims=True)
    var = jnp.mean(jnp.square(y - mu), axis=-1, keepdims=True)
    return (y - mu) * lax.rsqrt(var + LN_EPS) * g.astype(jnp.float32)[None, :, None, :]


def memory_cross_attention(x, mem, wq, wk, wv, wo):
    B, S, D = x.shape
    M = mem.shape[1]
    q = (x @ wq).reshape(B, S, N_HEADS_MEM, MEM_HEAD_DIM)
    k = (mem @ wk).reshape(B, M, N_HEADS_MEM, MEM_HEAD_DIM)
    v = (mem @ wv).reshape(B, M, N_HEADS_MEM, MEM_HEAD_DIM)
    s = jnp.einsum('bshd,bmhd->bhsm', q, k).astype(jnp.float32) * (MEM_HEAD_DIM ** -0.5)
    p = jax.nn.softmax(s, axis=-1)
    o = jnp.einsum('bhsm,bmhd->bshd', p, v.astype(jnp.float32)).reshape(B, S, D).astype(x.dtype)
    return o @ wo


def moe(x, router_w, router_b, w_gate, b_gate, w_up, b_up, w_down, b_down):
    B, S, D = x.shape
    N = B * S
    xf = x.reshape(N, D)
    logits = (xf @ router_w).astype(jnp.float32) + router_b.astype(jnp.float32)
    top_val, top_idx = lax.top_k(logits, TOP_K)
    top_w = jax.nn.softmax(top_val, axis=-1)
    A = N * TOP_K
    flat_e = top_idx.reshape(A)
    flat_tok = jnp.arange(A, dtype=jnp.int32) // TOP_K
    flat_w = top_w.reshape(A)
    order = jnp.argsort(flat_e)
    sorted_e = flat_e[order]
    counts = jnp.bincount(flat_e, length=N_EXPERTS)
    starts = jnp.cumsum(counts) - counts
    padded = (counts + MOE_BLOCK - 1) // MOE_BLOCK * MOE_BLOCK
    padded_end = jnp.cumsum(padded)
    padded_start = padded_end - padded
    dest = padded_start[sorted_e] + jnp.arange(A) - starts[sorted_e]
    n_blocks = -(-A // MOE_BLOCK) + N_EXPERTS
    P = n_blocks * MOE_BLOCK
    slot_tok = jnp.zeros((P,), jnp.int32).at[dest].set(flat_tok[order])
    slot_w = jnp.zeros((P,), jnp.float32).at[dest].set(flat_w[order])
    block_e = jnp.minimum(jnp.searchsorted(padded_end, jnp.arange(n_blocks) * MOE_BLOCK, side='right'),
                          N_EXPERTS - 1)

    def step(y, blk):
        e, tok, wt = blk
        xb = xf[tok]
        gate = jnp.minimum(xb @ w_gate[e] + b_gate[e], SWIGLU_LIMIT)
        up = jnp.clip(xb @ w_up[e] + b_up[e], -SWIGLU_LIMIT, SWIGLU_LIMIT)
        hmid = gate * jax.nn.sigmoid(SWIGLU_ALPHA * gate) * (up + 1.0)
        out = hmid @ w_down[e] + b_down[e]
        return y.at[tok].add((out * wt[:, None]).astype(y.dtype)), None

    y, _ = lax.scan(step, jnp.zeros_like(xf),
                    (block_e, slot_tok.reshape(n_blocks, MOE_BLOCK), slot_w.reshape(n_blocks, MOE_BLOCK)))
    return y.reshape(B, S, D)


def setup_inputs(seed: int = 0) -> dict:
    key = jax.random.key(seed)
    ks = jax.random.split(key, 24)
    L = DEPTH
    nrm = lambda k, shape, scale: jax.random.normal(k, shape, jnp.float32) * scale
    gain = lambda k, shape: 1.0 + nrm(k, shape, 0.02)
    col_scale = jnp.concatenate([
        jnp.ones((2 * W_DIL,), jnp.float32), jnp.full((W_DIL,), DEEPNORM_BETA, jnp.float32),
        jnp.ones((2 * W_RET,), jnp.float32), jnp.full((W_RET,), DEEPNORM_BETA, jnp.float32),
        jnp.ones((W_RET,), jnp.float32)])
    return {
        "x": nrm(ks[0], (BATCH, SEQ, D_MODEL), 1.0),
        "mem": nrm(ks[1], (BATCH, N_MEM, D_MODEL), 1.0),
        "w_in": nrm(ks[2], (L, D_MODEL, N_IN), D_MODEL ** -0.5) * col_scale,
        "ret_norm_g": gain(ks[3], (L, N_HEADS_RET, HEAD_DIM)),
        "w_out": nrm(ks[4], (L, MIX_WIDTH, D_MODEL), MIX_WIDTH ** -0.5 * DEEPNORM_BETA),
        "ln1_g": gain(ks[5], (L, D_MODEL)),
        "ln1_b": nrm(ks[6], (L, D_MODEL), 0.02),
        "mem_wq": nrm(ks[7], (L, D_MODEL, D_MODEL), D_MODEL ** -0.5),
        "mem_wk": nrm(ks[8], (L, D_MODEL, D_MODEL), D_MODEL ** -0.5),
        "mem_wv": nrm(ks[9], (L, D_MODEL, D_MODEL), D_MODEL ** -0.5 * DEEPNORM_BETA),
        "mem_wo": nrm(ks[10], (L, D_MODEL, D_MODEL), D_MODEL ** -0.5 * DEEPNORM_BETA),
        "ln2_g": gain(ks[11], (L, D_MODEL)),
        "ln2_b": nrm(ks[12], (L, D_MODEL), 0.02),
        "router_w": nrm(ks[13], (L, D_MODEL, N_EXPERTS), D_MODEL ** -0.5),
        "router_b": nrm(ks[14], (L, N_EXPERTS), 0.01),
        "w_gate": nrm(ks[15], (L, N_EXPERTS, D_MODEL, D_FF_EXPERT), D_MODEL ** -0.5 * DEEPNORM_BETA),
        "b_gate": nrm(ks[16], (L, N_EXPERTS, D_FF_EXPERT), 0.02),
        "w_up": nrm(ks[17], (L, N_EXPERTS, D_MODEL, D_FF_EXPERT), D_MODEL ** -0.5 * DEEPNORM_BETA),
        "b_up": nrm(ks[18], (L, N_EXPERTS, D_FF_EXPERT), 0.02),
        "w_down": nrm(ks[19], (L, N_EXPERTS, D_FF_EXPERT, D_MODEL), D_FF_EXPERT ** -0.5 * DEEPNORM_BETA),
        "b_down": nrm(ks[20], (L, N_EXPERTS, D_MODEL), 0.02),
        "ln3_g": gain(ks[21], (L, D_MODEL)),
        "ln3_b": nrm(ks[22], (L, D_MODEL), 0.02),
    }


def reference(x, mem, w_in, ret_norm_g, w_out, ln1_g, ln1_b, mem_wq, mem_wk, mem_wv, mem_wo,
              ln2_g, ln2_b, router_w, router_b, w_gate, b_gate, w_up, b_up, w_down, b_down,
              ln3_g, ln3_b):
    for l in range(DEPTH):
        h = x @ w_in[l]
        qa, ka, va, qr, kr, vr, gr = jnp.split(h, IN_SPLITS, axis=-1)
        qa = rotary(to_heads(qa, N_HEADS_DIL), ROPE_THETA, ROT_DIM) * (HEAD_DIM ** -0.5)
        ka = rotary(to_heads(ka, N_HEADS_DIL), ROPE_THETA, ROT_DIM)
        oa = dilated_attention(qa, ka, to_heads(va, N_HEADS_DIL))
        qr = rotary(to_heads(qr, N_HEADS_RET), RET_THETA, HEAD_DIM)
        kr = rotary(to_heads(kr, N_HEADS_RET), RET_THETA, HEAD_DIM) * (HEAD_DIM ** -0.5)
        orr = head_norm(retention(qr, kr, to_heads(vr, N_HEADS_RET)), ret_norm_g[l])
        orr = from_heads(orr) * jax.nn.silu(gr.astype(jnp.float32))
        mixed = jnp.concatenate([from_heads(oa), orr], axis=-1).astype(x.dtype) @ w_out[l]
        x = layer_norm(DEEPNORM_ALPHA * x + mixed, ln1_g[l], ln1_b[l])
        c = memory_cross_attention(x, mem, mem_wq[l], mem_wk[l], mem_wv[l], mem_wo[l])
        x = layer_norm(DEEPNORM_ALPHA * x + c, ln2_g[l], ln2_b[l])
        f = moe(x, router_w[l], router_b[l], w_gate[l], b_gate[l], w_up[l], b_up[l], w_down[l], b_down[l])
        x = layer_norm(DEEPNORM_ALPHA * x + f, ln3_g[l], ln3_b[l])
    return x
```

```python
import math
from contextlib import ExitStack

import numpy as np
import ml_dtypes
import concourse.bass as bass
import concourse.mybir as mybir
from concourse.bass_utils import run_bass_kernel_spmd

F32 = mybir.dt.float32
BF16 = mybir.dt.bfloat16
AF = mybir.ActivationFunctionType
ALU = mybir.AluOpType
AX = mybir.AxisListType

D = 1024
NIN = 3584
NEXP = 32
NMEM = 256
ALPHA = 2.0 ** 0.25
EPS = 1e-5
NOFF = 17
TC = 1024
VS = 80
SPARSE = True


class Res:
    __slots__ = ("name", "w", "r")

    def __init__(self, name=""):
        self.name = name
        self.w = {}
        self.r = {}


class Sched:
    ENG = ("pe", "act", "dve", "pool", "sp")

    def __init__(self, nc, stack, n_dma_sems=24):
        self.nc = nc
        self.streams = {e: [] for e in self.ENG}
        self.sems = {}
        self.count = {}
        self.waited = {e: {} for e in self.ENG}
        for e in self.ENG:
            s = stack.enter_context(nc.semaphore("s_" + e))
            self.sems[e] = s
            self.count[e] = 0
        self.dq = {}
        for q in ("sp", "pool", "act"):
            lst = []
            for i in range(n_dma_sems):
                key = "d_%s_%d" % (q, i)
                self.sems[key] = stack.enter_context(nc.semaphore(key))
                self.count[key] = 0
                lst.append(key)
            self.dq[q] = [lst, 0]
        self.n_instr = 0

    def _wait(self, eng, key, val):
        if val <= 0:
            return
        if self.waited[eng].get(key, 0) >= val:
            return
        self.waited[eng][key] = val
        sem = self.sems[key]
        self.streams[eng].append(lambda e, sem=sem, val=val: e.wait_ge(sem, val))

    def _deps(self, eng, reads, writes, extra):
        deps = {}

        def add(d):
            for k, v in d.items():
                if deps.get(k, 0) < v:
                    deps[k] = v
        for r in reads:
            add(r.w)
        for w in writes:
            add(w.r)
            add(w.w)
        for h in extra:
            add(h)
        for k, v in deps.items():
            if k == eng and eng == "pe":
                continue
            self._wait(eng, k, v)

    def _post(self, reads, writes, h):
        for r in reads:
            for k, v in h.items():
                if r.r.get(k, 0) < v:
                    r.r[k] = v
        for w in writes:
            w.w = dict(h)
            w.r = {}

    def op(self, eng, fn, reads=(), writes=(), extra=()):
        self._deps(eng, reads, writes, extra)
        self.count[eng] += 1
        val = self.count[eng]
        sem = self.sems[eng]
        self.streams[eng].append(lambda e, fn=fn, sem=sem: fn(e).then_inc(sem, 1))
        h = {eng: val}
        self._post(reads, writes, h)
        self.n_instr += 1
        return h

    def ops(self, eng, fns, reads=(), writes=(), extra=()):
        self._deps(eng, reads, writes, extra)
        for fn in fns[:-1]:
            self.streams[eng].append(lambda e, fn=fn: fn(e))
        self.count[eng] += 1
        val = self.count[eng]
        sem = self.sems[eng]
        fn = fns[-1]
        self.streams[eng].append(lambda e, fn=fn, sem=sem: fn(e).then_inc(sem, 1))
        h = {eng: val}
        self._post(reads, writes, h)
        self.n_instr += len(fns)
        return h

    def dma(self, q, out, in_, reads=(), writes=(), extra=()):
        self._deps(q, reads, writes, extra)
        lst, i = self.dq[q]
        key = lst[i % len(lst)]
        self.dq[q][1] = i + 1
        self._wait(q, key, self.count[key])
        self.count[key] += 16
        val = self.count[key]
        sem = self.sems[key]
        self.streams[q].append(lambda e, out=out, in_=in_, sem=sem: e.dma_start(out=out, in_=in_).then_inc(sem, 16))
        h = {key: val}
        self._post(reads, writes, h)
        self.n_instr += 1
        return h

    def dma_fn(self, q, fn, reads=(), writes=(), extra=()):
        self._deps(q, reads, writes, extra)
        lst, i = self.dq[q]
        key = lst[i % len(lst)]
        self.dq[q][1] = i + 1
        self._wait(q, key, self.count[key])
        self.count[key] += 16
        val = self.count[key]
        sem = self.sems[key]
        self.streams[q].append(lambda e, fn=fn, sem=sem: fn(e).then_inc(sem, 16))
        h = {key: val}
        self._post(reads, writes, h)
        self.n_instr += 1
        return h

    def barrier(self):
        allh = {k: v for k, v in self.count.items() if v > 0}
        for e in self.ENG:
            for k, v in allh.items():
                if k != e:
                    self._wait(e, k, v)

    def final_wait(self, eng, handles):
        for h in handles:
            for k, v in h.items():
                self._wait(eng, k, v)

    def emit(self, block):
        nc = self.nc
        m = {"pe": block.tensor, "act": block.scalar, "dve": block.vector, "pool": block.gpsimd, "sp": block.sync}
        for name in self.ENG:
            stream = self.streams[name]

            def body(e, stream=stream):
                for f in stream:
                    f(e)
            m[name](body)


class Arena:
    def __init__(self, nc, limit=206 * 1024):
        self.nc = nc
        self.off = 0
        self.limit = limit
        self.base = nc.alloc_sbuf_tensor("arena", [128, limit], mybir.dt.uint8)

    def mark(self):
        return self.off

    def reset(self, m):
        self.off = m

    def alloc(self, shape, dtype, name=None):
        esz = 2 if dtype == BF16 else 4
        nbytes = esz
        for s in shape[1:]:
            nbytes *= s
        off = (self.off + 63) // 64 * 64
        assert off + nbytes <= self.limit, ("SBUF overflow", name, off, nbytes)
        self.off = off + nbytes
        v = self.base[0:shape[0], off:off + nbytes].bitcast(dtype)
        if len(shape) == 3:
            v = v.rearrange("p (a b) -> p a b", a=shape[1])
        elif len(shape) == 4:
            v = v.rearrange("p (a b c) -> p a b c", a=shape[1], b=shape[2])
        return v


def _meta_consts(S):
    nblk = (S * 4) // 512 + NEXP
    p = np.arange(128, dtype=np.float32)[:, None]
    thr16 = np.broadcast_to((512.0 * np.arange(17, dtype=np.float32))[None, :], (128, 17))
    blkthr = np.broadcast_to((512.0 * np.arange(nblk, dtype=np.float32))[None, :], (128, nblk))
    iota_e = np.broadcast_to(np.arange(NEXP, dtype=np.float32)[None, :], (128, NEXP))
    kcp = 128.0 * np.arange(8, dtype=np.float32)[None, :] + p
    tril = np.broadcast_to(np.tril(np.ones((NEXP, NEXP), np.float32)).reshape(1, NEXP * NEXP), (128, NEXP * NEXP))
    return np.ascontiguousarray(np.concatenate([thr16, blkthr, iota_e, kcp, p, tril], axis=1).astype(np.float32)), nblk


def _const_tables(S):
    pos = np.arange(S, dtype=np.float64)
    inv_a = 1.0 / (500000.0 ** (np.arange(8, dtype=np.float64) / 8))
    ang = pos[:, None] * inv_a[None, :]
    ca, sa = np.cos(ang), np.sin(ang)
    inv_r = 1.0 / (10000.0 ** (np.arange(32, dtype=np.float64) / 32))
    angr = pos[:, None] * inv_r[None, :]
    cr, sr = np.cos(angr), np.sin(angr)
    tab = np.concatenate([
        np.concatenate([ca, ca], 1) / 8.0, np.concatenate([-sa, sa], 1) / 8.0,
        np.concatenate([ca, ca], 1), np.concatenate([-sa, sa], 1),
        np.concatenate([cr, cr], 1), np.concatenate([-sr, sr], 1),
    ], axis=1).astype(np.float32)
    h = np.arange(8, dtype=np.float64)
    log_g = np.log1p(-(2.0 ** (-5.0 - h)))
    i = np.arange(128, dtype=np.float64)
    kfac = np.exp(-log_g[None, :] * (i[:, None] + 1.0)) / 8.0
    qdec = np.exp(log_g[None, :] * (i[:, None] + 1.0))
    cd = np.exp(log_g * 128.0)
    cdp = np.zeros((128, 4), np.float64)
    for p in range(4):
        cdp[:64, p] = cd[2 * p]
        cdp[64:, p] = cd[2 * p + 1]
    rtab = np.concatenate([kfac, qdec, cdp], axis=1).astype(np.float32)
    kk = np.arange(128)[:, None]
    qq = np.arange(128)[None, :]
    cm = (kk <= qq).astype(np.float32)
    am = np.zeros((128, NOFF, 128), np.float32)
    for j in range(NOFF):
        o = 16 - j
        dl = 128 * o + qq - kk
        m = ((dl >= 0) & (dl <= 128)).astype(np.float32)
        m += ((dl >= 0) & (dl % 4 == 0) & (dl <= 512)).astype(np.float32)
        m += ((dl >= 0) & (dl % 16 == 0) & (dl <= 2048)).astype(np.float32)
        am[:, j, :] = m
    return dict(
        tab=tab, rtab=rtab, cm=cm.astype(np.float32), am=am.astype(ml_dtypes.bfloat16),
        identb=np.eye(128, dtype=np.float32).astype(ml_dtypes.bfloat16),
        identf=np.eye(128, dtype=np.float32),
        ustrict=(kk < qq).astype(np.float32),
    )


def build_program(NT=64, stop_after="B", debug=False):
    S = NT * 128
    nc = bass.Bass("TRN2", target_bir_lowering=False)

    def din(name, shape, dt=F32):
        return nc.dram_tensor(name, list(shape), dt, kind="ExternalInput").ap()

    x_d = din("x", [S, D])
    mem_d = din("mem", [NMEM, D])
    w_in_d = din("w_in", [D, NIN])
    w_out_d = din("w_out", [D, D])
    wq_d = din("mem_wq", [D, D])
    wk_d = din("mem_wk", [D, D])
    wv_d = din("mem_wv", [D, D])
    wo_d = din("mem_wo", [D, D])
    rw_d = din("router_w", [D, NEXP])
    rb_d = din("router_b", [1, NEXP])
    wg_d = din("w_gate", [NEXP, D, D])
    wu_d = din("w_up", [NEXP, D, D])
    wd_d = din("w_down", [NEXP, D, D])
    bgT_d = din("b_gateT", [128, NEXP, 8])
    buT_d = din("b_upT", [128, NEXP, 8])
    bd_d = din("b_down", [NEXP, D])
    lnp_d = din("lnp", [128, 6, D])
    rng_d = din("rng", [128, 512])
    tab_d = din("tab", [S, 192])
    rtab_d = din("rtab", [128, 20])
    cm_d = din("cm", [128, 128])
    am_d = din("am", [128, NOFF, 128], BF16)
    identb_d = din("identb", [128, 128], BF16)
    identf_d = din("identf", [128, 128])
    ustrict_d = din("ustrict", [128, 128])
    NBLK = (S * 4) // 512 + NEXP
    NSLOT = NBLK * 512
    CMW = 17 + NBLK + NEXP + 8 + 1 + NEXP * NEXP
    cmeta_d = din("cmeta", [128, CMW])
    out_d = nc.dram_tensor("out", [S, D], F32, kind="ExternalOutput").ap()

    def dscr(name, shape, dt):
        if debug:
            return nc.dram_tensor(name, list(shape), dt, kind="ExternalOutput").ap()
        return nc.dram_tensor(name, list(shape), dt).ap()

    QAT = dscr("QAT", [128, 4, S], BF16)
    KAT = dscr("KAT", [128, 4, S], BF16)
    QRT = dscr("QRT", [128, 4, S], BF16)
    KRT = dscr("KRT", [128, 4, S], BF16)
    VA = dscr("VA", [S, 8 * VS], BF16)
    KR = dscr("KR", [S, 512], BF16)
    VR = dscr("VR", [S, 512], BF16)
    GS = dscr("GS", [S, 512], F32)
    X2 = dscr("X2", [S, D], F32)
    X2T = dscr("X2T", [128, 8, S], BF16)
    GT = dscr("GT", [S, NEXP], F32)
    RKD = dscr("RKD", [S, NEXP], F32)
    CNT = dscr("CNT", [128, NEXP], F32)
    X2B = dscr("X2B", [S, D], BF16)
    XS = dscr("XS", [NSLOT, D], BF16)
    YS = dscr("YS", [NSLOT, D], F32)

    dbg = {}
    if debug:
        def dout(name, shape, dt=F32):
            dbg[name] = nc.dram_tensor(name, list(shape), dt, kind="ExternalOutput").ap()
            return dbg[name]

    with ExitStack() as stack:
        sc = Sched(nc, stack)
        ar = Arena(nc)
        banks = [nc.alloc_psum_tensor("bank%d" % i, [128, 512], F32) for i in range(8)]
        bres = [Res("bank%d" % i) for i in range(8)]

        def bank_f32(i):
            return banks[i][:]

        def bank_bf16(i):
            return banks[i][:].bitcast(BF16)

        identb = ar.alloc([128, 128], BF16, "identb")
        identf = ar.alloc([128, 128], F32, "identf")
        r_const = Res("const")
        sc.dma("sp", identb[:], identb_d[:, :], writes=[r_const])
        sc.dma("sp", identf[:], identf_d[:, :], writes=[r_const])
        epsc = ar.alloc([128, 1], F32, "epsc")
        sc.op("pool", lambda e: e.memset(epsc[:], EPS), writes=[r_const])
        base_mark = ar.mark()
        out_handles = []

        w_in = ar.alloc([128, 8, NIN], BF16, "w_in")
        r_w_in = Res("w_in")
        w_in_view = w_in_d.rearrange("(k p) n -> p k n", p=128)
        for c in range(7):
            sc.dma("pool", w_in[:, :, c * 512:(c + 1) * 512], w_in_view[:, :, c * 512:(c + 1) * 512], writes=[r_w_in])
        rtab = ar.alloc([128, 20], F32, "rtab")
        sc.dma("sp", rtab[:], rtab_d[:, :], writes=[r_const])

        NB = 2
        xin = [ar.alloc([128, D], F32, "xin") for _ in range(NB)]
        r_xin = [Res("xin") for _ in range(NB)]
        tab = [ar.alloc([128, 192], F32, "tab") for _ in range(NB)]
        r_tab = [Res("tab") for _ in range(NB)]
        xb = [ar.alloc([128, D], BF16, "xb") for _ in range(NB)]
        r_xb = [Res("xb") for _ in range(NB)]
        xT = [ar.alloc([128, 8, 128], BF16, "xT") for _ in range(NB)]
        r_xT = [Res("xT") for _ in range(NB)]
        qa_b = [ar.alloc([128, 8, 64], BF16, "qa_b") for _ in range(NB)]
        ka_b = [ar.alloc([128, 8, 64], BF16, "ka_b") for _ in range(NB)]
        qr_b = [ar.alloc([128, 8, 64], BF16, "qr_b") for _ in range(NB)]
        kr_b = [ar.alloc([128, 8, 64], BF16, "kr_b") for _ in range(NB)]
        vr_b = [ar.alloc([128, 512], BF16, "vr_b") for _ in range(NB)]
        va_b = [ar.alloc([128, 8, VS], BF16, "va_b") for _ in range(NB)]
        gs_f = [ar.alloc([128, 512], F32, "gs_f") for _ in range(NB)]
        r_qa = [Res() for _ in range(NB)]
        r_ka = [Res() for _ in range(NB)]
        r_qr = [Res() for _ in range(NB)]
        r_kr = [Res() for _ in range(NB)]
        r_vr = [Res() for _ in range(NB)]
        r_va = [Res() for _ in range(NB)]
        r_gs = [Res() for _ in range(NB)]
        qaT = [ar.alloc([128, 4, 128], BF16, "qaT") for _ in range(NB)]
        kaT = [ar.alloc([128, 4, 128], BF16, "kaT") for _ in range(NB)]
        qrT = [ar.alloc([128, 4, 128], BF16, "qrT") for _ in range(NB)]
        krT = [ar.alloc([128, 4, 128], BF16, "krT") for _ in range(NB)]
        r_qaT = [Res() for _ in range(NB)]
        r_kaT = [Res() for _ in range(NB)]
        r_qrT = [Res() for _ in range(NB)]
        r_krT = [Res() for _ in range(NB)]
        rt_a = ar.alloc([128, 8, 64], F32, "rt_a")
        rt_b = ar.alloc([128, 8, 64], F32, "rt_b")
        r_rt = Res("rt")
        r_rtb = Res("rtb")
        for b in range(NB):
            sc.op("pool", lambda e, b=b: e.memset(va_b[b][:], 1.0), writes=[r_va[b]])

        def bc_heads(ap2d, n):
            return ap2d.unsqueeze(1).to_broadcast([128, 8, n])

        def rotary(pb, r_pb, dst, r_dst, tb, r_tb, c_off, n, post_scale=None):
            hn = n // 2
            p3 = pb.rearrange("p (h d) -> p h d", h=8)
            cc = bc_heads(tb[:, c_off:c_off + n], n)
            s1 = bc_heads(tb[:, c_off + n:c_off + n + hn], hn)
            s2 = bc_heads(tb[:, c_off + n + hn:c_off + 2 * n], hn)
            ta = rt_a[:, :, 0:n]
            tb_ = rt_b[:, :, 0:n]
            sc.op("dve", lambda e: e.tensor_tensor(out=ta, in0=p3[:, :, 0:n], in1=cc, op=ALU.mult),
                  reads=[r_tb, r_pb], writes=[r_rt])
            sc.op("dve", lambda e: e.tensor_tensor(out=rt_b[:, :, 0:hn], in0=p3[:, :, hn:n], in1=s1, op=ALU.mult),
                  reads=[r_tb, r_pb], writes=[r_rtb])
            sc.op("dve", lambda e: e.tensor_tensor(out=rt_b[:, :, hn:n], in0=p3[:, :, 0:hn], in1=s2, op=ALU.mult),
                  reads=[r_tb, r_pb, r_rtb], writes=[r_rtb])
            if post_scale is None:
                sc.op("dve", lambda e: e.tensor_tensor(out=dst[:, :, 0:n], in0=ta, in1=tb_, op=ALU.add),
                      reads=[r_rt, r_rtb], writes=[r_dst])
            else:
                sc.op("dve", lambda e: e.tensor_tensor(out=ta, in0=ta, in1=tb_, op=ALU.add),
                      reads=[r_rt, r_rtb], writes=[r_rt])
                ps = post_scale.unsqueeze(2).to_broadcast([128, 8, n])
                sc.op("dve", lambda e: e.tensor_tensor(out=dst[:, :, 0:n], in0=ta, in1=ps, op=ALU.mult),
                      reads=[r_rt, r_const], writes=[r_dst])

        import os
        CUT = int(os.environ.get('KCUT', '99'))
        def a1_post(t):
            b = t % NB
            tok = slice(t * 128, (t + 1) * 128)
            for (srcs, bk) in ((((qa_b, r_qa, qaT, r_qaT), (ka_b, r_ka, kaT, r_kaT)), 5), (((qr_b, r_qr, qrT, r_qrT), (kr_b, r_kr, krT, r_krT)), 6)):
                pv = bank_bf16(bk).rearrange("p (a c t) -> p a c t", a=2, c=4)
                fns = []
                rd = [r_const]
                for ai, (src, rsrc, dstT, rdst) in enumerate(srcs):
                    sflat = src[b][:].rearrange("p h d -> p (h d)")
                    rd.append(rsrc[b])
                    for c4 in range(4):
                        fns.append(lambda e, sflat=sflat, ai=ai, c4=c4, pv=pv: e.transpose(out=pv[:, ai, c4, :], in_=sflat[:, c4 * 128:(c4 + 1) * 128], identity=identb[:]))
                sc.ops("pe", fns, reads=rd, writes=[bres[bk]])
                if os.environ.get('KNOCOPY'):
                    continue
                for ai, (src, rsrc, dstT, rdst) in enumerate(srcs):
                    eng = "act" if bk == 5 else "dve"
                    if os.environ.get('KFORCE'):
                        eng = os.environ.get('KFORCE')
                    if os.environ.get('KONLY') and os.environ.get('KONLY') != eng:
                        continue
                    if eng == "act":
                        sc.op("act", lambda e, dstT=dstT, ai=ai, pv=pv, b=b: e.activation(out=dstT[b][:], in_=pv[:, ai, :, :], func=AF.Copy),
                              reads=[bres[bk]], writes=[rdst[b]])
                    else:
                        sc.op("dve", lambda e, dstT=dstT, ai=ai, pv=pv, b=b: e.tensor_copy(out=dstT[b][:], in_=pv[:, ai, :, :]),
                              reads=[bres[bk]], writes=[rdst[b]])
            sc.dma("sp", QAT[:, :, tok], qaT[b][:], reads=[r_qaT[b]])
            sc.dma("sp", KAT[:, :, tok], kaT[b][:], reads=[r_kaT[b]])
            sc.dma("sp", QRT[:, :, tok], qrT[b][:], reads=[r_qrT[b]])
            sc.dma("sp", KRT[:, :, tok], krT[b][:], reads=[r_krT[b]])
            sc.dma("sp", VA[tok, :], va_b[b][:].rearrange("p h d -> p (h d)"), reads=[r_va[b]])
            sc.dma("sp", KR[tok, :], kr_b[b][:].rearrange("p h d -> p (h d)"), reads=[r_kr[b]])
            sc.dma("sp", VR[tok, :], vr_b[b][:], reads=[r_vr[b]])
            sc.dma("sp", GS[tok, :], gs_f[b][:], reads=[r_gs[b]])

        for t in range(NT if CUT > 0 else 0):
            b = t % NB
            tok = slice(t * 128, (t + 1) * 128)
            sc.dma("sp", xin[b][:], x_d[tok, :], writes=[r_xin[b]])
            sc.dma("sp", tab[b][:], tab_d[tok, :], writes=[r_tab[b]])
            sc.op("pool", lambda e, b=b: e.tensor_copy(out=xb[b][:], in_=xin[b][:]), reads=[r_xin[b]], writes=[r_xb[b]])
            if CUT < 2:
                continue
            pT = bank_bf16(0).rearrange("p (k t) -> p k t", k=8)
            sc.ops("pe", [lambda e, b=b, k=k: e.transpose(out=pT[:, k, :], in_=xb[b][:, k * 128:(k + 1) * 128], identity=identb[:])
                          for k in range(8)], reads=[r_xb[b], r_const], writes=[bres[0]])
            sc.op("act", lambda e, b=b: e.activation(out=xT[b][:], in_=pT, func=AF.Copy), reads=[bres[0]], writes=[r_xT[b]])
            if CUT < 3:
                continue
            for c in range(7 if CUT > 3 else 0):
                bk = 1 + (c % 4)
                pb = bank_f32(bk)
                sc.ops("pe", [lambda e, b=b, k=k, c=c, pb=pb: e.matmul(pb, lhsT=xT[b][:, k, :], rhs=w_in[:, k, c * 512:(c + 1) * 512],
                                                                    start=(k == 0), stop=(k == 7)) for k in range(8)],
                       reads=[r_xT[b], r_w_in], writes=[bres[bk]])
                p3 = pb.rearrange("p (h d) -> p h d", h=8)
                if c == 0:
                    rotary(pb, bres[bk], qa_b[b], r_qa[b], tab[b], r_tab[b], 0, 16)
                    sc.op("act", lambda e, b=b, p3=p3: e.activation(out=qa_b[b][:, :, 16:64], in_=p3[:, :, 16:64], func=AF.Copy, scale=0.125),
                          reads=[bres[bk]], writes=[r_qa[b]])
                elif c == 1:
                    rotary(pb, bres[bk], ka_b[b], r_ka[b], tab[b], r_tab[b], 32, 16)
                    sc.op("act", lambda e, b=b, p3=p3: e.activation(out=ka_b[b][:, :, 16:64], in_=p3[:, :, 16:64], func=AF.Copy),
                          reads=[bres[bk]], writes=[r_ka[b]])
                elif c == 2:
                    sc.op("act", lambda e, b=b, p3=p3: e.activation(out=va_b[b][:, :, 0:64], in_=p3, func=AF.Copy),
                          reads=[bres[bk]], writes=[r_va[b]])
                elif c == 3:
                    rotary(pb, bres[bk], qr_b[b], r_qr[b], tab[b], r_tab[b], 64, 64)
                elif c == 4:
                    rotary(pb, bres[bk], kr_b[b], r_kr[b], tab[b], r_tab[b], 64, 64, post_scale=rtab[:, 0:8])
                elif c == 5:
                    sc.op("act", lambda e, b=b, pb=pb: e.activation(out=vr_b[b][:], in_=pb, func=AF.Copy),
                          reads=[bres[bk]], writes=[r_vr[b]])
                else:
                    sc.op("act", lambda e, b=b, pb=pb: e.activation(out=gs_f[b][:], in_=pb, func=AF.Silu),
                          reads=[bres[bk]], writes=[r_gs[b]])
            if t > 0:
                a1_post(t - 1)

        a1_post(NT - 1)
        sc.barrier()
        def mm(out, lhsT, rhs, start=True, stop=True):
            return lambda e: e.matmul(out, lhsT=lhsT, rhs=rhs, start=start, stop=stop)

        def tr(out, in_, ident):
            return lambda e: e.transpose(out=out, in_=in_, identity=ident)

        def actf(out, in_, func, **kw):
            return lambda e: e.activation(out=out, in_=in_, func=func, **kw)

        def tt(out, a, b_, op):
            return lambda e: e.tensor_tensor(out=out, in0=a, in1=b_, op=op)

        def ts(out, a, s1, s2, op0, op1=None):
            if op1 is None:
                return lambda e: e.tensor_scalar(out=out, in0=a, scalar1=s1, scalar2=None, op0=op0)
            return lambda e: e.tensor_scalar(out=out, in0=a, scalar1=s1, scalar2=s2, op0=op0, op1=op1)

        def stt(out, a, s, b_, op0, op1):
            return lambda e: e.scalar_tensor_tensor(out=out, in0=a, scalar=s, in1=b_, op0=op0, op1=op1)

        def cp(out, in_):
            return lambda e: e.tensor_copy(out=out, in_=in_)

        def red(out, in_, op=ALU.add):
            return lambda e: e.tensor_reduce(out=out, in_=in_, axis=AX.X, op=op)

        def layer_norm(src, r_src, dst, r_dst, g_ap, b_ap, tmp):
            st6, mv, rstd, nmr = tmp
            r_t = Res()
            sc.ops("dve", [lambda e: e.bn_stats(out=st6[:, 0:6], in_=src[:, 0:512]),
                           lambda e: e.bn_stats(out=st6[:, 6:12], in_=src[:, 512:1024])], reads=[r_src], writes=[r_t])
            sc.op("dve", lambda e: e.bn_aggr(out=mv[:, 0:2], in_=st6[:, 0:12]), reads=[r_t], writes=[r_t])
            sc.op("act", actf(rstd[:, 0:1], mv[:, 1:2], AF.Ln, bias=epsc[:, 0:1]), reads=[r_t, r_const], writes=[r_t])
            sc.op("act", actf(rstd[:, 0:1], rstd[:, 0:1], AF.Exp, scale=-0.5), reads=[r_t], writes=[r_t])
            sc.op("dve", stt(nmr[:, 0:1], mv[:, 0:1], -1.0, rstd[:, 0:1], ALU.mult, ALU.mult), reads=[r_t], writes=[r_t])
            sc.op("act", actf(dst, src, AF.Identity, bias=nmr[:, 0:1], scale=rstd[:, 0:1]), reads=[r_src, r_t], writes=[r_dst])
            sc.op("dve", tt(dst, dst, g_ap, ALU.mult), reads=[r_dst, r_const], writes=[r_dst])
            sc.op("dve", tt(dst, dst, b_ap, ALU.add), reads=[r_dst, r_const], writes=[r_dst])

        if stop_after != "A1":
            ar.reset(base_mark)
            w_out_b = ar.alloc([128, 8, D], BF16, "w_out_b")
            wq_b = ar.alloc([128, 8, D], BF16, "wq_b")
            wo_b = ar.alloc([128, 8, D], BF16, "wo_b")
            kmT = ar.alloc([128, 8, NMEM], BF16, "kmT")
            vm_b = ar.alloc([128, 2, D], BF16, "vm_b")
            lnp = ar.alloc([128, 4, D], F32, "lnp")
            rng = ar.alloc([128, 512], F32, "rng")
            am = ar.alloc([128, NOFF, 128], BF16, "am")
            cm = ar.alloc([128, 128], F32, "cm")
            rtab2 = ar.alloc([128, 20], F32, "rtab2")
            rw_f = ar.alloc([128, 8, NEXP], F32, "rw_f")
            rb_f = ar.alloc([1, NEXP], F32, "rb_f")
            ones_f = ar.alloc([128, 128], F32, "ones_f")
            ustr = ar.alloc([128, 128], F32, "ustr")
            rbase = ar.alloc([128, NEXP], F32, "rbase"); r_rbase = Res()
            rkt = [ar.alloc([128, NEXP], F32, "rkt") for _ in range(2)]; r_rkt = [Res(), Res()]
            ones_b = ar.alloc([128, 8], BF16, "ones_b")
            r_w2 = Res("w2")
            for (dst, src) in ((w_out_b, w_out_d), (wq_b, wq_d), (wo_b, wo_d)):
                v = src.rearrange("(k p) n -> p k n", p=128)
                for hh in range(2):
                    sc.dma("pool", dst[:, :, hh * 512:(hh + 1) * 512], v[:, :, hh * 512:(hh + 1) * 512], writes=[r_w2])
            sc.dma("sp", lnp[:], lnp_d[:, 0:4, :], writes=[r_const])
            sc.dma("sp", rng[:], rng_d[:, :], writes=[r_const])
            sc.dma("sp", am[:], am_d[:, :, :], writes=[r_const])
            sc.dma("sp", cm[:], cm_d[:, :], writes=[r_const])
            sc.dma("sp", rtab2[:], rtab_d[:, :], writes=[r_const])
            sc.dma("sp", rw_f[:], rw_d.rearrange("(k p) n -> p k n", p=128), writes=[r_const])
            sc.dma("sp", rb_f[:], rb_d[:, :], writes=[r_const])
            sc.op("pool", lambda e: e.memset(ones_f[:], 1.0), writes=[r_const])
            sc.op("pool", lambda e: e.memset(rbase[:], 0.0), writes=[r_rbase])
            sc.dma("sp", ustr[:], ustrict_d[:, :], writes=[r_const])
            sc.op("pool", lambda e: e.memset(ones_b[:], 1.0), writes=[r_const])
            m2 = ar.mark()
            wk_b = ar.alloc([128, 8, D], BF16, "wk_b")
            wv_b = ar.alloc([128, 8, D], BF16, "wv_b")
            memf = ar.alloc([128, 2, D], F32, "memf")
            memb = ar.alloc([128, 2, D], BF16, "memb")
            memT = ar.alloc([128, 8, NMEM], BF16, "memT")
            r_wkv = Res()
            r_memf = Res(); r_memb = Res(); r_memT = Res(); r_km = Res(); r_vm = Res()
            for (dst, src) in ((wk_b, wk_d), (wv_b, wv_d)):
                v = src.rearrange("(k p) n -> p k n", p=128)
                for hh in range(2):
                    sc.dma("pool", dst[:, :, hh * 512:(hh + 1) * 512], v[:, :, hh * 512:(hh + 1) * 512], writes=[r_wkv])
            sc.dma("sp", memf[:], mem_d.rearrange("(c p) d -> p c d", p=128), writes=[r_memf])
            sc.op("pool", cp(memb[:], memf[:]), reads=[r_memf], writes=[r_memb])
            for mc in range(2):
                pTm = bank_bf16(mc).rearrange("p (k t) -> p k t", k=8)
                sc.ops("pe", [tr(pTm[:, k, :], memb[:, mc, k * 128:(k + 1) * 128], identb[:]) for k in range(8)],
                       reads=[r_memb, r_const], writes=[bres[mc]])
                sc.op("act", actf(memT[:, :, mc * 128:(mc + 1) * 128], pTm, AF.Copy), reads=[bres[mc]], writes=[r_memT])
            for fc in range(8):
                bk = 2 + fc % 2
                pk = bank_f32(bk)[:, 0:NMEM]
                sc.ops("pe", [mm(pk, wk_b[:, k, fc * 128:(fc + 1) * 128], memT[:, k, :], k == 0, k == 7) for k in range(8)],
                       reads=[r_wkv, r_memT], writes=[bres[bk]])
                sc.op("act", actf(kmT[:, fc, :], pk, AF.Copy), reads=[bres[bk]], writes=[r_km])
            for mc in range(2):
                for hh in range(2):
                    bk = 4 + (mc * 2 + hh) % 2
                    pvv = bank_f32(bk)
                    sc.ops("pe", [mm(pvv, memT[:, k, mc * 128:(mc + 1) * 128], wv_b[:, k, hh * 512:(hh + 1) * 512], k == 0, k == 7) for k in range(8)],
                           reads=[r_wkv, r_memT], writes=[bres[bk]])
                    sc.op("act", actf(vm_b[:, mc, hh * 512:(hh + 1) * 512], pvv, AF.Copy), reads=[bres[bk]], writes=[r_vm])
            sc.barrier()
            ar.reset(m2)
            R = 18
            kring = ar.alloc([128, 4, R * 128], BF16, "kring")
            vring = ar.alloc([128, R, 8 * VS], BF16, "vring")
            r_kring = [Res() for _ in range(R)]
            r_vring = [Res() for _ in range(R)]
            NB2 = 2
            def dbl(shape, dt, name):
                return [ar.alloc(shape, dt, name) for _ in range(NB2)], [Res(name) for _ in range(NB2)]
            qaT2, r_qaT2 = dbl([128, 4, 128], BF16, "qaT2")
            qrT2, r_qrT2 = dbl([128, 4, 128], BF16, "qrT2")
            krT2, r_krT2 = dbl([128, 4, 128], BF16, "krT2")
            kr2, r_kr2 = dbl([128, 512], BF16, "kr2")
            vr2, r_vr2 = dbl([128, 512], BF16, "vr2")
            gs2, r_gs2 = dbl([128, 512], F32, "gs2")
            xin2, r_xin2 = dbl([128, D], F32, "xin2")
            NSB = 3
            SBK = [0, 1, 4]
            Et = [ar.alloc([128, 512], BF16, "Et") for _ in range(NSB)]; r_Et = [Res() for _ in range(NSB)]
            Pt = [ar.alloc([128, 512], BF16, "Pt") for _ in range(NSB)]; r_Pt = [Res() for _ in range(NSB)]
            rec8 = ar.alloc([128, 8], F32, "rec8"); r_rec8 = Res()
            mixed_b = ar.alloc([128, D], BF16, "mixed_b"); r_mixa = Res(); r_mixr = Res()
            mixedT = ar.alloc([128, 8, 128], BF16, "mixedT"); r_mixedT = Res()
            Pr = ar.alloc([128, 8, 128], BF16, "Pr"); r_Pr = Res()
            state_f = ar.alloc([128, 4, 128], F32, "state_f"); r_state_f = Res()
            state_b = ar.alloc([128, 4, 128], BF16, "state_b"); r_state_b = Res()
            yv = ar.alloc([128, 8, 64], F32, "yv"); r_yv = Res()
            ysq = ar.alloc([128, 8, 64], F32, "ysq"); r_ysq = Res()
            st8 = ar.alloc([128, 32], F32, "st8"); r_st8 = Res()
            r1 = ar.alloc([128, D], F32, "r1"); r_r1 = Res()
            x1 = ar.alloc([128, D], F32, "x1"); r_x1 = Res()
            x1_b = ar.alloc([128, D], BF16, "x1_b"); r_x1b = Res()
            x1T = ar.alloc([128, 8, 128], BF16, "x1T"); r_x1T = Res()
            qcT = ar.alloc([128, 8, 128], BF16, "qcT"); r_qcT = Res()
            ET = ar.alloc([128, 8, 128], BF16, "ET"); r_ET = Res()
            oc_b = ar.alloc([128, D], BF16, "oc_b"); r_ocb = Res()
            ocT = ar.alloc([128, 8, 128], BF16, "ocT"); r_ocT = Res()
            rec4 = ar.alloc([128, 4], F32, "rec4"); r_rec4 = Res()
            r2 = r1; r_r2 = r_r1
            x2s, r_x2s = dbl([128, D], F32, "x2s")
            x2_b = ar.alloc([128, D], BF16, "x2_b"); r_x2b = Res()
            x2T_b, r_x2Tb = dbl([128, 8, 128], BF16, "x2T_b")
            x2T_f = ar.alloc([128, 8, 128], F32, "x2T_f"); r_x2Tf = Res()
            lg = ar.alloc([128, NEXP], F32, "lg"); r_lg = Res()
            mx8 = ar.alloc([128, 8], F32, "mx8")
            msk = ar.alloc([128, NEXP], F32, "msk")
            ex = ar.alloc([128, NEXP], F32, "ex")
            sm = ar.alloc([128, 4], F32, "sm")
            gat, r_gat = dbl([128, NEXP], F32, "gat")
            lntmp = (ar.alloc([128, 12], F32, "st6"), ar.alloc([128, 2], F32, "mv"), ar.alloc([128, 1], F32, "rstd"), ar.alloc([128, 1], F32, "nmr"))
            sc.op("pool", lambda e: e.memset(state_f[:], 0.0), writes=[r_state_f])
            sc.op("pool", lambda e: e.memset(state_b[:], 0.0), writes=[r_state_b])
            kfac_t = rtab2[:, 0:8]; qdec_t = rtab2[:, 8:16]; cdp_t = rtab2[:, 16:20]
            r_rt2 = Res()
            print("A2 arena top", ar.off, "kring off", kring.offset, "qaT2", qaT2[0].offset, qaT2[1].offset)

            CUT2 = int(os.environ.get('KCUT2', '99'))
            KATT = int(os.environ.get('KATT', '99'))
            KRET = int(os.environ.get('KRET', '99'))
            def a2_loads(t):
                b = t % NB2
                tok = slice(t * 128, (t + 1) * 128)
                slot = t % R
                sc.dma("sp", qaT2[b][:], QAT[:, :, tok], writes=[r_qaT2[b]])
                sc.dma("sp", kring[:, :, slot * 128:(slot + 1) * 128], KAT[:, :, tok], writes=[r_kring[slot]])
                sc.dma("sp", vring[:, slot, :], VA[tok, :], writes=[r_vring[slot]])
                sc.dma("sp", qrT2[b][:], QRT[:, :, tok], writes=[r_qrT2[b]])
                sc.dma("sp", krT2[b][:], KRT[:, :, tok], writes=[r_krT2[b]])
                sc.dma("sp", kr2[b][:], KR[tok, :], writes=[r_kr2[b]])
                sc.dma("sp", vr2[b][:], VR[tok, :], writes=[r_vr2[b]])
                sc.dma("sp", gs2[b][:], GS[tok, :], writes=[r_gs2[b]])
                sc.dma("sp", xin2[b][:], x_d[tok, :], writes=[r_xin2[b]])

            def a2_att_thunks(t):
                b = t % NB2
                tok = slice(t * 128, (t + 1) * 128)
                slot = t % R
                kbs = list(range(max(0, t - 16), t + 1))
                steps = []
                for h in range(0, 8, 2 if os.environ.get('KEVEN') else 1):
                    groups = [kbs[i:i + 4] for i in range(0, len(kbs), 4)]
                    for gi, g in enumerate(groups):
                        steps.append((h, g, gi == 0, gi == len(groups) - 1))
                Ob = [bank_f32(2)[:, 0:4 * VS].rearrange("p (h d) -> p h d", h=4), bank_f32(3)[:, 0:4 * VS].rearrange("p (h d) -> p h d", h=4)]

                def stageA(i, st):
                    h, g, first, last = st
                    si = i % NSB
                    sb = SBK[si]
                    p, hb = h // 2, 64 * (h % 2)
                    n = len(g)
                    Sb = bank_f32(sb)
                    if os.environ.get('KFULLK') == '2':
                        fns = [mm(Sb[:, j * 128:(j + 1) * 128], kmT[:, 0, 0:128], kmT[:, 1, 0:128])
                               for j, kb in enumerate(g)]
                    elif os.environ.get('KFULLK') == '3':
                        fns = [mm(Sb[:, j * 128:(j + 1) * 128], kmT[:, 0, 0:128], qaT2[b][:, p, :])
                               for j, kb in enumerate(g)]
                    elif os.environ.get('KFULLK'):
                        fns = [mm(Sb[:, j * 128:(j + 1) * 128], kring[:, p, (kb % R) * 128:(kb % R + 1) * 128], qaT2[b][:, p, :])
                               for j, kb in enumerate(g)]
                    else:
                        fns = [mm(Sb[:, j * 128:(j + 1) * 128], kring[hb:hb + 64, p, (kb % R) * 128:(kb % R + 1) * 128], qaT2[b][hb:hb + 64, p, :])
                               for j, kb in enumerate(g)]
                    if os.environ.get('KNOREADS'):
                        sc.ops("pe", fns, reads=[], writes=[bres[sb]])
                    else:
                        sc.ops("pe", fns, reads=[r_qaT2[b]] + [r_kring[kb % R] for kb in g], writes=[bres[sb]])
                    if KATT < 2:
                        return
                    sc.op("act", actf(Et[si][:, 0:n * 128], Sb[:, 0:n * 128], AF.Exp), reads=[bres[sb]], writes=[r_Et[si]])
                    if KATT < 3:
                        return
                    j0 = g[0] - t + 16
                    msk_ap = am[:, j0:j0 + n, :].rearrange("p j q -> p (j q)")
                    eng = "dve"
                    sc.op(eng, tt(Pt[si][:, 0:n * 128], Et[si][:, 0:n * 128], msk_ap, ALU.mult), reads=[r_Et[si], r_const], writes=[r_Pt[si]])

                def stageB(i, st):
                    if KATT < 4:
                        return
                    h, g, first, last = st
                    si = i % NSB
                    fns = []
                    for j, kb in enumerate(g):
                        fns.append(mm(Ob[h // 4][:, h % 4, :], Pt[si][:, j * 128:(j + 1) * 128], vring[:, kb % R, h * VS:(h + 1) * VS],
                                      first and j == 0, last and j == len(g) - 1))
                    sc.ops("pe", fns, reads=[r_Pt[si]] + [r_vring[kb % R] for kb in g], writes=[bres[2 + h // 4]])

                def att_final():
                    for i_ in range(max(0, len(steps) - (NSB - 1)), len(steps)):
                        stageB(i_, steps[i_])
                    mix3 = mixed_b[:, 0:512].rearrange("p (h d) -> p h d", h=8)
                    for hh in range(2):
                        sc.op("dve", lambda e, hh=hh: e.reciprocal(out=rec8[:, hh * 4:(hh + 1) * 4], in_=Ob[hh][:, :, 64]), reads=[bres[2 + hh]], writes=[r_rec8])
                        sc.op("dve", tt(mix3[:, hh * 4:(hh + 1) * 4, :], Ob[hh][:, :, 0:64], rec8[:, hh * 4:(hh + 1) * 4].unsqueeze(2).to_broadcast([128, 4, 64]), ALU.mult),
                              reads=[bres[2 + hh], r_rec8], writes=[r_mixa])

                thunks = []
                for i, st in enumerate(steps):
                    def th(i=i, st=st):
                        stageA(i, st)
                        if i >= NSB - 1:
                            stageB(i - (NSB - 1), steps[i - (NSB - 1)])
                    thunks.append(th)
                thunks.append(att_final)
                return thunks

            def a2_ret(t):
                b = t % NB2
                tok = slice(t * 128, (t + 1) * 128)
                slot = t % R
                Sr = [bank_f32(4).rearrange("p (h q) -> p h q", h=4), bank_f32(5).rearrange("p (h q) -> p h q", h=4)]
                for hh in range(2):
                    fns = []
                    for hl in range(4):
                        h = hh * 4 + hl
                        p, hb = h // 2, 64 * (h % 2)
                        fns.append(mm(Sr[hh][:, hl, :], krT2[b][hb:hb + 64, p, :], qrT2[b][hb:hb + 64, p, :]))
                        fns.append(mm(bank_f32(1)[:, 0:128], identb[:], identb[:]))
                    sc.ops("pe", fns, reads=[r_krT2[b], r_qrT2[b], r_const], writes=[bres[4 + hh], bres[1]])
                    sc.op("dve", tt(Pr[:, hh * 4:(hh + 1) * 4, :], Sr[hh], cm[:, :].unsqueeze(1).to_broadcast([128, 4, 128]), ALU.mult),
                          reads=[bres[4 + hh], r_const], writes=[r_Pr])
                Rb = bank_f32(6).rearrange("p (h d) -> p h d", h=8)
                Cb = bank_f32(0).rearrange("p (h d) -> p h d", h=8)
                fns = []
                for h in range(8):
                    fns.append(mm(Rb[:, h, :], Pr[:, h, :], vr2[b][:, h * 64:(h + 1) * 64], True, True))
                sc.ops("pe", fns, reads=[r_Pr, r_vr2[b]], writes=[bres[6]])
                fns = []
                for h in range(8):
                    p, hb = h // 2, 64 * (h % 2)
                    fns.append(mm(Cb[:, h, :], qrT2[b][hb:hb + 64, p, :], state_b[hb:hb + 64, p, hb:hb + 64], True, True))
                    fns.append(mm(bank_f32(1)[:, 0:128], identb[:], identb[:]))
                sc.ops("pe", fns, reads=[r_qrT2[b], r_state_b, r_const], writes=[bres[0], bres[1]])
                KVb = bank_f32(7).rearrange("p (a c) -> p a c", a=4)
                sc.ops("pe", [mm(KVb[:, p, :], kr2[b][:, p * 128:(p + 1) * 128], vr2[b][:, p * 128:(p + 1) * 128]) for p in range(4)],
                       reads=[r_kr2[b], r_vr2[b]], writes=[bres[7]])
                sc.op("dve", tt(state_f[:], state_f[:], KVb, ALU.add), reads=[bres[7], r_state_f], writes=[r_state_f])
                sc.op("dve", tt(state_f[:], state_f[:], cdp_t.unsqueeze(2).to_broadcast([128, 4, 128]), ALU.mult), reads=[r_state_f, r_const], writes=[r_state_f])
                sc.op("act", actf(state_b[:], state_f[:], AF.Copy), reads=[r_state_f], writes=[r_state_b])
                sc.op("dve", tt(yv[:], Rb, qdec_t.unsqueeze(2).to_broadcast([128, 8, 64]), ALU.mult), reads=[bres[6], r_const], writes=[r_yv])
                sc.op("dve", tt(ysq[:], Cb, qdec_t.unsqueeze(2).to_broadcast([128, 8, 64]), ALU.mult), reads=[bres[0], r_const], writes=[r_ysq])
                sc.op("dve", tt(yv[:], yv[:], ysq[:], ALU.add), reads=[r_yv, r_ysq], writes=[r_yv])
                sc.op("dve", red(st8[:, 0:8], yv[:]), reads=[r_yv], writes=[r_st8])
                sc.op("dve", ts(st8[:, 8:16], st8[:, 0:8], 1.0 / 64.0, None, ALU.mult), reads=[r_st8], writes=[r_st8])
                sc.op("dve", tt(yv[:], yv[:], st8[:, 8:16].unsqueeze(2).to_broadcast([128, 8, 64]), ALU.subtract), reads=[r_yv, r_st8], writes=[r_yv])
                sc.op("dve", tt(ysq[:], yv[:], yv[:], ALU.mult), reads=[r_yv], writes=[r_ysq])
                sc.op("dve", red(st8[:, 16:24], ysq[:]), reads=[r_ysq], writes=[r_st8])
                sc.op("act", actf(st8[:, 24:32], st8[:, 16:24], AF.Ln, bias=epsc[:, 0:1], scale=1.0 / 64.0), reads=[r_st8, r_const], writes=[r_st8])
                sc.op("act", actf(st8[:, 24:32], st8[:, 24:32], AF.Exp, scale=-0.5), reads=[r_st8], writes=[r_st8])
                sc.op("dve", tt(yv[:], yv[:], st8[:, 24:32].unsqueeze(2).to_broadcast([128, 8, 64]), ALU.mult), reads=[r_yv, r_st8], writes=[r_yv])
                yflat = yv[:].rearrange("p h d -> p (h d)")
                sc.op("dve", tt(yflat, yflat, rng[:], ALU.mult), reads=[r_yv, r_const], writes=[r_yv])
                sc.op("dve", tt(mixed_b[:, 512:1024], yflat, gs2[b][:], ALU.mult), reads=[r_yv, r_gs2[b]], writes=[r_mixr])

            def a2_tail(t):
                b = t % NB2
                tok = slice(t * 128, (t + 1) * 128)
                slot = t % R
                pTm = bank_bf16(7).rearrange("p (k t) -> p k t", k=8)
                sc.ops("pe", [tr(pTm[:, k, :], mixed_b[:, k * 128:(k + 1) * 128], identb[:]) for k in range(8)],
                       reads=[r_mixa, r_mixr, r_const], writes=[bres[7]])
                sc.op("act", actf(mixedT[:], pTm, AF.Copy), reads=[bres[7]], writes=[r_mixedT])
                yield
                for hh in range(2):
                    pb = bank_f32(5 + hh)
                    sc.ops("pe", [mm(pb, mixedT[:, k, :], w_out_b[:, k, hh * 512:(hh + 1) * 512], k == 0, k == 7) for k in range(8)],
                           reads=[r_mixedT, r_w2], writes=[bres[5 + hh]])
                    sc.op("dve", stt(r1[:, hh * 512:(hh + 1) * 512], xin2[b][:, hh * 512:(hh + 1) * 512], ALPHA, pb, ALU.mult, ALU.add),
                          reads=[bres[5 + hh], r_xin2[b]], writes=[r_r1])
                    yield
                layer_norm(r1[:], r_r1, x1[:], r_x1, lnp[:, 0, :], lnp[:, 1, :], lntmp)
                yield
                sc.op("act", actf(x1_b[:], x1[:], AF.Copy), reads=[r_x1], writes=[r_x1b])
                yield
                pTm = bank_bf16(7).rearrange("p (k t) -> p k t", k=8)
                sc.ops("pe", [tr(pTm[:, k, :], x1_b[:, k * 128:(k + 1) * 128], identb[:]) for k in range(8)], reads=[r_x1b, r_const], writes=[bres[7]])
                sc.op("act", actf(x1T[:], pTm, AF.Copy), reads=[bres[7]], writes=[r_x1T])
                yield
                for hh in range(2):
                    Qb = bank_f32([7, 5][hh]).rearrange("p (c t) -> p c t", c=4)
                    fns = []
                    for fl in range(4):
                        fc = hh * 4 + fl
                        fns += [mm(Qb[:, fl, :], wq_b[:, k, fc * 128:(fc + 1) * 128], x1T[:, k, :], k == 0, k == 7) for k in range(8)]
                    sc.ops("pe", fns, reads=[r_x1T, r_w2], writes=[bres[[7, 5][hh]]])
                    sc.op("act", actf(qcT[:, hh * 4:(hh + 1) * 4, :], Qb, AF.Copy, scale=1.0 / 16.0), reads=[bres[[7, 5][hh]]], writes=[r_qcT])
                    yield
                for hh in range(2):
                    Scb = bank_f32(6 + hh).rearrange("p (c t) -> p c t", c=4)
                    fns = []
                    for il in range(4):
                        idx = hh * 4 + il
                        hm, mc = idx // 2, idx % 2
                        for c in range(2):
                            fns.append(mm(Scb[:, il, :], kmT[:, 2 * hm + c, mc * 128:(mc + 1) * 128], qcT[:, 2 * hm + c, :], c == 0, c == 1))
                    sc.ops("pe", fns, reads=[r_qcT, r_km], writes=[bres[6 + hh]])
                    sc.op("act", actf(ET[:, hh * 4:(hh + 1) * 4, :], Scb, AF.Exp), reads=[bres[6 + hh]], writes=[r_ET])
                    yield
                denb = bank_f32(6)[:, 0:32].rearrange("p (h c) -> p h c", h=4)
                fns = []
                for hm in range(4):
                    for mc in range(2):
                        fns.append(mm(denb[:, hm, :], ET[:, hm * 2 + mc, :], ones_b[:, 0:8], mc == 0, mc == 1))
                sc.ops("pe", fns, reads=[r_ET, r_const], writes=[bres[6]])
                sc.op("dve", lambda e: e.reciprocal(out=rec4[:, :], in_=denb[:, :, 0]), reads=[bres[6]], writes=[r_rec4])
                yield
                for hh in range(2):
                    Ocb = bank_f32([7, 5][hh]).rearrange("p (c t) -> p c t", c=2)
                    fns = []
                    for hl in range(2):
                        hm = hh * 2 + hl
                        for mc in range(2):
                            fns.append(mm(Ocb[:, hl, :], ET[:, hm * 2 + mc, :], vm_b[:, mc, hm * 256:(hm + 1) * 256], mc == 0, mc == 1))
                    sc.ops("pe", fns, reads=[r_ET, r_vm], writes=[bres[[7, 5][hh]]])
                    sc.op("dve", tt(oc_b[:, hh * 512:(hh + 1) * 512].rearrange("p (c t) -> p c t", c=2), Ocb,
                                    rec4[:, hh * 2:(hh + 1) * 2].unsqueeze(2).to_broadcast([128, 2, 256]), ALU.mult),
                          reads=[bres[[7, 5][hh]], r_rec4], writes=[r_ocb])
                    yield
                pTm = bank_bf16(7).rearrange("p (k t) -> p k t", k=8)
                sc.ops("pe", [tr(pTm[:, k, :], oc_b[:, k * 128:(k + 1) * 128], identb[:]) for k in range(8)], reads=[r_ocb, r_const], writes=[bres[7]])
                sc.op("act", actf(ocT[:], pTm, AF.Copy), reads=[bres[7]], writes=[r_ocT])
                yield
                for hh in range(2):
                    pb = bank_f32(6 + hh)
                    sc.ops("pe", [mm(pb, ocT[:, k, :], wo_b[:, k, hh * 512:(hh + 1) * 512], k == 0, k == 7) for k in range(8)],
                           reads=[r_ocT, r_w2], writes=[bres[6 + hh]])
                    sc.op("dve", stt(r2[:, hh * 512:(hh + 1) * 512], x1[:, hh * 512:(hh + 1) * 512], ALPHA, pb, ALU.mult, ALU.add),
                          reads=[bres[6 + hh], r_x1], writes=[r_r2])
                    yield
                layer_norm(r2[:], r_r2, x2s[b][:], r_x2s[b], lnp[:, 2, :], lnp[:, 3, :], lntmp)
                yield
                sc.dma("sp", X2[tok, :], x2s[b][:], reads=[r_x2s[b]])
                sc.op("act", actf(x2_b[:], x2s[b][:], AF.Copy), reads=[r_x2s[b]], writes=[r_x2b])
                yield
                sc.dma("sp", X2B[tok, :], x2_b[:], reads=[r_x2b])
                pTm = bank_bf16(7).rearrange("p (k t) -> p k t", k=8)
                sc.ops("pe", [tr(pTm[:, k, :], x2_b[:, k * 128:(k + 1) * 128], identb[:]) for k in range(8)], reads=[r_x2b, r_const], writes=[bres[7]])
                sc.op("act", actf(x2T_b[b][:], pTm, AF.Copy), reads=[bres[7]], writes=[r_x2Tb[b]])
                yield
                sc.dma("sp", X2T[:, :, tok], x2T_b[b][:], reads=[r_x2Tb[b]])
                for hh in range(2):
                    pTf = bank_f32(5 + hh).rearrange("p (k t) -> p k t", k=4)
                    sc.ops("pe", [tr(pTf[:, k, :], x2s[b][:, (hh * 4 + k) * 128:(hh * 4 + k + 1) * 128], identf[:]) for k in range(4)],
                           reads=[r_x2s[b], r_const], writes=[bres[5 + hh]])
                    sc.op("dve", cp(x2T_f[:, hh * 4:(hh + 1) * 4, :], pTf), reads=[bres[5 + hh]], writes=[r_x2Tf])
                    yield
                lgb = bank_f32(7)[:, 0:NEXP]
                fns = [mm(lgb, x2T_f[:, k, :], rw_f[:, k, :], k == 0, False) for k in range(8)]
                fns.append(mm(lgb, ones_f[0:1, :], rb_f[0:1, :], False, True))
                sc.ops("pe", fns, reads=[r_x2Tf, r_const], writes=[bres[7]])
                sc.op("dve", cp(lg[:], lgb), reads=[bres[7]], writes=[r_lg])
                yield
                sc.op("dve", lambda e: e.max(out=mx8[:, 0:8], in_=lg[:, :]), reads=[r_lg], writes=[r_rt2])
                yield
                sc.op("dve", ts(msk[:], lg[:], mx8[:, 3:4], None, ALU.is_ge), reads=[r_lg, r_rt2], writes=[r_rt2])
                yield
                sc.op("dve", ts(sm[:, 0:1], mx8[:, 0:1], -1.0, None, ALU.mult), reads=[r_rt2], writes=[r_rt2])
                yield
                sc.op("act", actf(ex[:], lg[:], AF.Exp, bias=sm[:, 0:1]), reads=[r_lg, r_rt2], writes=[r_rt2])
                yield
                sc.op("dve", tt(ex[:], ex[:], msk[:], ALU.mult), reads=[r_rt2], writes=[r_rt2])
                yield
                sc.op("dve", red(sm[:, 1:2], ex[:]), reads=[r_rt2], writes=[r_rt2])
                yield
                sc.op("dve", lambda e: e.reciprocal(out=sm[:, 2:3], in_=sm[:, 1:2]), reads=[r_rt2], writes=[r_rt2])
                yield
                sc.op("dve", ts(gat[b][:], ex[:], sm[:, 2:3], None, ALU.mult), reads=[r_rt2], writes=[r_gat[b]])
                yield
                sc.dma("sp", GT[tok, :], gat[b][:], reads=[r_gat[b]])
                rkb = bank_f32(7)[:, 32:96]
                sc.ops("pe", [mm(rkb[:, 0:32], ustr[:], msk[:]), mm(rkb[:, 32:64], ones_f[:], msk[:])], reads=[r_rt2, r_const], writes=[bres[7]])
                sc.op("dve", tt(rkt[b][:], rbase[:], rkb[:, 0:32], ALU.add), reads=[bres[7], r_rbase], writes=[r_rkt[b]])
                yield
                sc.op("dve", tt(rbase[:], rbase[:], rkb[:, 32:64], ALU.add), reads=[bres[7], r_rbase], writes=[r_rbase])
                yield
                sc.dma("sp", RKD[tok, :], rkt[b][:], reads=[r_rkt[b]])
                yield

            a2_loads(0)
            for th in a2_att_thunks(0):
                th()
            a2_ret(0)
            for t in range(NT):
                gen = a2_tail(t)
                next(gen)
                alive = True
                if t + 1 < NT:
                    a2_loads(t + 1)
                    for th in a2_att_thunks(t + 1):
                        th()
                        if alive:
                            try:
                                next(gen)
                            except StopIteration:
                                alive = False
                for _ in gen:
                    pass
                if t + 1 < NT:
                    a2_ret(t + 1)
            sc.dma("sp", CNT[:, :], rbase[:], reads=[r_rbase])
            sc.barrier()

        if stop_after == "B" and SPARSE:
            I32 = mybir.dt.int32
            ar.reset(base_mark)
            slk_i = ar.alloc([128, NT, 4], I32, "slk_i"); r_slk = Res()
            wk = ar.alloc([128, NT, 4], F32, "wk"); r_wk = Res()
            widx_i = ar.alloc([128, NBLK, 8], I32, "widx_i"); r_widx = Res()
            ebf = ar.alloc([128, NBLK], F32, "ebf"); r_ebf = Res()
            widx2_i = ar.alloc([128, NBLK], I32, "widx2_i"); r_widx2 = Res()
            cmeta = ar.alloc([128, CMW], F32, "cmeta")
            bgT = ar.alloc([128, NEXP, 8], F32, "bgT"); buT = ar.alloc([128, NEXP, 8], F32, "buT")
            bd32 = ar.alloc([32, D], F32, "bd32"); bd16 = ar.alloc([32, D], BF16, "bd16")
            lnp3 = ar.alloc([128, 2, D], F32, "lnp3")
            c7 = ar.alloc([128, 512], F32, "c7")
            o_ = 0
            thr16 = cmeta[:, o_:o_ + 16]; o_ += 17
            blkthr = cmeta[:, o_:o_ + NBLK]; o_ += NBLK
            iota_e = cmeta[:, o_:o_ + NEXP]; o_ += NEXP
            kcp = cmeta[:, o_:o_ + 8]; o_ += 8
            iota_p = cmeta[:, o_:o_ + 1]; o_ += 1
            tril = cmeta[:, o_:o_ + NEXP * NEXP].rearrange("p (a b) -> p a b", a=NEXP)
            sc.dma("sp", cmeta[:], cmeta_d[:, :], writes=[r_const])
            sc.dma("sp", bgT[:], bgT_d[:, :, :], writes=[r_const])
            sc.dma("sp", buT[:], buT_d[:, :, :], writes=[r_const])
            sc.dma("sp", lnp3[:], lnp_d[:, 4:6, :], writes=[r_const])
            sc.dma("sp", bd32[:], bd_d[:, :], writes=[r_const])
            sc.op("pool", lambda e: e.memset(c7[:], 7.0), writes=[r_const])
            sc.op("dve", ts(buT[:], buT[:], 7.0, None, ALU.add), reads=[r_const], writes=[r_const])
            sc.op("dve", cp(bd16[:], bd32[:]), reads=[r_const], writes=[r_const])
            mB = ar.mark()
            oh = ar.alloc([128, NBLK, NEXP], F32, "oh"); r_oh = Res()
            rankA = ar.alloc([128, NT, NEXP], F32, "rankA"); r_rankA = Res()
            gatA = ar.alloc([128, NT, NEXP], F32, "gatA"); r_gatA = Res()
            maskA = ar.alloc([128, NT, NEXP], F32, "maskA"); r_maskA = Res()
            top8A = ar.alloc([128, NT, 8], F32, "top8A"); r_top8 = Res()
            cntt = ar.alloc([128, NEXP], F32, "cntt"); r_cnt = Res()
            cmp16 = ar.alloc([128, NEXP, 16], F32, "cmp16")
            t32 = ar.alloc([128, NEXP, NEXP], F32, "t32")
            nblk_t = ar.alloc([128, NEXP], F32, "nblk_t"); padded = ar.alloc([128, NEXP], F32, "padded")
            pend = ar.alloc([128, NEXP], F32, "pend"); pstart = ar.alloc([128, NEXP], F32, "pstart")
            eq = ar.alloc([128, 4, NEXP], F32, "eq"); r_eq = Res()
            xs_t = [ar.alloc([128, D], BF16, "xs_t") for _ in range(3)]; r_xs = [Res() for _ in range(3)]
            r_m = Res()
            sc.dma("sp", rankA[:], RKD.rearrange("(t p) e -> p t e", p=128), writes=[r_rankA])
            sc.dma("sp", gatA[:], GT.rearrange("(t p) e -> p t e", p=128), writes=[r_gatA])
            sc.dma("sp", cntt[:], CNT[:, :], writes=[r_cnt])
            sc.op("dve", tt(cmp16[:], cntt[:, :].unsqueeze(2).to_broadcast([128, NEXP, 16]), thr16.unsqueeze(1).to_broadcast([128, NEXP, 16]), ALU.is_gt),
                  reads=[r_cnt, r_const], writes=[r_m])
            sc.op("dve", red(nblk_t[:], cmp16[:]), reads=[r_m], writes=[r_m])
            sc.op("dve", ts(padded[:], nblk_t[:], 512.0, None, ALU.mult), reads=[r_m], writes=[r_m])
            sc.op("dve", tt(t32[:], tril, padded[:, :].unsqueeze(1).to_broadcast([128, NEXP, NEXP]), ALU.mult), reads=[r_m, r_const], writes=[r_m])
            sc.op("dve", red(pend[:], t32[:]), reads=[r_m], writes=[r_m])
            sc.op("dve", tt(pstart[:], pend[:], padded[:], ALU.subtract), reads=[r_m], writes=[r_m])
            sc.op("dve", tt(oh[:], pend[:, :].unsqueeze(1).to_broadcast([128, NBLK, NEXP]), blkthr.unsqueeze(2).to_broadcast([128, NBLK, NEXP]), ALU.is_le),
                  reads=[r_m, r_const], writes=[r_oh])
            sc.op("dve", red(ebf[:], oh[:]), reads=[r_oh], writes=[r_ebf])
            sc.op("dve", ts(ebf[:], ebf[:], float(NEXP - 1), None, ALU.min), reads=[r_ebf], writes=[r_ebf])
            sc.op("dve", stt(widx_i[:], ebf[:, :].unsqueeze(2).to_broadcast([128, NBLK, 8]), 1024.0, kcp.unsqueeze(1).to_broadcast([128, NBLK, 8]), ALU.mult, ALU.add),
                  reads=[r_ebf, r_const], writes=[r_widx])
            sc.op("dve", stt(widx2_i[:], ebf[:], 128.0, iota_p.to_broadcast([128, NBLK]), ALU.mult, ALU.add), reads=[r_ebf, r_const], writes=[r_widx2])
            sc.op("dve", tt(rankA[:], rankA[:], pstart[:, :].unsqueeze(1).to_broadcast([128, NT, NEXP]), ALU.add), reads=[r_rankA, r_m], writes=[r_rankA])
            sc.op("dve", ts(maskA[:], gatA[:], 0.0, None, ALU.is_gt), reads=[r_gatA], writes=[r_maskA])
            sc.op("dve", stt(rankA[:], rankA[:], 1.0, maskA[:], ALU.add, ALU.mult), reads=[r_rankA, r_maskA], writes=[r_rankA])
            for t in range(NT):
                sc.op("dve", lambda e, t=t: e.max(out=top8A[:, t, :], in_=rankA[:, t, :]), reads=[r_rankA], writes=[r_top8])
            sc.op("dve", ts(slk_i[:], top8A[:, :, 0:4], -1.0, None, ALU.add), reads=[r_top8], writes=[r_slk])
            for t in range(NT):
                sc.op("dve", tt(eq[:], rankA[:, t, :].unsqueeze(1).to_broadcast([128, 4, NEXP]), top8A[:, t, 0:4].unsqueeze(2).to_broadcast([128, 4, NEXP]), ALU.is_equal),
                      reads=[r_rankA, r_top8], writes=[r_eq])
                sc.op("dve", tt(eq[:], eq[:], gatA[:, t, :].unsqueeze(1).to_broadcast([128, 4, NEXP]), ALU.mult), reads=[r_eq, r_gatA], writes=[r_eq])
                sc.op("dve", red(wk[:, t, :], eq[:]), reads=[r_eq], writes=[r_wk])
            zt = ar.alloc([128, 4, D], BF16, "zt"); r_zt = Res()
            sc.op("pool", lambda e: e.memset(zt[:], 0.0), writes=[r_zt])
            hz = {}
            for zb in range(NBLK):
                hh_ = sc.dma("sp", XS[zb * 512:(zb + 1) * 512, :].rearrange("(i p) d -> p i d", p=128), zt[:], reads=[r_zt])
                for k_, v_ in hh_.items():
                    hz[k_] = max(hz.get(k_, 0), v_)
            for t in range(NT):
                xb_ = t % 3
                sc.dma("sp", xs_t[xb_][:], X2B[t * 128:(t + 1) * 128, :], writes=[r_xs[xb_]])
                for k4 in range(4):
                    sc.dma_fn("pool", lambda e, xb_=xb_, t=t, k4=k4: e.indirect_dma_start(
                        out=XS[:, :], out_offset=bass.IndirectOffsetOnAxis(ap=slk_i[:, t, k4:k4 + 1], axis=0),
                        in_=xs_t[xb_][:, :], in_offset=None),
                        reads=[r_xs[xb_], r_slk], extra=[hz])
            sc.barrier()
            ar.reset(mB)
            wsl = [[ar.alloc([128, 8, D], BF16, "w%d%d" % (i, j)) for j in range(3)] for i in range(2)]
            r_wsl = [[Res() for j in range(3)] for i in range(2)]
            xg = [ar.alloc([128, 4, D], BF16, "xg") for _ in range(2)]; r_xg = [Res(), Res()]
            xgT = [ar.alloc([128, 8, 512], BF16, "xgT") for _ in range(2)]; r_xgT = [Res(), Res()]
            hT = [ar.alloc([128, 8, 512], BF16, "hT") for _ in range(2)]; r_hT = [Res(), Res()]
            g1 = [ar.alloc([128, 512], F32, "g1") for _ in range(2)]; r_g1 = [Res(), Res()]
            sg = [ar.alloc([128, 512], F32, "sg") for _ in range(2)]; r_sg = [Res(), Res()]
            u1 = [ar.alloc([128, 512], F32, "u1") for _ in range(2)]; r_u1 = [Res(), Res()]
            yout = [ar.alloc([128, D], F32, "yout") for _ in range(2)]; r_yout = [Res(), Res()]
            bsel = [ar.alloc([128, 2, 8], F32, "bsel") for _ in range(2)]; r_bsel = [Res(), Res()]
            btmp = ar.alloc([128, 8, NEXP], F32, "btmp"); r_btmp = Res()
            ohb = ar.alloc([128, NEXP], F32, "ohb"); r_ohb = Res()
            oht = [ar.alloc([32, 128], BF16, "oht") for _ in range(2)]; r_oht = [Res(), Res()]
            wflat = [wg_d.rearrange("e r n -> (e r) n"), wu_d.rearrange("e r n -> (e r) n"), wd_d.rearrange("e r n -> (e r) n")]
            wpm = [wg_d.rearrange("e (p j) n -> (e p) (j n)", j=8), wu_d.rearrange("e (p j) n -> (e p) (j n)", j=8)]
            bgv = bgT[:].rearrange("p e f -> p f e")
            buv = buT[:].rearrange("p e f -> p f e")
            dcnt = 0

            def load_block(blk):
                sl = blk % 2
                if os.environ.get('KS_NOW') and blk >= 2:
                    return
                for j in range(2):
                    sc.dma_fn("pool", lambda e, sl=sl, j=j, blk=blk: e.indirect_dma_start(
                        out=wsl[sl][j][:, :, :].rearrange("p k n -> p (k n)"), out_offset=None,
                        in_=wpm[j][:, :],
                        in_offset=bass.IndirectOffsetOnAxis(ap=widx2_i[:, blk:blk + 1], axis=0)),
                        reads=[r_widx2], writes=[r_wsl[sl][j]])
                for j in (2,):
                    for kc in range(8):
                        sc.dma_fn("pool", lambda e, sl=sl, j=j, kc=kc, blk=blk: e.indirect_dma_start(
                            out=wsl[sl][j][:, kc, :], out_offset=None, in_=wflat[j][:, :],
                            in_offset=bass.IndirectOffsetOnAxis(ap=widx_i[:, blk, kc:kc + 1], axis=0)),
                            reads=[r_widx], writes=[r_wsl[sl][j]])

            def x_load(blk):
                sc.dma("sp", xg[blk % 2][:], XS[blk * 512:(blk + 1) * 512, :].rearrange("(i p) d -> p i d", p=128), writes=[r_xg[blk % 2]])

            def x_transposes(blk):
                xs_ = blk % 2
                for k2 in range(4):
                    bk = 6 + k2 % 2
                    pv = bank_bf16(bk).rearrange("p (a t) -> p a t", a=2)
                    fns = []
                    for a in range(2):
                        k = k2 * 2 + a
                        for i in range(4):
                            fns.append(tr(pv[:, a, i * 128:(i + 1) * 128], xg[xs_][:, i, :].rearrange("p (q j) -> p q j", j=8)[:, :, k], identb[:]))
                    sc.ops("pe", fns, reads=[r_xg[xs_], r_const], writes=[bres[bk]])
                    sc.op("act", actf(xgT[xs_][:, k2 * 2:k2 * 2 + 2, :], pv, AF.Copy), reads=[bres[bk]], writes=[r_xgT[xs_]])

            load_block(0)
            x_load(0)
            x_transposes(0)
            ycnt = 0
            for blk in range(NBLK):
                sl = blk % 2
                if blk + 1 < NBLK:
                    load_block(blk + 1)
                    x_load(blk + 1)
                sc.op("dve", ts(ohb[:], iota_e, ebf[:, blk:blk + 1], None, ALU.is_equal), reads=[r_ebf, r_const], writes=[r_ohb])
                for bi, bv in enumerate((bgv, buv)):
                    sc.op("dve", tt(btmp[:], bv, ohb[:, :].unsqueeze(1).to_broadcast([128, 8, NEXP]), ALU.mult), reads=[r_ohb, r_const], writes=[r_btmp])
                    sc.op("dve", red(bsel[sl][:, bi, :], btmp[:]), reads=[r_btmp], writes=[r_bsel[sl]])
                sc.op("dve", ts(oht[sl][:], ebf[0:32, blk:blk + 1].to_broadcast([32, 128]), iota_p[0:32, 0:1], None, ALU.is_equal), reads=[r_ebf, r_const], writes=[r_oht[sl]])
                for ffc in range(8):
                    fb = ffc % 2
                    pg = bank_f32(0 + fb); pu = bank_f32(2 + fb)
                    sc.ops("pe", [mm(pg, wsl[sl][0][:, k, ffc * 128:(ffc + 1) * 128], xgT[sl][:, k, :], k == 0, k == 7) for k in range(8)],
                           reads=[r_wsl[sl][0], r_xgT[sl]], writes=[bres[0 + fb]])
                    sc.ops("pe", [mm(pu, wsl[sl][1][:, k, ffc * 128:(ffc + 1) * 128], xgT[sl][:, k, :], k == 0, k == 7) for k in range(8)],
                           reads=[r_wsl[sl][1], r_xgT[sl]], writes=[bres[2 + fb]])
                    sc.op("dve", stt(g1[fb][:], pg, bsel[sl][:, 0, ffc:ffc + 1], c7[:], ALU.add, ALU.min), reads=[bres[0 + fb], r_bsel[sl], r_const], writes=[r_g1[fb]])
                    sc.op("act", actf(sg[fb][:], g1[fb][:], AF.Silu, scale=1.702), reads=[r_g1[fb]], writes=[r_sg[fb]])
                    sc.op("act", actf(u1[fb][:], pu, AF.Relu, bias=bsel[sl][:, 1, ffc:ffc + 1]), reads=[bres[2 + fb], r_bsel[sl]], writes=[r_u1[fb]])
                    sc.op("dve", ts(u1[fb][:], u1[fb][:], 14.0, -6.0, ALU.min, ALU.add), reads=[r_u1[fb]], writes=[r_u1[fb]])
                    sc.op("dve", stt(hT[sl][:, ffc, :], sg[fb][:], 1.0 / 1.702, u1[fb][:], ALU.mult, ALU.mult), reads=[r_sg[fb], r_u1[fb]], writes=[r_hT[sl]])
                if blk + 1 < NBLK:
                    x_transposes(blk + 1)
                for i in range(4):
                    yb_ = ycnt % 2
                    ycnt += 1
                    for colh in range(2):
                        bk = 4 + dcnt % 2
                        dcnt += 1
                        pd = bank_f32(bk)
                        fns = [mm(pd, hT[sl][:, k, i * 128:(i + 1) * 128], wsl[sl][2][:, k, colh * 512:(colh + 1) * 512], k == 0, False) for k in range(8)]
                        fns.append(mm(pd, oht[sl][0:32, :], bd16[0:32, colh * 512:(colh + 1) * 512], False, True))
                        sc.ops("pe", fns, reads=[r_hT[sl], r_wsl[sl][2], r_oht[sl], r_const], writes=[bres[bk]])
                        if bk == 4:
                            sc.op("act", actf(yout[yb_][:, colh * 512:(colh + 1) * 512], pd, AF.Copy), reads=[bres[bk]], writes=[r_yout[yb_]])
                        else:
                            sc.op("dve", cp(yout[yb_][:, colh * 512:(colh + 1) * 512], pd), reads=[bres[bk]], writes=[r_yout[yb_]])
                    sc.dma("sp", YS[blk * 512 + i * 128:blk * 512 + (i + 1) * 128, :], yout[yb_][:], reads=[r_yout[yb_]])
            sc.barrier()
            ar.reset(mB)
            acc = [ar.alloc([128, D], F32, "acc") for _ in range(2)]; r_acc = [Res(), Res()]
            gk = [[ar.alloc([128, D], F32, "gk") for _ in range(4)] for _ in range(2)]; r_gk = [[Res() for _ in range(4)] for _ in range(2)]
            outt = [ar.alloc([128, D], F32, "outt") for _ in range(2)]; r_outt = [Res(), Res()]
            lntmp3 = (ar.alloc([128, 12], F32, "st6b"), ar.alloc([128, 2], F32, "mvb"), ar.alloc([128, 1], F32, "rstdb"), ar.alloc([128, 1], F32, "nmrb"))
            for t in range(NT):
                b = t % 2
                tok = slice(t * 128, (t + 1) * 128)
                sc.dma("sp", acc[b][:], X2[tok, :], writes=[r_acc[b]])
                for k4 in range(4):
                    sc.dma_fn("pool", lambda e, b=b, t=t, k4=k4: e.indirect_dma_start(
                        out=gk[b][k4][:, :], out_offset=None, in_=YS[:, :],
                        in_offset=bass.IndirectOffsetOnAxis(ap=slk_i[:, t, k4:k4 + 1], axis=0)),
                        reads=[r_slk], writes=[r_gk[b][k4]])
                sc.op("dve", ts(acc[b][:], acc[b][:], ALPHA, None, ALU.mult), reads=[r_acc[b]], writes=[r_acc[b]])
                for k4 in range(4):
                    sc.op("dve", stt(acc[b][:], gk[b][k4][:], wk[:, t, k4:k4 + 1], acc[b][:], ALU.mult, ALU.add), reads=[r_gk[b][k4], r_wk, r_acc[b]], writes=[r_acc[b]])
                layer_norm(acc[b][:], r_acc[b], outt[b][:], r_outt[b], lnp3[:, 0, :], lnp3[:, 1, :], lntmp3)
                out_handles.append(sc.dma("sp", out_d[tok, :], outt[b][:], reads=[r_outt[b]]))
            sc.barrier()

        if stop_after == "B" and not SPARSE:
            ar.reset(base_mark)
            wsl = [[ar.alloc([128, 8, D], BF16, "w%d%d" % (i, j)) for j in range(3)] for i in range(2)]
            r_wsl = [[Res() for j in range(3)] for i in range(2)]
            x2T_c = ar.alloc([128, 8, TC], BF16, "x2T_c"); r_x2Tc = Res()
            Yacc = ar.alloc([128, 8, D], F32, "Yacc"); r_Y = [Res() for _ in range(8)]
            hT = [ar.alloc([128, 8, 512], BF16, "hT") for _ in range(2)]; r_hT = [Res(), Res()]
            g1 = [ar.alloc([128, 512], F32, "g1") for _ in range(2)]; r_g1 = [Res(), Res()]
            sg = [ar.alloc([128, 512], F32, "sg") for _ in range(2)]; r_sg = [Res(), Res()]
            u1 = [ar.alloc([128, 512], F32, "u1") for _ in range(2)]; r_u1 = [Res(), Res()]
            Gc = ar.alloc([128, 8, NEXP], F32, "Gc"); r_Gc = Res()
            GTs = ar.alloc([32, 128], F32, "GTs"); r_GTs = Res()
            bd32 = ar.alloc([32, D], F32, "bd32")
            bgT = ar.alloc([128, NEXP, 8], F32, "bgT"); buT = ar.alloc([128, NEXP, 8], F32, "buT")
            lnp3 = ar.alloc([128, 2, D], F32, "lnp3")
            c7 = ar.alloc([128, 512], F32, "c7")
            outt = [ar.alloc([128, D], F32, "outt") for _ in range(2)]; r_outt = [Res(), Res()]
            lntmp3 = (ar.alloc([128, 12], F32, "st6b"), ar.alloc([128, 2], F32, "mvb"), ar.alloc([128, 1], F32, "rstdb"), ar.alloc([128, 1], F32, "nmrb"))
            sc.dma("sp", bgT[:], bgT_d[:, :, :], writes=[r_const])
            sc.dma("sp", buT[:], buT_d[:, :, :], writes=[r_const])
            sc.dma("sp", lnp3[:], lnp_d[:, 4:6, :], writes=[r_const])
            sc.dma("sp", bd32[:], bd_d[:, :], writes=[r_const])
            sc.op("pool", lambda e: e.memset(c7[:], 7.0), writes=[r_const])
            sc.op("dve", ts(buT[:], buT[:], 7.0, None, ALU.add), reads=[r_const], writes=[r_const])
            wsrc = [wg_d, wu_d, wd_d]
            cnt = 0
            dcnt = 0
            ocnt = 0
            for c in range(S // TC):
                ctok = slice(c * TC, (c + 1) * TC)
                sc.dma("sp", x2T_c[:], X2T[:, :, ctok], writes=[r_x2Tc])
                sc.dma("sp", Gc[:], GT[ctok, :].rearrange("(t p) e -> p t e", p=128), writes=[r_Gc])
                for i in range(8):
                    sc.dma("sp", Yacc[:, i, :], X2[c * TC + i * 128:c * TC + (i + 1) * 128, :], writes=[r_Y[i]])
                    pgt = bank_f32(7)[0:32, 0:128]
                    sc.op("pe", tr(pgt, Gc[:, i, :], identf[:]), reads=[r_Gc, r_const], writes=[bres[7]])
                    sc.op("dve", cp(GTs[:], pgt), reads=[bres[7]], writes=[r_GTs])
                    for colh in range(2):
                        bk = 4 + dcnt % 3
                        dcnt += 1
                        pd = bank_f32(bk)
                        sc.op("pe", mm(pd, GTs[0:32, :], bd32[0:32, colh * 512:(colh + 1) * 512]), reads=[r_GTs, r_const], writes=[bres[bk]])
                        ysl = Yacc[:, i, colh * 512:(colh + 1) * 512]
                        sc.op("dve", stt(ysl, ysl, ALPHA, pd, ALU.mult, ALU.add), reads=[bres[bk], r_Y[i]], writes=[r_Y[i]])
                for ex_ in range(NEXP):
                    sl = cnt % 2
                    cnt += 1
                    for j in range(3):
                        v = wsrc[j][ex_].rearrange("(k p) n -> p k n", p=128)
                        for hh in range(2):
                            sc.dma("pool", wsl[sl][j][:, :, hh * 512:(hh + 1) * 512], v[:, :, hh * 512:(hh + 1) * 512], writes=[r_wsl[sl][j]])
                    for half in range(2):
                        hb_ = (cnt + half) % 2
                        for ffc in range(8):
                            fb = ffc % 2
                            pg = bank_f32(0 + fb); pu = bank_f32(2 + fb)
                            sc.ops("pe", [mm(pg, wsl[sl][0][:, k, ffc * 128:(ffc + 1) * 128], x2T_c[:, k, half * 512:(half + 1) * 512], k == 0, k == 7) for k in range(8)],
                                   reads=[r_wsl[sl][0], r_x2Tc], writes=[bres[0 + fb]])
                            sc.ops("pe", [mm(pu, wsl[sl][1][:, k, ffc * 128:(ffc + 1) * 128], x2T_c[:, k, half * 512:(half + 1) * 512], k == 0, k == 7) for k in range(8)],
                                   reads=[r_wsl[sl][1], r_x2Tc], writes=[bres[2 + fb]])
                            sc.op("dve", stt(g1[fb][:], pg, bgT[:, ex_, ffc:ffc + 1], c7[:], ALU.add, ALU.min), reads=[bres[0 + fb], r_const], writes=[r_g1[fb]])
                            sc.op("act", actf(sg[fb][:], g1[fb][:], AF.Silu, scale=1.702), reads=[r_g1[fb]], writes=[r_sg[fb]])
                            sc.op("act", actf(u1[fb][:], pu, AF.Relu, bias=buT[:, ex_, ffc:ffc + 1]), reads=[bres[2 + fb], r_const], writes=[r_u1[fb]])
                            sc.op("dve", ts(u1[fb][:], u1[fb][:], 14.0, -6.0, ALU.min, ALU.add), reads=[r_u1[fb]], writes=[r_u1[fb]])
                            sc.op("dve", stt(hT[hb_][:, ffc, :], sg[fb][:], 1.0 / 1.702, u1[fb][:], ALU.mult, ALU.mult), reads=[r_sg[fb], r_u1[fb]], writes=[r_hT[hb_]])
                        for i in range(4):
                            tl = half * 4 + i
                            for colh in range(2):
                                bk = 4 + dcnt % 3
                                dcnt += 1
                                pd = bank_f32(bk)
                                sc.ops("pe", [mm(pd, hT[hb_][:, k, i * 128:(i + 1) * 128], wsl[sl][2][:, k, colh * 512:(colh + 1) * 512], k == 0, k == 7) for k in range(8)],
                                       reads=[r_hT[hb_], r_wsl[sl][2]], writes=[bres[bk]])
                                ysl = Yacc[:, tl, colh * 512:(colh + 1) * 512]
                                sc.op("dve", stt(ysl, pd, Gc[:, tl, ex_:ex_ + 1], ysl, ALU.mult, ALU.add), reads=[bres[bk], r_Gc, r_Y[tl]], writes=[r_Y[tl]])
                for i in range(8):
                    ob = ocnt % 2
                    ocnt += 1
                    layer_norm(Yacc[:, i, :], r_Y[i], outt[ob][:], r_outt[ob], lnp3[:, 0, :], lnp3[:, 1, :], lntmp3)
                    out_handles.append(sc.dma("sp", out_d[c * TC + i * 128:c * TC + (i + 1) * 128, :], outt[ob][:], reads=[r_outt[ob]]))
            sc.barrier()

        sc.barrier()
        blk = stack.enter_context(nc.Block())
        sc.emit(blk)
    return nc, dbg


def _prep_shared(inputs, S):
    f = lambda a: np.ascontiguousarray(np.asarray(a, dtype=np.float32))
    sh = {}
    sh["w_in"] = f(inputs["w_in"][0])
    sh["w_out"] = f(inputs["w_out"][0])
    for k in ("mem_wq", "mem_wk", "mem_wv", "mem_wo"):
        sh[k] = f(inputs[k][0])
    sh["router_w"] = f(inputs["router_w"][0])
    sh["router_b"] = f(inputs["router_b"][0]).reshape(1, NEXP)
    sh["w_gate"] = f(inputs["w_gate"][0])
    sh["w_up"] = f(inputs["w_up"][0])
    sh["w_down"] = f(inputs["w_down"][0])
    sh["b_gateT"] = f(np.asarray(inputs["b_gate"][0]).reshape(NEXP, 8, 128).transpose(2, 0, 1))
    sh["b_upT"] = f(np.asarray(inputs["b_up"][0]).reshape(NEXP, 8, 128).transpose(2, 0, 1))
    sh["b_down"] = f(inputs["b_down"][0])
    lnp = np.stack([np.asarray(inputs[k][0], dtype=np.float32) for k in ("ln1_g", "ln1_b", "ln2_g", "ln2_b", "ln3_g", "ln3_b")], 0)
    sh["lnp"] = f(np.broadcast_to(lnp[None], (128, 6, D)))
    sh["rng"] = f(np.broadcast_to(np.asarray(inputs["ret_norm_g"][0], dtype=np.float32).reshape(1, 512), (128, 512)))
    sh.update(_const_tables(S))
    sh["cmeta"] = _meta_consts(S)[0]
    return sh


def kernel(**inputs):
    x = np.asarray(inputs["x"], dtype=np.float32)
    mem = np.asarray(inputs["mem"], dtype=np.float32)
    B, S, _ = x.shape
    sh = _prep_shared(inputs, S)
    nc, _ = build_program(NT=S // 128)
    in_maps = []
    for b in range(B):
        m = dict(sh)
        m["x"] = np.ascontiguousarray(x[b])
        m["mem"] = np.ascontiguousarray(mem[b])
        in_maps.append(m)
    res = run_bass_kernel_spmd(nc, in_maps, core_ids=list(range(B)))
    return np.stack([np.asarray(r["out"], dtype=np.float32) for r in res.results], axis=0)
```

```python
import math
from contextlib import ExitStack

import numpy as np
import ml_dtypes
import concourse.bass as bass
import concourse.mybir as mybir
from concourse.bass_utils import run_bass_kernel_spmd

F32 = mybir.dt.float32
BF16 = mybir.dt.bfloat16
AF = mybir.ActivationFunctionType
ALU = mybir.AluOpType
AX = mybir.AxisListType

D = 1024
NIN = 3584
NEXP = 32
NMEM = 256
ALPHA = 2.0 ** 0.25
EPS = 1e-5
NOFF = 17
TC = 1024
VS = 80
SPARSE = True


class Res:
    __slots__ = ("name", "w", "r")

    def __init__(self, name=""):
        self.name = name
        self.w = {}
        self.r = {}


class Sched:
    ENG = ("pe", "act", "dve", "pool", "sp")

    def __init__(self, nc, stack, n_dma_sems=24):
        self.nc = nc
        self.streams = {e: [] for e in self.ENG}
        self.sems = {}
        self.count = {}
        self.waited = {e: {} for e in self.ENG}
        for e in self.ENG:
            s = stack.enter_context(nc.semaphore("s_" + e))
            self.sems[e] = s
            self.count[e] = 0
        self.dq = {}
        for q in ("sp", "pool", "act"):
            lst = []
            for i in range(n_dma_sems):
                key = "d_%s_%d" % (q, i)
                self.sems[key] = stack.enter_context(nc.semaphore(key))
                self.count[key] = 0
                lst.append(key)
            self.dq[q] = [lst, 0]
        self.n_instr = 0

    def _wait(self, eng, key, val):
        if val <= 0:
            return
        if self.waited[eng].get(key, 0) >= val:
            return
        self.waited[eng][key] = val
        sem = self.sems[key]
        self.streams[eng].append(lambda e, sem=sem, val=val: e.wait_ge(sem, val))

    def _deps(self, eng, reads, writes, extra):
        deps = {}

        def add(d):
            for k, v in d.items():
                if deps.get(k, 0) < v:
                    deps[k] = v
        for r in reads:
            add(r.w)
        for w in writes:
            add(w.r)
            add(w.w)
        for h in extra:
            add(h)
        for k, v in deps.items():
            if k == eng and eng == "pe":
                continue
            self._wait(eng, k, v)

    def _post(self, reads, writes, h):
        for r in reads:
            for k, v in h.items():
                if r.r.get(k, 0) < v:
                    r.r[k] = v
        for w in writes:
            w.w = dict(h)
            w.r = {}

    def op(self, eng, fn, reads=(), writes=(), extra=()):
        self._deps(eng, reads, writes, extra)
        self.count[eng] += 1
        val = self.count[eng]
        sem = self.sems[eng]
        self.streams[eng].append(lambda e, fn=fn, sem=sem: fn(e).then_inc(sem, 1))
        h = {eng: val}
        self._post(reads, writes, h)
        self.n_instr += 1
        return h

    def ops(self, eng, fns, reads=(), writes=(), extra=()):
        self._deps(eng, reads, writes, extra)
        for fn in fns[:-1]:
            self.streams[eng].append(lambda e, fn=fn: fn(e))
        self.count[eng] += 1
        val = self.count[eng]
        sem = self.sems[eng]
        fn = fns[-1]
        self.streams[eng].append(lambda e, fn=fn, sem=sem: fn(e).then_inc(sem, 1))
        h = {eng: val}
        self._post(reads, writes, h)
        self.n_instr += len(fns)
        return h

    def dma(self, q, out, in_, reads=(), writes=(), extra=()):
        self._deps(q, reads, writes, extra)
        lst, i = self.dq[q]
        key = lst[i % len(lst)]
        self.dq[q][1] = i + 1
        self._wait(q, key, self.count[key])
        self.count[key] += 16
        val = self.count[key]
        sem = self.sems[key]
        self.streams[q].append(lambda e, out=out, in_=in_, sem=sem: e.dma_start(out=out, in_=in_).then_inc(sem, 16))
        h = {key: val}
        self._post(reads, writes, h)
        self.n_instr += 1
        return h

    def dma_fn(self, q, fn, reads=(), writes=(), extra=()):
        self._deps(q, reads, writes, extra)
        lst, i = self.dq[q]
        key = lst[i % len(lst)]
        self.dq[q][1] = i + 1
        self._wait(q, key, self.count[key])
        self.count[key] += 16
        val = self.count[key]
        sem = self.sems[key]
        self.streams[q].append(lambda e, fn=fn, sem=sem: fn(e).then_inc(sem, 16))
        h = {key: val}
        self._post(reads, writes, h)
        self.n_instr += 1
        return h

    def barrier(self):
        allh = {k: v for k, v in self.count.items() if v > 0}
        for e in self.ENG:
            for k, v in allh.items():
                if k != e:
                    self._wait(e, k, v)

    def final_wait(self, eng, handles):
        for h in handles:
            for k, v in h.items():
                self._wait(eng, k, v)

    def emit(self, block):
        nc = self.nc
        m = {"pe": block.tensor, "act": block.scalar, "dve": block.vector, "pool": block.gpsimd, "sp": block.sync}
        for name in self.ENG:
            stream = self.streams[name]

            def body(e, stream=stream):
                for f in stream:
                    f(e)
            m[name](body)


class Arena:
    def __init__(self, nc, limit=206 * 1024):
        self.nc = nc
        self.off = 0
        self.limit = limit
        self.base = nc.alloc_sbuf_tensor("arena", [128, limit], mybir.dt.uint8)

    def mark(self):
        return self.off

    def reset(self, m):
        self.off = m

    def alloc(self, shape, dtype, name=None):
        esz = 2 if dtype == BF16 else 4
        nbytes = esz
        for s in shape[1:]:
            nbytes *= s
        off = (self.off + 63) // 64 * 64
        assert off + nbytes <= self.limit, ("SBUF overflow", name, off, nbytes)
        self.off = off + nbytes
        v = self.base[0:shape[0], off:off + nbytes].bitcast(dtype)
        if len(shape) == 3:
            v = v.rearrange("p (a b) -> p a b", a=shape[1])
        elif len(shape) == 4:
            v = v.rearrange("p (a b c) -> p a b c", a=shape[1], b=shape[2])
        return v


def _meta_consts(S):
    nblk = (S * 4) // 512 + NEXP
    p = np.arange(128, dtype=np.float32)[:, None]
    thr16 = np.broadcast_to((512.0 * np.arange(17, dtype=np.float32))[None, :], (128, 17))
    blkthr = np.broadcast_to((512.0 * np.arange(nblk, dtype=np.float32))[None, :], (128, nblk))
    iota_e = np.broadcast_to(np.arange(NEXP, dtype=np.float32)[None, :], (128, NEXP))
    kcp = 128.0 * np.arange(8, dtype=np.float32)[None, :] + p
    tril = np.broadcast_to(np.tril(np.ones((NEXP, NEXP), np.float32)).reshape(1, NEXP * NEXP), (128, NEXP * NEXP))
    return np.ascontiguousarray(np.concatenate([thr16, blkthr, iota_e, kcp, p, tril], axis=1).astype(np.float32)), nblk


def _const_tables(S):
    pos = np.arange(S, dtype=np.float64)
    inv_a = 1.0 / (500000.0 ** (np.arange(8, dtype=np.float64) / 8))
    ang = pos[:, None] * inv_a[None, :]
    ca, sa = np.cos(ang), np.sin(ang)
    inv_r = 1.0 / (10000.0 ** (np.arange(32, dtype=np.float64) / 32))
    angr = pos[:, None] * inv_r[None, :]
    cr, sr = np.cos(angr), np.sin(angr)
    tab = np.concatenate([
        np.concatenate([ca, ca], 1) / 8.0, np.concatenate([-sa, sa], 1) / 8.0,
        np.concatenate([ca, ca], 1), np.concatenate([-sa, sa], 1),
        np.concatenate([cr, cr], 1), np.concatenate([-sr, sr], 1),
    ], axis=1).astype(np.float32)
    h = np.arange(8, dtype=np.float64)
    log_g = np.log1p(-(2.0 ** (-5.0 - h)))
    i = np.arange(128, dtype=np.float64)
    kfac = np.exp(-log_g[None, :] * (i[:, None] + 1.0)) / 8.0
    qdec = np.exp(log_g[None, :] * (i[:, None] + 1.0))
    cd = np.exp(log_g * 128.0)
    cdp = np.zeros((128, 4), np.float64)
    for p in range(4):
        cdp[:64, p] = cd[2 * p]
        cdp[64:, p] = cd[2 * p + 1]
    rtab = np.concatenate([kfac, qdec, cdp], axis=1).astype(np.float32)
    kk = np.arange(128)[:, None]
    qq = np.arange(128)[None, :]
    cm = (kk <= qq).astype(np.float32)
    am = np.zeros((128, NOFF, 128), np.float32)
    for j in range(NOFF):
        o = 16 - j
        dl = 128 * o + qq - kk
        m = ((dl >= 0) & (dl <= 128)).astype(np.float32)
        m += ((dl >= 0) & (dl % 4 == 0) & (dl <= 512)).astype(np.float32)
        m += ((dl >= 0) & (dl % 16 == 0) & (dl <= 2048)).astype(np.float32)
        am[:, j, :] = m
    return dict(
        tab=tab, rtab=rtab, cm=cm.astype(np.float32), am=am.astype(ml_dtypes.bfloat16),
        identb=np.eye(128, dtype=np.float32).astype(ml_dtypes.bfloat16),
        identf=np.eye(128, dtype=np.float32),
        ustrict=(kk < qq).astype(np.float32),
    )


def build_program(NT=64, stop_after="B", debug=False):
    S = NT * 128
    nc = bass.Bass("TRN2", target_bir_lowering=False)

    def din(name, shape, dt=F32):
        return nc.dram_tensor(name, list(shape), dt, kind="ExternalInput").ap()

    x_d = din("x", [S, D])
    mem_d = din("mem", [NMEM, D])
    w_in_d = din("w_in", [D, NIN])
    w_out_d = din("w_out", [D, D])
    wq_d = din("mem_wq", [D, D])
    wk_d = din("mem_wk", [D, D])
    wv_d = din("mem_wv", [D, D])
    wo_d = din("mem_wo", [D, D])
    rw_d = din("router_w", [D, NEXP])
    rb_d = din("router_b", [1, NEXP])
    wg_d = din("w_gate", [NEXP, D, D])
    wu_d = din("w_up", [NEXP, D, D])
    wd_d = din("w_down", [NEXP, D, D])
    bgT_d = din("b_gateT", [128, NEXP, 8])
    buT_d = din("b_upT", [128, NEXP, 8])
    bd_d = din("b_down", [NEXP, D])
    lnp_d = din("lnp", [128, 6, D])
    rng_d = din("rng", [128, 512])
    tab_d = din("tab", [S, 192])
    rtab_d = din("rtab", [128, 20])
    cm_d = din("cm", [128, 128])
    am_d = din("am", [128, NOFF, 128], BF16)
    identb_d = din("identb", [128, 128], BF16)
    identf_d = din("identf", [128, 128])
    ustrict_d = din("ustrict", [128, 128])
    NBLK = (S * 4) // 512 + NEXP
    NSLOT = NBLK * 512
    CMW = 17 + NBLK + NEXP + 8 + 1 + NEXP * NEXP
    cmeta_d = din("cmeta", [128, CMW])
    out_d = nc.dram_tensor("out", [S, D], F32, kind="ExternalOutput").ap()

    def dscr(name, shape, dt):
        if debug:
            return nc.dram_tensor(name, list(shape), dt, kind="ExternalOutput").ap()
        return nc.dram_tensor(name, list(shape), dt).ap()

    QAT = dscr("QAT", [128, 4, S], BF16)
    KAT = dscr("KAT", [128, 4, S], BF16)
    QRT = dscr("QRT", [128, 4, S], BF16)
    KRT = dscr("KRT", [128, 4, S], BF16)
    VA = dscr("VA", [S, 8 * VS], BF16)
    KR = dscr("KR", [S, 512], BF16)
    VR = dscr("VR", [S, 512], BF16)
    GS = dscr("GS", [S, 512], F32)
    X2 = dscr("X2", [S, D], F32)
    X2T = dscr("X2T", [128, 8, S], BF16)
    GT = dscr("GT", [S, NEXP], F32)
    RKD = dscr("RKD", [S, NEXP], F32)
    CNT = dscr("CNT", [128, NEXP], F32)
    X2B = dscr("X2B", [S, D], BF16)
    XS = dscr("XS", [NSLOT, D], BF16)
    YS = dscr("YS", [NSLOT, D], F32)

    dbg = {}
    if debug:
        def dout(name, shape, dt=F32):
            dbg[name] = nc.dram_tensor(name, list(shape), dt, kind="ExternalOutput").ap()
            return dbg[name]

    with ExitStack() as stack:
        sc = Sched(nc, stack)
        ar = Arena(nc)
        banks = [nc.alloc_psum_tensor("bank%d" % i, [128, 512], F32) for i in range(8)]
        bres = [Res("bank%d" % i) for i in range(8)]

        def bank_f32(i):
            return banks[i][:]

        def bank_bf16(i):
            return banks[i][:].bitcast(BF16)

        identb = ar.alloc([128, 128], BF16, "identb")
        identf = ar.alloc([128, 128], F32, "identf")
        r_const = Res("const")
        sc.dma("sp", identb[:], identb_d[:, :], writes=[r_const])
        sc.dma("sp", identf[:], identf_d[:, :], writes=[r_const])
        epsc = ar.alloc([128, 1], F32, "epsc")
        sc.op("pool", lambda e: e.memset(epsc[:], EPS), writes=[r_const])
        base_mark = ar.mark()
        out_handles = []

        w_in = ar.alloc([128, 8, NIN], BF16, "w_in")
        r_w_in = Res("w_in")
        w_in_view = w_in_d.rearrange("(k p) n -> p k n", p=128)
        for c in range(7):
            sc.dma("pool", w_in[:, :, c * 512:(c + 1) * 512], w_in_view[:, :, c * 512:(c + 1) * 512], writes=[r_w_in])
        rtab = ar.alloc([128, 20], F32, "rtab")
        sc.dma("sp", rtab[:], rtab_d[:, :], writes=[r_const])

        NB = 2
        xin = [ar.alloc([128, D], F32, "xin") for _ in range(NB)]
        r_xin = [Res("xin") for _ in range(NB)]
        tab = [ar.alloc([128, 192], F32, "tab") for _ in range(NB)]
        r_tab = [Res("tab") for _ in range(NB)]
        xb = [ar.alloc([128, D], BF16, "xb") for _ in range(NB)]
        r_xb = [Res("xb") for _ in range(NB)]
        xT = [ar.alloc([128, 8, 128], BF16, "xT") for _ in range(NB)]
        r_xT = [Res("xT") for _ in range(NB)]
        qa_b = [ar.alloc([128, 8, 64], BF16, "qa_b") for _ in range(NB)]
        ka_b = [ar.alloc([128, 8, 64], BF16, "ka_b") for _ in range(NB)]
        qr_b = [ar.alloc([128, 8, 64], BF16, "qr_b") for _ in range(NB)]
        kr_b = [ar.alloc([128, 8, 64], BF16, "kr_b") for _ in range(NB)]
        vr_b = [ar.alloc([128, 512], BF16, "vr_b") for _ in range(NB)]
        va_b = [ar.alloc([128, 8, VS], BF16, "va_b") for _ in range(NB)]
        gs_f = [ar.alloc([128, 512], F32, "gs_f") for _ in range(NB)]
        r_qa = [Res() for _ in range(NB)]
        r_ka = [Res() for _ in range(NB)]
        r_qr = [Res() for _ in range(NB)]
        r_kr = [Res() for _ in range(NB)]
        r_vr = [Res() for _ in range(NB)]
        r_va = [Res() for _ in range(NB)]
        r_gs = [Res() for _ in range(NB)]
        qaT = [ar.alloc([128, 4, 128], BF16, "qaT") for _ in range(NB)]
        kaT = [ar.alloc([128, 4, 128], BF16, "kaT") for _ in range(NB)]
        qrT = [ar.alloc([128, 4, 128], BF16, "qrT") for _ in range(NB)]
        krT = [ar.alloc([128, 4, 128], BF16, "krT") for _ in range(NB)]
        r_qaT = [Res() for _ in range(NB)]
        r_kaT = [Res() for _ in range(NB)]
        r_qrT = [Res() for _ in range(NB)]
        r_krT = [Res() for _ in range(NB)]
        rt_a = ar.alloc([128, 8, 64], F32, "rt_a")
        rt_b = ar.alloc([128, 8, 64], F32, "rt_b")
        r_rt = Res("rt")
        r_rtb = Res("rtb")
        for b in range(NB):
            sc.op("pool", lambda e, b=b: e.memset(va_b[b][:], 1.0), writes=[r_va[b]])

        def bc_heads(ap2d, n):
            return ap2d.unsqueeze(1).to_broadcast([128, 8, n])

        def rotary(pb, r_pb, dst, r_dst, tb, r_tb, c_off, n, post_scale=None):
            hn = n // 2
            p3 = pb.rearrange("p (h d) -> p h d", h=8)
            cc = bc_heads(tb[:, c_off:c_off + n], n)
            s1 = bc_heads(tb[:, c_off + n:c_off + n + hn], hn)
            s2 = bc_heads(tb[:, c_off + n + hn:c_off + 2 * n], hn)
            ta = rt_a[:, :, 0:n]
            tb_ = rt_b[:, :, 0:n]
            sc.op("dve", lambda e: e.tensor_tensor(out=ta, in0=p3[:, :, 0:n], in1=cc, op=ALU.mult),
                  reads=[r_tb, r_pb], writes=[r_rt])
            sc.op("dve", lambda e: e.tensor_tensor(out=rt_b[:, :, 0:hn], in0=p3[:, :, hn:n], in1=s1, op=ALU.mult),
                  reads=[r_tb, r_pb], writes=[r_rtb])
            sc.op("dve", lambda e: e.tensor_tensor(out=rt_b[:, :, hn:n], in0=p3[:, :, 0:hn], in1=s2, op=ALU.mult),
                  reads=[r_tb, r_pb, r_rtb], writes=[r_rtb])
            if post_scale is None:
                sc.op("dve", lambda e: e.tensor_tensor(out=dst[:, :, 0:n], in0=ta, in1=tb_, op=ALU.add),
                      reads=[r_rt, r_rtb], writes=[r_dst])
            else:
                sc.op("dve", lambda e: e.tensor_tensor(out=ta, in0=ta, in1=tb_, op=ALU.add),
                      reads=[r_rt, r_rtb], writes=[r_rt])
                ps = post_scale.unsqueeze(2).to_broadcast([128, 8, n])
                sc.op("dve", lambda e: e.tensor_tensor(out=dst[:, :, 0:n], in0=ta, in1=ps, op=ALU.mult),
                      reads=[r_rt, r_const], writes=[r_dst])

        import os
        CUT = int(os.environ.get('KCUT', '99'))
        def a1_post(t):
            b = t % NB
            tok = slice(t * 128, (t + 1) * 128)
            for (srcs, bk) in ((((qa_b, r_qa, qaT, r_qaT), (ka_b, r_ka, kaT, r_kaT)), 5), (((qr_b, r_qr, qrT, r_qrT), (kr_b, r_kr, krT, r_krT)), 6)):
                pv = bank_bf16(bk).rearrange("p (a c t) -> p a c t", a=2, c=4)
                fns = []
                rd = [r_const]
                for ai, (src, rsrc, dstT, rdst) in enumerate(srcs):
                    sflat = src[b][:].rearrange("p h d -> p (h d)")
                    rd.append(rsrc[b])
                    for c4 in range(4):
                        fns.append(lambda e, sflat=sflat, ai=ai, c4=c4, pv=pv: e.transpose(out=pv[:, ai, c4, :], in_=sflat[:, c4 * 128:(c4 + 1) * 128], identity=identb[:]))
                sc.ops("pe", fns, reads=rd, writes=[bres[bk]])
                if os.environ.get('KNOCOPY'):
                    continue
                for ai, (src, rsrc, dstT, rdst) in enumerate(srcs):
                    eng = "act" if bk == 5 else "dve"
                    if os.environ.get('KFORCE'):
                        eng = os.environ.get('KFORCE')
                    if os.environ.get('KONLY') and os.environ.get('KONLY') != eng:
                        continue
                    if eng == "act":
                        sc.op("act", lambda e, dstT=dstT, ai=ai, pv=pv, b=b: e.activation(out=dstT[b][:], in_=pv[:, ai, :, :], func=AF.Copy),
                              reads=[bres[bk]], writes=[rdst[b]])
                    else:
                        sc.op("dve", lambda e, dstT=dstT, ai=ai, pv=pv, b=b: e.tensor_copy(out=dstT[b][:], in_=pv[:, ai, :, :]),
                              reads=[bres[bk]], writes=[rdst[b]])
            sc.dma("sp", QAT[:, :, tok], qaT[b][:], reads=[r_qaT[b]])
            sc.dma("sp", KAT[:, :, tok], kaT[b][:], reads=[r_kaT[b]])
            sc.dma("sp", QRT[:, :, tok], qrT[b][:], reads=[r_qrT[b]])
            sc.dma("sp", KRT[:, :, tok], krT[b][:], reads=[r_krT[b]])
            sc.dma("sp", VA[tok, :], va_b[b][:].rearrange("p h d -> p (h d)"), reads=[r_va[b]])
            sc.dma("sp", KR[tok, :], kr_b[b][:].rearrange("p h d -> p (h d)"), reads=[r_kr[b]])
            sc.dma("sp", VR[tok, :], vr_b[b][:], reads=[r_vr[b]])
            sc.dma("sp", GS[tok, :], gs_f[b][:], reads=[r_gs[b]])

        for t in range(NT if CUT > 0 else 0):
            b = t % NB
            tok = slice(t * 128, (t + 1) * 128)
            sc.dma("sp", xin[b][:], x_d[tok, :], writes=[r_xin[b]])
            sc.dma("sp", tab[b][:], tab_d[tok, :], writes=[r_tab[b]])
            sc.op("act", lambda e, b=b: e.activation(out=xb[b][:], in_=xin[b][:], func=AF.Copy), reads=[r_xin[b]], writes=[r_xb[b]])
            if CUT < 2:
                continue
            pT = bank_bf16(0).rearrange("p (k t) -> p k t", k=8)
            sc.ops("pe", [lambda e, b=b, k=k: e.transpose(out=pT[:, k, :], in_=xb[b][:, k * 128:(k + 1) * 128], identity=identb[:])
                          for k in range(8)], reads=[r_xb[b], r_const], writes=[bres[0]])
            sc.op("act", lambda e, b=b: e.activation(out=xT[b][:], in_=pT, func=AF.Copy), reads=[bres[0]], writes=[r_xT[b]])
            if CUT < 3:
                continue
            for c in range(7 if CUT > 3 else 0):
                bk = 1 + (c % 4)
                pb = bank_f32(bk)
                sc.ops("pe", [lambda e, b=b, k=k, c=c, pb=pb: e.matmul(pb, lhsT=xT[b][:, k, :], rhs=w_in[:, k, c * 512:(c + 1) * 512],
                                                                    start=(k == 0), stop=(k == 7)) for k in range(8)],
                       reads=[r_xT[b], r_w_in], writes=[bres[bk]])
                p3 = pb.rearrange("p (h d) -> p h d", h=8)
                if c == 0:
                    rotary(pb, bres[bk], qa_b[b], r_qa[b], tab[b], r_tab[b], 0, 16)
                    sc.op("act", lambda e, b=b, p3=p3: e.activation(out=qa_b[b][:, :, 16:64], in_=p3[:, :, 16:64], func=AF.Copy, scale=0.125),
                          reads=[bres[bk]], writes=[r_qa[b]])
                elif c == 1:
                    rotary(pb, bres[bk], ka_b[b], r_ka[b], tab[b], r_tab[b], 32, 16)
                    sc.op("act", lambda e, b=b, p3=p3: e.activation(out=ka_b[b][:, :, 16:64], in_=p3[:, :, 16:64], func=AF.Copy),
                          reads=[bres[bk]], writes=[r_ka[b]])
                elif c == 2:
                    sc.op("act", lambda e, b=b, p3=p3: e.activation(out=va_b[b][:, :, 0:64], in_=p3, func=AF.Copy),
                          reads=[bres[bk]], writes=[r_va[b]])
                elif c == 3:
                    rotary(pb, bres[bk], qr_b[b], r_qr[b], tab[b], r_tab[b], 64, 64)
                elif c == 4:
                    rotary(pb, bres[bk], kr_b[b], r_kr[b], tab[b], r_tab[b], 64, 64, post_scale=rtab[:, 0:8])
                elif c == 5:
                    sc.op("act", lambda e, b=b, pb=pb: e.activation(out=vr_b[b][:], in_=pb, func=AF.Copy),
                          reads=[bres[bk]], writes=[r_vr[b]])
                else:
                    sc.op("act", lambda e, b=b, pb=pb: e.activation(out=gs_f[b][:], in_=pb, func=AF.Silu),
                          reads=[bres[bk]], writes=[r_gs[b]])
            if t > 0:
                a1_post(t - 1)

        a1_post(NT - 1)
        sc.barrier()
        def mm(out, lhsT, rhs, start=True, stop=True):
            return lambda e: e.matmul(out, lhsT=lhsT, rhs=rhs, start=start, stop=stop)

        def tr(out, in_, ident):
            return lambda e: e.transpose(out=out, in_=in_, identity=ident)

        def actf(out, in_, func, **kw):
            return lambda e: e.activation(out=out, in_=in_, func=func, **kw)

        def tt(out, a, b_, op):
            return lambda e: e.tensor_tensor(out=out, in0=a, in1=b_, op=op)

        def ts(out, a, s1, s2, op0, op1=None):
            if op1 is None:
                return lambda e: e.tensor_scalar(out=out, in0=a, scalar1=s1, scalar2=None, op0=op0)
            return lambda e: e.tensor_scalar(out=out, in0=a, scalar1=s1, scalar2=s2, op0=op0, op1=op1)

        def stt(out, a, s, b_, op0, op1):
            return lambda e: e.scalar_tensor_tensor(out=out, in0=a, scalar=s, in1=b_, op0=op0, op1=op1)

        def cp(out, in_):
            return lambda e: e.tensor_copy(out=out, in_=in_)

        def red(out, in_, op=ALU.add):
            return lambda e: e.tensor_reduce(out=out, in_=in_, axis=AX.X, op=op)

        def layer_norm(src, r_src, dst, r_dst, g_ap, b_ap, tmp):
            st6, mv, rstd, nmr = tmp
            r_t = Res()
            sc.ops("dve", [lambda e: e.bn_stats(out=st6[:, 0:6], in_=src[:, 0:512]),
                           lambda e: e.bn_stats(out=st6[:, 6:12], in_=src[:, 512:1024])], reads=[r_src], writes=[r_t])
            sc.op("dve", lambda e: e.bn_aggr(out=mv[:, 0:2], in_=st6[:, 0:12]), reads=[r_t], writes=[r_t])
            sc.op("act", actf(rstd[:, 0:1], mv[:, 1:2], AF.Ln, bias=epsc[:, 0:1]), reads=[r_t, r_const], writes=[r_t])
            sc.op("act", actf(rstd[:, 0:1], rstd[:, 0:1], AF.Exp, scale=-0.5), reads=[r_t], writes=[r_t])
            sc.op("dve", stt(nmr[:, 0:1], mv[:, 0:1], -1.0, rstd[:, 0:1], ALU.mult, ALU.mult), reads=[r_t], writes=[r_t])
            sc.op("act", actf(dst, src, AF.Identity, bias=nmr[:, 0:1], scale=rstd[:, 0:1]), reads=[r_src, r_t], writes=[r_dst])
            sc.op("dve", tt(dst, dst, g_ap, ALU.mult), reads=[r_dst, r_const], writes=[r_dst])
            sc.op("dve", tt(dst, dst, b_ap, ALU.add), reads=[r_dst, r_const], writes=[r_dst])

        if stop_after != "A1":
            ar.reset(base_mark)
            w_out_b = ar.alloc([128, 8, D], BF16, "w_out_b")
            wq_b = ar.alloc([128, 8, D], BF16, "wq_b")
            wo_b = ar.alloc([128, 8, D], BF16, "wo_b")
            kmT = ar.alloc([128, 8, NMEM], BF16, "kmT")
            vm_b = ar.alloc([128, 2, D], BF16, "vm_b")
            lnp = ar.alloc([128, 4, D], F32, "lnp")
            rng = ar.alloc([128, 512], F32, "rng")
            am = ar.alloc([128, NOFF, 128], BF16, "am")
            cm = ar.alloc([128, 128], F32, "cm")
            rtab2 = ar.alloc([128, 20], F32, "rtab2")
            rw_f = ar.alloc([128, 8, NEXP], F32, "rw_f")
            rb_f = ar.alloc([1, NEXP], F32, "rb_f")
            ones_f = ar.alloc([128, 128], F32, "ones_f")
            ustr = ar.alloc([128, 128], F32, "ustr")
            rbase = ar.alloc([128, NEXP], F32, "rbase"); r_rbase = Res()
            rkt = [ar.alloc([128, NEXP], F32, "rkt") for _ in range(2)]; r_rkt = [Res(), Res()]
            ones_b = ar.alloc([128, 8], BF16, "ones_b")
            r_w2 = Res("w2")
            for (dst, src) in ((w_out_b, w_out_d), (wq_b, wq_d), (wo_b, wo_d)):
                v = src.rearrange("(k p) n -> p k n", p=128)
                for hh in range(2):
                    sc.dma("pool", dst[:, :, hh * 512:(hh + 1) * 512], v[:, :, hh * 512:(hh + 1) * 512], writes=[r_w2])
            sc.dma("sp", lnp[:], lnp_d[:, 0:4, :], writes=[r_const])
            sc.dma("sp", rng[:], rng_d[:, :], writes=[r_const])
            sc.dma("sp", am[:], am_d[:, :, :], writes=[r_const])
            sc.dma("sp", cm[:], cm_d[:, :], writes=[r_const])
            sc.dma("sp", rtab2[:], rtab_d[:, :], writes=[r_const])
            sc.dma("sp", rw_f[:], rw_d.rearrange("(k p) n -> p k n", p=128), writes=[r_const])
            sc.dma("sp", rb_f[:], rb_d[:, :], writes=[r_const])
            sc.op("pool", lambda e: e.memset(ones_f[:], 1.0), writes=[r_const])
            sc.op("pool", lambda e: e.memset(rbase[:], 0.0), writes=[r_rbase])
            sc.dma("sp", ustr[:], ustrict_d[:, :], writes=[r_const])
            sc.op("pool", lambda e: e.memset(ones_b[:], 1.0), writes=[r_const])
            m2 = ar.mark()
            wk_b = ar.alloc([128, 8, D], BF16, "wk_b")
            wv_b = ar.alloc([128, 8, D], BF16, "wv_b")
            memf = ar.alloc([128, 2, D], F32, "memf")
            memb = ar.alloc([128, 2, D], BF16, "memb")
            memT = ar.alloc([128, 8, NMEM], BF16, "memT")
            r_wkv = Res()
            r_memf = Res(); r_memb = Res(); r_memT = Res(); r_km = Res(); r_vm = Res()
            for (dst, src) in ((wk_b, wk_d), (wv_b, wv_d)):
                v = src.rearrange("(k p) n -> p k n", p=128)
                for hh in range(2):
                    sc.dma("pool", dst[:, :, hh * 512:(hh + 1) * 512], v[:, :, hh * 512:(hh + 1) * 512], writes=[r_wkv])
            sc.dma("sp", memf[:], mem_d.rearrange("(c p) d -> p c d", p=128), writes=[r_memf])
            sc.op("pool", cp(memb[:], memf[:]), reads=[r_memf], writes=[r_memb])
            for mc in range(2):
                pTm = bank_bf16(mc).rearrange("p (k t) -> p k t", k=8)
                sc.ops("pe", [tr(pTm[:, k, :], memb[:, mc, k * 128:(k + 1) * 128], identb[:]) for k in range(8)],
                       reads=[r_memb, r_const], writes=[bres[mc]])
                sc.op("act", actf(memT[:, :, mc * 128:(mc + 1) * 128], pTm, AF.Copy), reads=[bres[mc]], writes=[r_memT])
            for fc in range(8):
                bk = 2 + fc % 2
                pk = bank_f32(bk)[:, 0:NMEM]
                sc.ops("pe", [mm(pk, wk_b[:, k, fc * 128:(fc + 1) * 128], memT[:, k, :], k == 0, k == 7) for k in range(8)],
                       reads=[r_wkv, r_memT], writes=[bres[bk]])
                sc.op("act", actf(kmT[:, fc, :], pk, AF.Copy), reads=[bres[bk]], writes=[r_km])
            for mc in range(2):
                for hh in range(2):
                    bk = 4 + (mc * 2 + hh) % 2
                    pvv = bank_f32(bk)
                    sc.ops("pe", [mm(pvv, memT[:, k, mc * 128:(mc + 1) * 128], wv_b[:, k, hh * 512:(hh + 1) * 512], k == 0, k == 7) for k in range(8)],
                           reads=[r_wkv, r_memT], writes=[bres[bk]])
                    sc.op("act", actf(vm_b[:, mc, hh * 512:(hh + 1) * 512], pvv, AF.Copy), reads=[bres[bk]], writes=[r_vm])
            sc.barrier()
            ar.reset(m2)
            R = 18
            kring = ar.alloc([128, 4, R * 128], BF16, "kring")
            vring = ar.alloc([128, R, 8 * VS], BF16, "vring")
            r_kring = [Res() for _ in range(R)]
            r_vring = [Res() for _ in range(R)]
            NB2 = 2
            def dbl(shape, dt, name):
                return [ar.alloc(shape, dt, name) for _ in range(NB2)], [Res(name) for _ in range(NB2)]
            qaT2, r_qaT2 = dbl([128, 4, 128], BF16, "qaT2")
            qrT2, r_qrT2 = dbl([128, 4, 128], BF16, "qrT2")
            krT2, r_krT2 = dbl([128, 4, 128], BF16, "krT2")
            kr2, r_kr2 = dbl([128, 512], BF16, "kr2")
            vr2, r_vr2 = dbl([128, 512], BF16, "vr2")
            gs2, r_gs2 = dbl([128, 512], F32, "gs2")
            xin2, r_xin2 = dbl([128, D], F32, "xin2")
            NSB = 3
            SBK = [0, 1, 4]
            Et = [ar.alloc([128, 512], BF16, "Et") for _ in range(NSB)]; r_Et = [Res() for _ in range(NSB)]
            Pt = [ar.alloc([128, 512], BF16, "Pt") for _ in range(NSB)]; r_Pt = [Res() for _ in range(NSB)]
            rec8 = ar.alloc([128, 8], F32, "rec8"); r_rec8 = Res()
            mixed_b = ar.alloc([128, D], BF16, "mixed_b"); r_mixa = Res(); r_mixr = Res()
            mixedT = ar.alloc([128, 8, 128], BF16, "mixedT"); r_mixedT = Res()
            Pr = ar.alloc([128, 8, 128], BF16, "Pr"); r_Pr = Res()
            state_f = ar.alloc([128, 4, 128], F32, "state_f"); r_state_f = Res()
            state_b = ar.alloc([128, 4, 128], BF16, "state_b"); r_state_b = Res()
            yv = ar.alloc([128, 8, 64], F32, "yv"); r_yv = Res()
            ysq = ar.alloc([128, 8, 64], F32, "ysq"); r_ysq = Res()
            st8 = ar.alloc([128, 32], F32, "st8"); r_st8 = Res()
            r1 = ar.alloc([128, D], F32, "r1"); r_r1 = Res()
            x1 = ar.alloc([128, D], F32, "x1"); r_x1 = Res()
            x1_b = ar.alloc([128, D], BF16, "x1_b"); r_x1b = Res()
            x1T = ar.alloc([128, 8, 128], BF16, "x1T"); r_x1T = Res()
            qcT = ar.alloc([128, 8, 128], BF16, "qcT"); r_qcT = Res()
            ET = ar.alloc([128, 8, 128], BF16, "ET"); r_ET = Res()
            oc_b = ar.alloc([128, D], BF16, "oc_b"); r_ocb = Res()
            ocT = ar.alloc([128, 8, 128], BF16, "ocT"); r_ocT = Res()
            rec4 = ar.alloc([128, 4], F32, "rec4"); r_rec4 = Res()
            r2 = r1; r_r2 = r_r1
            x2s, r_x2s = dbl([128, D], F32, "x2s")
            x2_b = ar.alloc([128, D], BF16, "x2_b"); r_x2b = Res()
            x2T_b, r_x2Tb = dbl([128, 8, 128], BF16, "x2T_b")
            x2T_f = ar.alloc([128, 8, 128], F32, "x2T_f"); r_x2Tf = Res()
            lg = ar.alloc([128, NEXP], F32, "lg"); r_lg = Res()
            mx8 = ar.alloc([128, 8], F32, "mx8")
            msk = ar.alloc([128, NEXP], F32, "msk")
            ex = ar.alloc([128, NEXP], F32, "ex")
            sm = ar.alloc([128, 4], F32, "sm")
            gat, r_gat = dbl([128, NEXP], F32, "gat")
            lntmp = (ar.alloc([128, 12], F32, "st6"), ar.alloc([128, 2], F32, "mv"), ar.alloc([128, 1], F32, "rstd"), ar.alloc([128, 1], F32, "nmr"))
            sc.op("pool", lambda e: e.memset(state_f[:], 0.0), writes=[r_state_f])
            sc.op("pool", lambda e: e.memset(state_b[:], 0.0), writes=[r_state_b])
            kfac_t = rtab2[:, 0:8]; qdec_t = rtab2[:, 8:16]; cdp_t = rtab2[:, 16:20]
            r_rt2 = Res()
            print("A2 arena top", ar.off, "kring off", kring.offset, "qaT2", qaT2[0].offset, qaT2[1].offset)

            CUT2 = int(os.environ.get('KCUT2', '99'))
            KATT = int(os.environ.get('KATT', '99'))
            KRET = int(os.environ.get('KRET', '99'))
            def a2_loads(t):
                b = t % NB2
                tok = slice(t * 128, (t + 1) * 128)
                slot = t % R
                sc.dma("sp", qaT2[b][:], QAT[:, :, tok], writes=[r_qaT2[b]])
                sc.dma("sp", kring[:, :, slot * 128:(slot + 1) * 128], KAT[:, :, tok], writes=[r_kring[slot]])
                sc.dma("sp", vring[:, slot, :], VA[tok, :], writes=[r_vring[slot]])
                sc.dma("sp", qrT2[b][:], QRT[:, :, tok], writes=[r_qrT2[b]])
                sc.dma("sp", krT2[b][:], KRT[:, :, tok], writes=[r_krT2[b]])
                sc.dma("sp", kr2[b][:], KR[tok, :], writes=[r_kr2[b]])
                sc.dma("sp", vr2[b][:], VR[tok, :], writes=[r_vr2[b]])
                sc.dma("sp", gs2[b][:], GS[tok, :], writes=[r_gs2[b]])
                sc.dma("sp", xin2[b][:], x_d[tok, :], writes=[r_xin2[b]])

            def a2_att_thunks(t):
                b = t % NB2
                tok = slice(t * 128, (t + 1) * 128)
                slot = t % R
                kbs = list(range(max(0, t - 16), t + 1))
                steps = []
                for h in range(0, 8, 2 if os.environ.get('KEVEN') else 1):
                    groups = [kbs[i:i + 4] for i in range(0, len(kbs), 4)]
                    for gi, g in enumerate(groups):
                        steps.append((h, g, gi == 0, gi == len(groups) - 1))
                Ob = [bank_f32(2)[:, 0:4 * VS].rearrange("p (h d) -> p h d", h=4), bank_f32(3)[:, 0:4 * VS].rearrange("p (h d) -> p h d", h=4)]

                def stageA(i, st):
                    h, g, first, last = st
                    si = i % NSB
                    sb = SBK[si]
                    p, hb = h // 2, 64 * (h % 2)
                    n = len(g)
                    Sb = bank_f32(sb)
                    if os.environ.get('KFULLK') == '2':
                        fns = [mm(Sb[:, j * 128:(j + 1) * 128], kmT[:, 0, 0:128], kmT[:, 1, 0:128])
                               for j, kb in enumerate(g)]
                    elif os.environ.get('KFULLK') == '3':
                        fns = [mm(Sb[:, j * 128:(j + 1) * 128], kmT[:, 0, 0:128], qaT2[b][:, p, :])
                               for j, kb in enumerate(g)]
                    elif os.environ.get('KFULLK'):
                        fns = [mm(Sb[:, j * 128:(j + 1) * 128], kring[:, p, (kb % R) * 128:(kb % R + 1) * 128], qaT2[b][:, p, :])
                               for j, kb in enumerate(g)]
                    else:
                        fns = [mm(Sb[:, j * 128:(j + 1) * 128], kring[hb:hb + 64, p, (kb % R) * 128:(kb % R + 1) * 128], qaT2[b][hb:hb + 64, p, :])
                               for j, kb in enumerate(g)]
                    if os.environ.get('KNOREADS'):
                        sc.ops("pe", fns, reads=[], writes=[bres[sb]])
                    else:
                        sc.ops("pe", fns, reads=[r_qaT2[b]] + [r_kring[kb % R] for kb in g], writes=[bres[sb]])
                    if KATT < 2:
                        return
                    sc.op("act", actf(Et[si][:, 0:n * 128], Sb[:, 0:n * 128], AF.Exp), reads=[bres[sb]], writes=[r_Et[si]])
                    if KATT < 3:
                        return
                    j0 = g[0] - t + 16
                    msk_ap = am[:, j0:j0 + n, :].rearrange("p j q -> p (j q)")
                    eng = "dve"
                    sc.op(eng, tt(Pt[si][:, 0:n * 128], Et[si][:, 0:n * 128], msk_ap, ALU.mult), reads=[r_Et[si], r_const], writes=[r_Pt[si]])

                def stageB(i, st):
                    if KATT < 4:
                        return
                    h, g, first, last = st
                    si = i % NSB
                    fns = []
                    for j, kb in enumerate(g):
                        fns.append(mm(Ob[h // 4][:, h % 4, :], Pt[si][:, j * 128:(j + 1) * 128], vring[:, kb % R, h * VS:(h + 1) * VS],
                                      first and j == 0, last and j == len(g) - 1))
                    sc.ops("pe", fns, reads=[r_Pt[si]] + [r_vring[kb % R] for kb in g], writes=[bres[2 + h // 4]])

                def att_final():
                    for i_ in range(max(0, len(steps) - (NSB - 1)), len(steps)):
                        stageB(i_, steps[i_])
                    mix3 = mixed_b[:, 0:512].rearrange("p (h d) -> p h d", h=8)
                    for hh in range(2):
                        sc.op("dve", lambda e, hh=hh: e.reciprocal(out=rec8[:, hh * 4:(hh + 1) * 4], in_=Ob[hh][:, :, 64]), reads=[bres[2 + hh]], writes=[r_rec8])
                        sc.op("dve", tt(mix3[:, hh * 4:(hh + 1) * 4, :], Ob[hh][:, :, 0:64], rec8[:, hh * 4:(hh + 1) * 4].unsqueeze(2).to_broadcast([128, 4, 64]), ALU.mult),
                              reads=[bres[2 + hh], r_rec8], writes=[r_mixa])

                thunks = []
                for i, st in enumerate(steps):
                    def th(i=i, st=st):
                        stageA(i, st)
                        if i >= NSB - 1:
                            stageB(i - (NSB - 1), steps[i - (NSB - 1)])
                    thunks.append(th)
                thunks.append(att_final)
                return thunks

            def a2_ret(t):
                b = t % NB2
                tok = slice(t * 128, (t + 1) * 128)
                slot = t % R
                Sr = [bank_f32(4).rearrange("p (h q) -> p h q", h=4), bank_f32(5).rearrange("p (h q) -> p h q", h=4)]
                for hh in range(2):
                    fns = []
                    for hl in range(4):
                        h = hh * 4 + hl
                        p, hb = h // 2, 64 * (h % 2)
                        fns.append(mm(Sr[hh][:, hl, :], krT2[b][hb:hb + 64, p, :], qrT2[b][hb:hb + 64, p, :]))
                        fns.append(mm(bank_f32(1)[:, 0:128], identb[:], identb[:]))
                    sc.ops("pe", fns, reads=[r_krT2[b], r_qrT2[b], r_const], writes=[bres[4 + hh], bres[1]])
                    sc.op("dve", tt(Pr[:, hh * 4:(hh + 1) * 4, :], Sr[hh], cm[:, :].unsqueeze(1).to_broadcast([128, 4, 128]), ALU.mult),
                          reads=[bres[4 + hh], r_const], writes=[r_Pr])
                Rb = bank_f32(6).rearrange("p (h d) -> p h d", h=8)
                Cb = bank_f32(0).rearrange("p (h d) -> p h d", h=8)
                fns = []
                for h in range(8):
                    fns.append(mm(Rb[:, h, :], Pr[:, h, :], vr2[b][:, h * 64:(h + 1) * 64], True, True))
                sc.ops("pe", fns, reads=[r_Pr, r_vr2[b]], writes=[bres[6]])
                fns = []
                for h in range(8):
                    p, hb = h // 2, 64 * (h % 2)
                    fns.append(mm(Cb[:, h, :], qrT2[b][hb:hb + 64, p, :], state_b[hb:hb + 64, p, hb:hb + 64], True, True))
                    fns.append(mm(bank_f32(1)[:, 0:128], identb[:], identb[:]))
                sc.ops("pe", fns, reads=[r_qrT2[b], r_state_b, r_const], writes=[bres[0], bres[1]])
                KVb = bank_f32(7).rearrange("p (a c) -> p a c", a=4)
                sc.ops("pe", [mm(KVb[:, p, :], kr2[b][:, p * 128:(p + 1) * 128], vr2[b][:, p * 128:(p + 1) * 128]) for p in range(4)],
                       reads=[r_kr2[b], r_vr2[b]], writes=[bres[7]])
                sc.op("dve", tt(state_f[:], state_f[:], KVb, ALU.add), reads=[bres[7], r_state_f], writes=[r_state_f])
                sc.op("dve", tt(state_f[:], state_f[:], cdp_t.unsqueeze(2).to_broadcast([128, 4, 128]), ALU.mult), reads=[r_state_f, r_const], writes=[r_state_f])
                sc.op("act", actf(state_b[:], state_f[:], AF.Copy), reads=[r_state_f], writes=[r_state_b])
                sc.op("dve", tt(yv[:], Rb, qdec_t.unsqueeze(2).to_broadcast([128, 8, 64]), ALU.mult), reads=[bres[6], r_const], writes=[r_yv])
                sc.op("dve", tt(ysq[:], Cb, qdec_t.unsqueeze(2).to_broadcast([128, 8, 64]), ALU.mult), reads=[bres[0], r_const], writes=[r_ysq])
                sc.op("dve", tt(yv[:], yv[:], ysq[:], ALU.add), reads=[r_yv, r_ysq], writes=[r_yv])
                sc.op("dve", red(st8[:, 0:8], yv[:]), reads=[r_yv], writes=[r_st8])
                sc.op("dve", ts(st8[:, 8:16], st8[:, 0:8], 1.0 / 64.0, None, ALU.mult), reads=[r_st8], writes=[r_st8])
                sc.op("dve", tt(yv[:], yv[:], st8[:, 8:16].unsqueeze(2).to_broadcast([128, 8, 64]), ALU.subtract), reads=[r_yv, r_st8], writes=[r_yv])
                sc.op("dve", tt(ysq[:], yv[:], yv[:], ALU.mult), reads=[r_yv], writes=[r_ysq])
                sc.op("dve", red(st8[:, 16:24], ysq[:]), reads=[r_ysq], writes=[r_st8])
                sc.op("act", actf(st8[:, 24:32], st8[:, 16:24], AF.Ln, bias=epsc[:, 0:1], scale=1.0 / 64.0), reads=[r_st8, r_const], writes=[r_st8])
                sc.op("act", actf(st8[:, 24:32], st8[:, 24:32], AF.Exp, scale=-0.5), reads=[r_st8], writes=[r_st8])
                sc.op("dve", tt(yv[:], yv[:], st8[:, 24:32].unsqueeze(2).to_broadcast([128, 8, 64]), ALU.mult), reads=[r_yv, r_st8], writes=[r_yv])
                yflat = yv[:].rearrange("p h d -> p (h d)")
                sc.op("dve", tt(yflat, yflat, rng[:], ALU.mult), reads=[r_yv, r_const], writes=[r_yv])
                sc.op("dve", tt(mixed_b[:, 512:1024], yflat, gs2[b][:], ALU.mult), reads=[r_yv, r_gs2[b]], writes=[r_mixr])

            def a2_tail(t):
                b = t % NB2
                tok = slice(t * 128, (t + 1) * 128)
                slot = t % R
                pTm = bank_bf16(7).rearrange("p (k t) -> p k t", k=8)
                sc.ops("pe", [tr(pTm[:, k, :], mixed_b[:, k * 128:(k + 1) * 128], identb[:]) for k in range(8)],
                       reads=[r_mixa, r_mixr, r_const], writes=[bres[7]])
                sc.op("act", actf(mixedT[:], pTm, AF.Copy), reads=[bres[7]], writes=[r_mixedT])
                yield
                for hh in range(2):
                    pb = bank_f32(5 + hh)
                    sc.ops("pe", [mm(pb, mixedT[:, k, :], w_out_b[:, k, hh * 512:(hh + 1) * 512], k == 0, k == 7) for k in range(8)],
                           reads=[r_mixedT, r_w2], writes=[bres[5 + hh]])
                    sc.op("dve", stt(r1[:, hh * 512:(hh + 1) * 512], xin2[b][:, hh * 512:(hh + 1) * 512], ALPHA, pb, ALU.mult, ALU.add),
                          reads=[bres[5 + hh], r_xin2[b]], writes=[r_r1])
                    yield
                layer_norm(r1[:], r_r1, x1[:], r_x1, lnp[:, 0, :], lnp[:, 1, :], lntmp)
                yield
                sc.op("act", actf(x1_b[:], x1[:], AF.Copy), reads=[r_x1], writes=[r_x1b])
                yield
                pTm = bank_bf16(7).rearrange("p (k t) -> p k t", k=8)
                sc.ops("pe", [tr(pTm[:, k, :], x1_b[:, k * 128:(k + 1) * 128], identb[:]) for k in range(8)], reads=[r_x1b, r_const], writes=[bres[7]])
                sc.op("act", actf(x1T[:], pTm, AF.Copy), reads=[bres[7]], writes=[r_x1T])
                yield
                for hh in range(2):
                    Qb = bank_f32([7, 5][hh]).rearrange("p (c t) -> p c t", c=4)
                    fns = []
                    for fl in range(4):
                        fc = hh * 4 + fl
                        fns += [mm(Qb[:, fl, :], wq_b[:, k, fc * 128:(fc + 1) * 128], x1T[:, k, :], k == 0, k == 7) for k in range(8)]
                    sc.ops("pe", fns, reads=[r_x1T, r_w2], writes=[bres[[7, 5][hh]]])
                    sc.op("act", actf(qcT[:, hh * 4:(hh + 1) * 4, :], Qb, AF.Copy, scale=1.0 / 16.0), reads=[bres[[7, 5][hh]]], writes=[r_qcT])
                    yield
                for hh in range(2):
                    Scb = bank_f32(6 + hh).rearrange("p (c t) -> p c t", c=4)
                    fns = []
                    for il in range(4):
                        idx = hh * 4 + il
                        hm, mc = idx // 2, idx % 2
                        for c in range(2):
                            fns.append(mm(Scb[:, il, :], kmT[:, 2 * hm + c, mc * 128:(mc + 1) * 128], qcT[:, 2 * hm + c, :], c == 0, c == 1))
                    sc.ops("pe", fns, reads=[r_qcT, r_km], writes=[bres[6 + hh]])
                    sc.op("act", actf(ET[:, hh * 4:(hh + 1) * 4, :], Scb, AF.Exp), reads=[bres[6 + hh]], writes=[r_ET])
                    yield
                denb = bank_f32(6)[:, 0:32].rearrange("p (h c) -> p h c", h=4)
                fns = []
                for hm in range(4):
                    for mc in range(2):
                        fns.append(mm(denb[:, hm, :], ET[:, hm * 2 + mc, :], ones_b[:, 0:8], mc == 0, mc == 1))
                sc.ops("pe", fns, reads=[r_ET, r_const], writes=[bres[6]])
                sc.op("dve", lambda e: e.reciprocal(out=rec4[:, :], in_=denb[:, :, 0]), reads=[bres[6]], writes=[r_rec4])
                yield
                for hh in range(2):
                    Ocb = bank_f32([7, 5][hh]).rearrange("p (c t) -> p c t", c=2)
                    fns = []
                    for hl in range(2):
                        hm = hh * 2 + hl
                        for mc in range(2):
                            fns.append(mm(Ocb[:, hl, :], ET[:, hm * 2 + mc, :], vm_b[:, mc, hm * 256:(hm + 1) * 256], mc == 0, mc == 1))
                    sc.ops("pe", fns, reads=[r_ET, r_vm], writes=[bres[[7, 5][hh]]])
                    sc.op("dve", tt(oc_b[:, hh * 512:(hh + 1) * 512].rearrange("p (c t) -> p c t", c=2), Ocb,
                                    rec4[:, hh * 2:(hh + 1) * 2].unsqueeze(2).to_broadcast([128, 2, 256]), ALU.mult),
                          reads=[bres[[7, 5][hh]], r_rec4], writes=[r_ocb])
                    yield
                pTm = bank_bf16(7).rearrange("p (k t) -> p k t", k=8)
                sc.ops("pe", [tr(pTm[:, k, :], oc_b[:, k * 128:(k + 1) * 128], identb[:]) for k in range(8)], reads=[r_ocb, r_const], writes=[bres[7]])
                sc.op("act", actf(ocT[:], pTm, AF.Copy), reads=[bres[7]], writes=[r_ocT])
                yield
                for hh in range(2):
                    pb = bank_f32(6 + hh)
                    sc.ops("pe", [mm(pb, ocT[:, k, :], wo_b[:, k, hh * 512:(hh + 1) * 512], k == 0, k == 7) for k in range(8)],
                           reads=[r_ocT, r_w2], writes=[bres[6 + hh]])
                    sc.op("dve", stt(r2[:, hh * 512:(hh + 1) * 512], x1[:, hh * 512:(hh + 1) * 512], ALPHA, pb, ALU.mult, ALU.add),
                          reads=[bres[6 + hh], r_x1], writes=[r_r2])
                    yield
                layer_norm(r2[:], r_r2, x2s[b][:], r_x2s[b], lnp[:, 2, :], lnp[:, 3, :], lntmp)
                yield
                sc.dma("sp", X2[tok, :], x2s[b][:], reads=[r_x2s[b]])
                sc.op("act", actf(x2_b[:], x2s[b][:], AF.Copy), reads=[r_x2s[b]], writes=[r_x2b])
                yield
                sc.dma("sp", X2B[tok, :], x2_b[:], reads=[r_x2b])
                pTm = bank_bf16(7).rearrange("p (k t) -> p k t", k=8)
                sc.ops("pe", [tr(pTm[:, k, :], x2_b[:, k * 128:(k + 1) * 128], identb[:]) for k in range(8)], reads=[r_x2b, r_const], writes=[bres[7]])
                sc.op("act", actf(x2T_b[b][:], pTm, AF.Copy), reads=[bres[7]], writes=[r_x2Tb[b]])
                yield
                sc.dma("sp", X2T[:, :, tok], x2T_b[b][:], reads=[r_x2Tb[b]])
                for hh in range(2):
                    pTf = bank_f32(5 + hh).rearrange("p (k t) -> p k t", k=4)
                    sc.ops("pe", [tr(pTf[:, k, :], x2s[b][:, (hh * 4 + k) * 128:(hh * 4 + k + 1) * 128], identf[:]) for k in range(4)],
                           reads=[r_x2s[b], r_const], writes=[bres[5 + hh]])
                    sc.op("dve", cp(x2T_f[:, hh * 4:(hh + 1) * 4, :], pTf), reads=[bres[5 + hh]], writes=[r_x2Tf])
                    yield
                lgb = bank_f32(7)[:, 0:NEXP]
                fns = [mm(lgb, x2T_f[:, k, :], rw_f[:, k, :], k == 0, False) for k in range(8)]
                fns.append(mm(lgb, ones_f[0:1, :], rb_f[0:1, :], False, True))
                sc.ops("pe", fns, reads=[r_x2Tf, r_const], writes=[bres[7]])
                sc.op("dve", cp(lg[:], lgb), reads=[bres[7]], writes=[r_lg])
                yield
                sc.op("dve", lambda e: e.max(out=mx8[:, 0:8], in_=lg[:, :]), reads=[r_lg], writes=[r_rt2])
                yield
                sc.op("dve", ts(msk[:], lg[:], mx8[:, 3:4], None, ALU.is_ge), reads=[r_lg, r_rt2], writes=[r_rt2])
                yield
                sc.op("dve", ts(sm[:, 0:1], mx8[:, 0:1], -1.0, None, ALU.mult), reads=[r_rt2], writes=[r_rt2])
                yield
                sc.op("act", actf(ex[:], lg[:], AF.Exp, bias=sm[:, 0:1]), reads=[r_lg, r_rt2], writes=[r_rt2])
                yield
                sc.op("dve", tt(ex[:], ex[:], msk[:], ALU.mult), reads=[r_rt2], writes=[r_rt2])
                yield
                sc.op("dve", red(sm[:, 1:2], ex[:]), reads=[r_rt2], writes=[r_rt2])
                yield
                sc.op("dve", lambda e: e.reciprocal(out=sm[:, 2:3], in_=sm[:, 1:2]), reads=[r_rt2], writes=[r_rt2])
                yield
                sc.op("dve", ts(gat[b][:], ex[:], sm[:, 2:3], None, ALU.mult), reads=[r_rt2], writes=[r_gat[b]])
                yield
                sc.dma("sp", GT[tok, :], gat[b][:], reads=[r_gat[b]])
                rkb = bank_f32(7)[:, 32:96]
                sc.ops("pe", [mm(rkb[:, 0:32], ustr[:], msk[:]), mm(rkb[:, 32:64], ones_f[:], msk[:])], reads=[r_rt2, r_const], writes=[bres[7]])
                sc.op("dve", tt(rkt[b][:], rbase[:], rkb[:, 0:32], ALU.add), reads=[bres[7], r_rbase], writes=[r_rkt[b]])
                yield
                sc.op("dve", tt(rbase[:], rbase[:], rkb[:, 32:64], ALU.add), reads=[bres[7], r_rbase], writes=[r_rbase])
                yield
                sc.dma("sp", RKD[tok, :], rkt[b][:], reads=[r_rkt[b]])
                yield

            a2_loads(0)
            for th in a2_att_thunks(0):
                th()
            a2_ret(0)
            LEAD = 6
            for t in range(NT):
                gen = a2_tail(t)
                alive = True
                if t + 1 < NT:
                    a2_loads(t + 1)
                    thunks = a2_att_thunks(t + 1)
                    for ti, th in enumerate(thunks):
                        if ti == len(thunks) - 1 and ti < LEAD:
                            next(gen)
                        th()
                        if alive and (ti >= LEAD - 1 or ti == len(thunks) - 1):
                            try:
                                next(gen)
                            except StopIteration:
                                alive = False
                for _ in gen:
                    pass
                if t + 1 < NT:
                    a2_ret(t + 1)
            sc.dma("sp", CNT[:, :], rbase[:], reads=[r_rbase])
            sc.barrier()

        if stop_after == "B" and SPARSE:
            I32 = mybir.dt.int32
            ar.reset(base_mark)
            slk_i = ar.alloc([128, NT, 4], I32, "slk_i"); r_slk = Res()
            wk = ar.alloc([128, NT, 4], F32, "wk"); r_wk = Res()
            widx_i = ar.alloc([128, NBLK, 8], I32, "widx_i"); r_widx = Res()
            ebf = ar.alloc([128, NBLK], F32, "ebf"); r_ebf = Res()
            widx2_i = ar.alloc([128, NBLK], I32, "widx2_i"); r_widx2 = Res()
            cmeta = ar.alloc([128, CMW], F32, "cmeta")
            bgT = ar.alloc([128, NEXP, 8], F32, "bgT"); buT = ar.alloc([128, NEXP, 8], F32, "buT")
            bd32 = ar.alloc([32, D], F32, "bd32"); bd16 = ar.alloc([32, D], BF16, "bd16")
            lnp3 = ar.alloc([128, 2, D], F32, "lnp3")
            c7 = ar.alloc([128, 512], F32, "c7")
            o_ = 0
            thr16 = cmeta[:, o_:o_ + 16]; o_ += 17
            blkthr = cmeta[:, o_:o_ + NBLK]; o_ += NBLK
            iota_e = cmeta[:, o_:o_ + NEXP]; o_ += NEXP
            kcp = cmeta[:, o_:o_ + 8]; o_ += 8
            iota_p = cmeta[:, o_:o_ + 1]; o_ += 1
            tril = cmeta[:, o_:o_ + NEXP * NEXP].rearrange("p (a b) -> p a b", a=NEXP)
            sc.dma("sp", cmeta[:], cmeta_d[:, :], writes=[r_const])
            sc.dma("sp", bgT[:], bgT_d[:, :, :], writes=[r_const])
            sc.dma("sp", buT[:], buT_d[:, :, :], writes=[r_const])
            sc.dma("sp", lnp3[:], lnp_d[:, 4:6, :], writes=[r_const])
            sc.dma("sp", bd32[:], bd_d[:, :], writes=[r_const])
            sc.op("pool", lambda e: e.memset(c7[:], 7.0), writes=[r_const])
            sc.op("dve", ts(buT[:], buT[:], 7.0, None, ALU.add), reads=[r_const], writes=[r_const])
            sc.op("dve", cp(bd16[:], bd32[:]), reads=[r_const], writes=[r_const])
            mB = ar.mark()
            oh = ar.alloc([128, NBLK, NEXP], F32, "oh"); r_oh = Res()
            rankA = ar.alloc([128, NT, NEXP], F32, "rankA"); r_rankA = Res()
            gatA = ar.alloc([128, NT, NEXP], F32, "gatA"); r_gatA = Res()
            maskA = ar.alloc([128, NT, NEXP], F32, "maskA"); r_maskA = Res()
            top8A = ar.alloc([128, NT, 8], F32, "top8A"); r_top8 = Res()
            cntt = ar.alloc([128, NEXP], F32, "cntt"); r_cnt = Res()
            cmp16 = ar.alloc([128, NEXP, 16], F32, "cmp16")
            t32 = ar.alloc([128, NEXP, NEXP], F32, "t32")
            nblk_t = ar.alloc([128, NEXP], F32, "nblk_t"); padded = ar.alloc([128, NEXP], F32, "padded")
            pend = ar.alloc([128, NEXP], F32, "pend"); pstart = ar.alloc([128, NEXP], F32, "pstart")
            eq = ar.alloc([128, 4, NEXP], F32, "eq"); r_eq = Res()
            xs_t = [ar.alloc([128, D], BF16, "xs_t") for _ in range(3)]; r_xs = [Res() for _ in range(3)]
            r_m = Res()
            sc.dma("sp", rankA[:], RKD.rearrange("(t p) e -> p t e", p=128), writes=[r_rankA])
            sc.dma("sp", gatA[:], GT.rearrange("(t p) e -> p t e", p=128), writes=[r_gatA])
            sc.dma("sp", cntt[:], CNT[:, :], writes=[r_cnt])
            sc.op("dve", tt(cmp16[:], cntt[:, :].unsqueeze(2).to_broadcast([128, NEXP, 16]), thr16.unsqueeze(1).to_broadcast([128, NEXP, 16]), ALU.is_gt),
                  reads=[r_cnt, r_const], writes=[r_m])
            sc.op("dve", red(nblk_t[:], cmp16[:]), reads=[r_m], writes=[r_m])
            sc.op("dve", ts(padded[:], nblk_t[:], 512.0, None, ALU.mult), reads=[r_m], writes=[r_m])
            sc.op("dve", tt(t32[:], tril, padded[:, :].unsqueeze(1).to_broadcast([128, NEXP, NEXP]), ALU.mult), reads=[r_m, r_const], writes=[r_m])
            sc.op("dve", red(pend[:], t32[:]), reads=[r_m], writes=[r_m])
            sc.op("dve", tt(pstart[:], pend[:], padded[:], ALU.subtract), reads=[r_m], writes=[r_m])
            sc.op("dve", tt(oh[:], pend[:, :].unsqueeze(1).to_broadcast([128, NBLK, NEXP]), blkthr.unsqueeze(2).to_broadcast([128, NBLK, NEXP]), ALU.is_le),
                  reads=[r_m, r_const], writes=[r_oh])
            sc.op("dve", red(ebf[:], oh[:]), reads=[r_oh], writes=[r_ebf])
            sc.op("dve", ts(ebf[:], ebf[:], float(NEXP - 1), None, ALU.min), reads=[r_ebf], writes=[r_ebf])
            sc.op("dve", stt(widx_i[:], ebf[:, :].unsqueeze(2).to_broadcast([128, NBLK, 8]), 1024.0, kcp.unsqueeze(1).to_broadcast([128, NBLK, 8]), ALU.mult, ALU.add),
                  reads=[r_ebf, r_const], writes=[r_widx])
            sc.op("dve", stt(widx2_i[:], ebf[:], 128.0, iota_p.to_broadcast([128, NBLK]), ALU.mult, ALU.add), reads=[r_ebf, r_const], writes=[r_widx2])
            sc.op("dve", tt(rankA[:], rankA[:], pstart[:, :].unsqueeze(1).to_broadcast([128, NT, NEXP]), ALU.add), reads=[r_rankA, r_m], writes=[r_rankA])
            sc.op("dve", ts(maskA[:], gatA[:], 0.0, None, ALU.is_gt), reads=[r_gatA], writes=[r_maskA])
            sc.op("dve", stt(rankA[:], rankA[:], 1.0, maskA[:], ALU.add, ALU.mult), reads=[r_rankA, r_maskA], writes=[r_rankA])
            for t in range(NT):
                sc.op("dve", lambda e, t=t: e.max(out=top8A[:, t, :], in_=rankA[:, t, :]), reads=[r_rankA], writes=[r_top8])
            sc.op("dve", ts(slk_i[:], top8A[:, :, 0:4], -1.0, None, ALU.add), reads=[r_top8], writes=[r_slk])
            for t in range(NT):
                sc.op("dve", tt(eq[:], rankA[:, t, :].unsqueeze(1).to_broadcast([128, 4, NEXP]), top8A[:, t, 0:4].unsqueeze(2).to_broadcast([128, 4, NEXP]), ALU.is_equal),
                      reads=[r_rankA, r_top8], writes=[r_eq])
                sc.op("dve", tt(eq[:], eq[:], gatA[:, t, :].unsqueeze(1).to_broadcast([128, 4, NEXP]), ALU.mult), reads=[r_eq, r_gatA], writes=[r_eq])
                sc.op("dve", red(wk[:, t, :], eq[:]), reads=[r_eq], writes=[r_wk])
            zt = ar.alloc([128, 4, D], BF16, "zt"); r_zt = Res()
            sc.op("pool", lambda e: e.memset(zt[:], 0.0), writes=[r_zt])
            hz = {}
            for zb in range(NBLK):
                hh_ = sc.dma("sp", XS[zb * 512:(zb + 1) * 512, :].rearrange("(i p) d -> p i d", p=128), zt[:], reads=[r_zt])
                for k_, v_ in hh_.items():
                    hz[k_] = max(hz.get(k_, 0), v_)
            for t in range(NT):
                xb_ = t % 3
                sc.dma("sp", xs_t[xb_][:], X2B[t * 128:(t + 1) * 128, :], writes=[r_xs[xb_]])
                for k4 in range(4):
                    sc.dma_fn("pool", lambda e, xb_=xb_, t=t, k4=k4: e.indirect_dma_start(
                        out=XS[:, :], out_offset=bass.IndirectOffsetOnAxis(ap=slk_i[:, t, k4:k4 + 1], axis=0),
                        in_=xs_t[xb_][:, :], in_offset=None),
                        reads=[r_xs[xb_], r_slk], extra=[hz])
            sc.barrier()
            ar.reset(mB)
            wsl = [[ar.alloc([128, 8, D], BF16, "w%d%d" % (i, j)) for j in range(3)] for i in range(2)]
            r_wsl = [[Res() for j in range(3)] for i in range(2)]
            xg = [ar.alloc([128, 4, D], BF16, "xg") for _ in range(2)]; r_xg = [Res(), Res()]
            xgT = [ar.alloc([128, 8, 512], BF16, "xgT") for _ in range(2)]; r_xgT = [Res(), Res()]
            hT = [ar.alloc([128, 8, 512], BF16, "hT") for _ in range(2)]; r_hT = [Res(), Res()]
            g1 = [ar.alloc([128, 512], F32, "g1") for _ in range(2)]; r_g1 = [Res(), Res()]
            sg = [ar.alloc([128, 512], F32, "sg") for _ in range(2)]; r_sg = [Res(), Res()]
            u1 = [ar.alloc([128, 512], F32, "u1") for _ in range(2)]; r_u1 = [Res(), Res()]
            yout = [ar.alloc([128, D], F32, "yout") for _ in range(2)]; r_yout = [Res(), Res()]
            bsel = [ar.alloc([128, 2, 8], F32, "bsel") for _ in range(2)]; r_bsel = [Res(), Res()]
            btmp = ar.alloc([128, 8, NEXP], F32, "btmp"); r_btmp = Res()
            ohb = ar.alloc([128, NEXP], F32, "ohb"); r_ohb = Res()
            oht = [ar.alloc([32, 128], BF16, "oht") for _ in range(2)]; r_oht = [Res(), Res()]
            wflat = [wg_d.rearrange("e r n -> (e r) n"), wu_d.rearrange("e r n -> (e r) n"), wd_d.rearrange("e r n -> (e r) n")]
            wpm = [wg_d.rearrange("e (p j) n -> (e p) (j n)", j=8), wu_d.rearrange("e (p j) n -> (e p) (j n)", j=8)]
            bgv = bgT[:].rearrange("p e f -> p f e")
            buv = buT[:].rearrange("p e f -> p f e")
            dcnt = 0

            def load_block(blk):
                sl = blk % 2
                if os.environ.get('KS_NOW') and blk >= 2:
                    return
                for j in range(2):
                    sc.dma_fn("pool", lambda e, sl=sl, j=j, blk=blk: e.indirect_dma_start(
                        out=wsl[sl][j][:, :, :].rearrange("p k n -> p (k n)"), out_offset=None,
                        in_=wpm[j][:, :],
                        in_offset=bass.IndirectOffsetOnAxis(ap=widx2_i[:, blk:blk + 1], axis=0)),
                        reads=[r_widx2], writes=[r_wsl[sl][j]])
                for j in (2,):
                    for kc in range(8):
                        sc.dma_fn("pool", lambda e, sl=sl, j=j, kc=kc, blk=blk: e.indirect_dma_start(
                            out=wsl[sl][j][:, kc, :], out_offset=None, in_=wflat[j][:, :],
                            in_offset=bass.IndirectOffsetOnAxis(ap=widx_i[:, blk, kc:kc + 1], axis=0)),
                            reads=[r_widx], writes=[r_wsl[sl][j]])

            def x_load(blk):
                sc.dma("sp", xg[blk % 2][:], XS[blk * 512:(blk + 1) * 512, :].rearrange("(i p) d -> p i d", p=128), writes=[r_xg[blk % 2]])

            def x_transposes(blk):
                xs_ = blk % 2
                for k2 in range(4):
                    bk = 6 + k2 % 2
                    pv = bank_bf16(bk).rearrange("p (a t) -> p a t", a=2)
                    fns = []
                    for a in range(2):
                        k = k2 * 2 + a
                        for i in range(4):
                            fns.append(tr(pv[:, a, i * 128:(i + 1) * 128], xg[xs_][:, i, :].rearrange("p (q j) -> p q j", j=8)[:, :, k], identb[:]))
                    sc.ops("pe", fns, reads=[r_xg[xs_], r_const], writes=[bres[bk]])
                    sc.op("act", actf(xgT[xs_][:, k2 * 2:k2 * 2 + 2, :], pv, AF.Copy), reads=[bres[bk]], writes=[r_xgT[xs_]])

            load_block(0)
            x_load(0)
            x_transposes(0)
            ycnt = 0
            for blk in range(NBLK):
                sl = blk % 2
                if blk + 1 < NBLK:
                    load_block(blk + 1)
                    x_load(blk + 1)
                sc.op("dve", ts(ohb[:], iota_e, ebf[:, blk:blk + 1], None, ALU.is_equal), reads=[r_ebf, r_const], writes=[r_ohb])
                for bi, bv in enumerate((bgv, buv)):
                    sc.op("dve", tt(btmp[:], bv, ohb[:, :].unsqueeze(1).to_broadcast([128, 8, NEXP]), ALU.mult), reads=[r_ohb, r_const], writes=[r_btmp])
                    sc.op("dve", red(bsel[sl][:, bi, :], btmp[:]), reads=[r_btmp], writes=[r_bsel[sl]])
                sc.op("dve", ts(oht[sl][:], ebf[0:32, blk:blk + 1].to_broadcast([32, 128]), iota_p[0:32, 0:1], None, ALU.is_equal), reads=[r_ebf, r_const], writes=[r_oht[sl]])
                for ffc in range(8):
                    fb = ffc % 2
                    pg = bank_f32(0 + fb); pu = bank_f32(2 + fb)
                    sc.ops("pe", [mm(pg, wsl[sl][0][:, k, ffc * 128:(ffc + 1) * 128], xgT[sl][:, k, :], k == 0, k == 7) for k in range(8)],
                           reads=[r_wsl[sl][0], r_xgT[sl]], writes=[bres[0 + fb]])
                    sc.ops("pe", [mm(pu, wsl[sl][1][:, k, ffc * 128:(ffc + 1) * 128], xgT[sl][:, k, :], k == 0, k == 7) for k in range(8)],
                           reads=[r_wsl[sl][1], r_xgT[sl]], writes=[bres[2 + fb]])
                    sc.op("dve", stt(g1[fb][:], pg, bsel[sl][:, 0, ffc:ffc + 1], c7[:], ALU.add, ALU.min), reads=[bres[0 + fb], r_bsel[sl], r_const], writes=[r_g1[fb]])
                    sc.op("act", actf(sg[fb][:], g1[fb][:], AF.Silu, scale=1.702), reads=[r_g1[fb]], writes=[r_sg[fb]])
                    sc.op("act", actf(u1[fb][:], pu, AF.Relu, bias=bsel[sl][:, 1, ffc:ffc + 1]), reads=[bres[2 + fb], r_bsel[sl]], writes=[r_u1[fb]])
                    sc.op("dve", ts(u1[fb][:], u1[fb][:], 14.0, -6.0, ALU.min, ALU.add), reads=[r_u1[fb]], writes=[r_u1[fb]])
                    sc.op("dve", stt(hT[sl][:, ffc, :], sg[fb][:], 1.0 / 1.702, u1[fb][:], ALU.mult, ALU.mult), reads=[r_sg[fb], r_u1[fb]], writes=[r_hT[sl]])
                if blk + 1 < NBLK:
                    x_transposes(blk + 1)
                for i in range(4):
                    yb_ = ycnt % 2
                    ycnt += 1
                    for colh in range(2):
                        bk = 4 + dcnt % 2
                        dcnt += 1
                        pd = bank_f32(bk)
                        fns = [mm(pd, hT[sl][:, k, i * 128:(i + 1) * 128], wsl[sl][2][:, k, colh * 512:(colh + 1) * 512], k == 0, False) for k in range(8)]
                        fns.append(mm(pd, oht[sl][0:32, :], bd16[0:32, colh * 512:(colh + 1) * 512], False, True))
                        sc.ops("pe", fns, reads=[r_hT[sl], r_wsl[sl][2], r_oht[sl], r_const], writes=[bres[bk]])
                        if bk == 4:
                            sc.op("act", actf(yout[yb_][:, colh * 512:(colh + 1) * 512], pd, AF.Copy), reads=[bres[bk]], writes=[r_yout[yb_]])
                        else:
                            sc.op("dve", cp(yout[yb_][:, colh * 512:(colh + 1) * 512], pd), reads=[bres[bk]], writes=[r_yout[yb_]])
                    sc.dma("sp", YS[blk * 512 + i * 128:blk * 512 + (i + 1) * 128, :], yout[yb_][:], reads=[r_yout[yb_]])
            sc.barrier()
            ar.reset(mB)
            acc = [ar.alloc([128, D], F32, "acc") for _ in range(2)]; r_acc = [Res(), Res()]
            gk = [[ar.alloc([128, D], F32, "gk") for _ in range(4)] for _ in range(2)]; r_gk = [[Res() for _ in range(4)] for _ in range(2)]
            outt = [ar.alloc([128, D], F32, "outt") for _ in range(2)]; r_outt = [Res(), Res()]
            lntmp3 = (ar.alloc([128, 12], F32, "st6b"), ar.alloc([128, 2], F32, "mvb"), ar.alloc([128, 1], F32, "rstdb"), ar.alloc([128, 1], F32, "nmrb"))
            for t in range(NT):
                b = t % 2
                tok = slice(t * 128, (t + 1) * 128)
                sc.dma("sp", acc[b][:], X2[tok, :], writes=[r_acc[b]])
                for k4 in range(4):
                    sc.dma_fn("pool", lambda e, b=b, t=t, k4=k4: e.indirect_dma_start(
                        out=gk[b][k4][:, :], out_offset=None, in_=YS[:, :],
                        in_offset=bass.IndirectOffsetOnAxis(ap=slk_i[:, t, k4:k4 + 1], axis=0)),
                        reads=[r_slk], writes=[r_gk[b][k4]])
                sc.op("dve", ts(acc[b][:], acc[b][:], ALPHA, None, ALU.mult), reads=[r_acc[b]], writes=[r_acc[b]])
                for k4 in range(4):
                    sc.op("dve", stt(acc[b][:], gk[b][k4][:], wk[:, t, k4:k4 + 1], acc[b][:], ALU.mult, ALU.add), reads=[r_gk[b][k4], r_wk, r_acc[b]], writes=[r_acc[b]])
                layer_norm(acc[b][:], r_acc[b], outt[b][:], r_outt[b], lnp3[:, 0, :], lnp3[:, 1, :], lntmp3)
                out_handles.append(sc.dma("sp", out_d[tok, :], outt[b][:], reads=[r_outt[b]]))
            sc.barrier()

        if stop_after == "B" and not SPARSE:
            ar.reset(base_mark)
            wsl = [[ar.alloc([128, 8, D], BF16, "w%d%d" % (i, j)) for j in range(3)] for i in range(2)]
            r_wsl = [[Res() for j in range(3)] for i in range(2)]
            x2T_c = ar.alloc([128, 8, TC], BF16, "x2T_c"); r_x2Tc = Res()
            Yacc = ar.alloc([128, 8, D], F32, "Yacc"); r_Y = [Res() for _ in range(8)]
            hT = [ar.alloc([128, 8, 512], BF16, "hT") for _ in range(2)]; r_hT = [Res(), Res()]
            g1 = [ar.alloc([128, 512], F32, "g1") for _ in range(2)]; r_g1 = [Res(), Res()]
            sg = [ar.alloc([128, 512], F32, "sg") for _ in range(2)]; r_sg = [Res(), Res()]
            u1 = [ar.alloc([128, 512], F32, "u1") for _ in range(2)]; r_u1 = [Res(), Res()]
            Gc = ar.alloc([128, 8, NEXP], F32, "Gc"); r_Gc = Res()
            GTs = ar.alloc([32, 128], F32, "GTs"); r_GTs = Res()
            bd32 = ar.alloc([32, D], F32, "bd32")
            bgT = ar.alloc([128, NEXP, 8], F32, "bgT"); buT = ar.alloc([128, NEXP, 8], F32, "buT")
            lnp3 = ar.alloc([128, 2, D], F32, "lnp3")
            c7 = ar.alloc([128, 512], F32, "c7")
            outt = [ar.alloc([128, D], F32, "outt") for _ in range(2)]; r_outt = [Res(), Res()]
            lntmp3 = (ar.alloc([128, 12], F32, "st6b"), ar.alloc([128, 2], F32, "mvb"), ar.alloc([128, 1], F32, "rstdb"), ar.alloc([128, 1], F32, "nmrb"))
            sc.dma("sp", bgT[:], bgT_d[:, :, :], writes=[r_const])
            sc.dma("sp", buT[:], buT_d[:, :, :], writes=[r_const])
            sc.dma("sp", lnp3[:], lnp_d[:, 4:6, :], writes=[r_const])
            sc.dma("sp", bd32[:], bd_d[:, :], writes=[r_const])
            sc.op("pool", lambda e: e.memset(c7[:], 7.0), writes=[r_const])
            sc.op("dve", ts(buT[:], buT[:], 7.0, None, ALU.add), reads=[r_const], writes=[r_const])
            wsrc = [wg_d, wu_d, wd_d]
            cnt = 0
            dcnt = 0
            ocnt = 0
            for c in range(S // TC):
                ctok = slice(c * TC, (c + 1) * TC)
                sc.dma("sp", x2T_c[:], X2T[:, :, ctok], writes=[r_x2Tc])
                sc.dma("sp", Gc[:], GT[ctok, :].rearrange("(t p) e -> p t e", p=128), writes=[r_Gc])
                for i in range(8):
                    sc.dma("sp", Yacc[:, i, :], X2[c * TC + i * 128:c * TC + (i + 1) * 128, :], writes=[r_Y[i]])
                    pgt = bank_f32(7)[0:32, 0:128]
                    sc.op("pe", tr(pgt, Gc[:, i, :], identf[:]), reads=[r_Gc, r_const], writes=[bres[7]])
                    sc.op("dve", cp(GTs[:], pgt), reads=[bres[7]], writes=[r_GTs])
                    for colh in range(2):
                        bk = 4 + dcnt % 3
                        dcnt += 1
                        pd = bank_f32(bk)
                        sc.op("pe", mm(pd, GTs[0:32, :], bd32[0:32, colh * 512:(colh + 1) * 512]), reads=[r_GTs, r_const], writes=[bres[bk]])
                        ysl = Yacc[:, i, colh * 512:(colh + 1) * 512]
                        sc.op("dve", stt(ysl, ysl, ALPHA, pd, ALU.mult, ALU.add), reads=[bres[bk], r_Y[i]], writes=[r_Y[i]])
                for ex_ in range(NEXP):
                    sl = cnt % 2
                    cnt += 1
                    for j in range(3):
                        v = wsrc[j][ex_].rearrange("(k p) n -> p k n", p=128)
                        for hh in range(2):
                            sc.dma("pool", wsl[sl][j][:, :, hh * 512:(hh + 1) * 512], v[:, :, hh * 512:(hh + 1) * 512], writes=[r_wsl[sl][j]])
                    for half in range(2):
                        hb_ = (cnt + half) % 2
                        for ffc in range(8):
                            fb = ffc % 2
                            pg = bank_f32(0 + fb); pu = bank_f32(2 + fb)
                            sc.ops("pe", [mm(pg, wsl[sl][0][:, k, ffc * 128:(ffc + 1) * 128], x2T_c[:, k, half * 512:(half + 1) * 512], k == 0, k == 7) for k in range(8)],
                                   reads=[r_wsl[sl][0], r_x2Tc], writes=[bres[0 + fb]])
                            sc.ops("pe", [mm(pu, wsl[sl][1][:, k, ffc * 128:(ffc + 1) * 128], x2T_c[:, k, half * 512:(half + 1) * 512], k == 0, k == 7) for k in range(8)],
                                   reads=[r_wsl[sl][1], r_x2Tc], writes=[bres[2 + fb]])
                            sc.op("dve", stt(g1[fb][:], pg, bgT[:, ex_, ffc:ffc + 1], c7[:], ALU.add, ALU.min), reads=[bres[0 + fb], r_const], writes=[r_g1[fb]])
                            sc.op("act", actf(sg[fb][:], g1[fb][:], AF.Silu, scale=1.702), reads=[r_g1[fb]], writes=[r_sg[fb]])
                            sc.op("act", actf(u1[fb][:], pu, AF.Relu, bias=buT[:, ex_, ffc:ffc + 1]), reads=[bres[2 + fb], r_const], writes=[r_u1[fb]])
                            sc.op("dve", ts(u1[fb][:], u1[fb][:], 14.0, -6.0, ALU.min, ALU.add), reads=[r_u1[fb]], writes=[r_u1[fb]])
                            sc.op("dve", stt(hT[hb_][:, ffc, :], sg[fb][:], 1.0 / 1.702, u1[fb][:], ALU.mult, ALU.mult), reads=[r_sg[fb], r_u1[fb]], writes=[r_hT[hb_]])
                        for i in range(4):
                            tl = half * 4 + i
                            for colh in range(2):
                                bk = 4 + dcnt % 3
                                dcnt += 1
                                pd = bank_f32(bk)
                                sc.ops("pe", [mm(pd, hT[hb_][:, k, i * 128:(i + 1) * 128], wsl[sl][2][:, k, colh * 512:(colh + 1) * 512], k == 0, k == 7) for k in range(8)],
                                       reads=[r_hT[hb_], r_wsl[sl][2]], writes=[bres[bk]])
                                ysl = Yacc[:, tl, colh * 512:(colh + 1) * 512]
                                sc.op("dve", stt(ysl, pd, Gc[:, tl, ex_:ex_ + 1], ysl, ALU.mult, ALU.add), reads=[bres[bk], r_Gc, r_Y[tl]], writes=[r_Y[tl]])
                for i in range(8):
                    ob = ocnt % 2
                    ocnt += 1
                    layer_norm(Yacc[:, i, :], r_Y[i], outt[ob][:], r_outt[ob], lnp3[:, 0, :], lnp3[:, 1, :], lntmp3)
                    out_handles.append(sc.dma("sp", out_d[c * TC + i * 128:c * TC + (i + 1) * 128, :], outt[ob][:], reads=[r_outt[ob]]))
            sc.barrier()

        sc.barrier()
        blk = stack.enter_context(nc.Block())
        sc.emit(blk)
    return nc, dbg


def _prep_shared(inputs, S):
    f = lambda a: np.ascontiguousarray(np.asarray(a, dtype=np.float32))
    sh = {}
    sh["w_in"] = f(inputs["w_in"][0])
    sh["w_out"] = f(inputs["w_out"][0])
    for k in ("mem_wq", "mem_wk", "mem_wv", "mem_wo"):
        sh[k] = f(inputs[k][0])
    sh["router_w"] = f(inputs["router_w"][0])
    sh["router_b"] = f(inputs["router_b"][0]).reshape(1, NEXP)
    sh["w_gate"] = f(inputs["w_gate"][0])
    sh["w_up"] = f(inputs["w_up"][0])
    sh["w_down"] = f(inputs["w_down"][0])
    sh["b_gateT"] = f(np.asarray(inputs["b_gate"][0]).reshape(NEXP, 8, 128).transpose(2, 0, 1))
    sh["b_upT"] = f(np.asarray(inputs["b_up"][0]).reshape(NEXP, 8, 128).transpose(2, 0, 1))
    sh["b_down"] = f(inputs["b_down"][0])
    lnp = np.stack([np.asarray(inputs[k][0], dtype=np.float32) for k in ("ln1_g", "ln1_b", "ln2_g", "ln2_b", "ln3_g", "ln3_b")], 0)
    sh["lnp"] = f(np.broadcast_to(lnp[None], (128, 6, D)))
    sh["rng"] = f(np.broadcast_to(np.asarray(inputs["ret_norm_g"][0], dtype=np.float32).reshape(1, 512), (128, 512)))
    sh.update(_const_tables(S))
    sh["cmeta"] = _meta_consts(S)[0]
    return sh


def kernel(**inputs):
    x = np.asarray(inputs["x"], dtype=np.float32)
    mem = np.asarray(inputs["mem"], dtype=np.float32)
    B, S, _ = x.shape
    sh = _prep_shared(inputs, S)
    nc, _ = build_program(NT=S // 128)
    in_maps = []
    for b in range(B):
        m = dict(sh)
        m["x"] = np.ascontiguousarray(x[b])
        m["mem"] = np.ascontiguousarray(mem[b])
        in_maps.append(m)
    res = run_bass_kernel_spmd(nc, in_maps, core_ids=list(range(B)))
    return np.stack([np.asarray(r["out"], dtype=np.float32) for r in res.results], axis=0)
```

```python
import math
from contextlib import ExitStack

import numpy as np
import ml_dtypes
import concourse.bass as bass
import concourse.mybir as mybir
from concourse.bass_utils import run_bass_kernel_spmd

F32 = mybir.dt.float32
BF16 = mybir.dt.bfloat16
AF = mybir.ActivationFunctionType
ALU = mybir.AluOpType
AX = mybir.AxisListType

D = 1024
NIN = 3584
NEXP = 32
NMEM = 256
ALPHA = 2.0 ** 0.25
EPS = 1e-5
NOFF = 17
TC = 1024
VS = 80
SPARSE = True


class Res:
    __slots__ = ("name", "w", "r")

    def __init__(self, name=""):
        self.name = name
        self.w = {}
        self.r = {}


class Sched:
    ENG = ("pe", "act", "dve", "pool", "sp")

    def __init__(self, nc, stack, n_dma_sems=24):
        self.nc = nc
        self.streams = {e: [] for e in self.ENG}
        self.sems = {}
        self.count = {}
        self.waited = {e: {} for e in self.ENG}
        for e in self.ENG:
            s = stack.enter_context(nc.semaphore("s_" + e))
            self.sems[e] = s
            self.count[e] = 0
        self.dq = {}
        for q in ("sp", "pool", "act"):
            lst = []
            for i in range(n_dma_sems):
                key = "d_%s_%d" % (q, i)
                self.sems[key] = stack.enter_context(nc.semaphore(key))
                self.count[key] = 0
                lst.append(key)
            self.dq[q] = [lst, 0]
        self.n_instr = 0

    def _wait(self, eng, key, val):
        if val <= 0:
            return
        if self.waited[eng].get(key, 0) >= val:
            return
        self.waited[eng][key] = val
        sem = self.sems[key]
        self.streams[eng].append(lambda e, sem=sem, val=val: e.wait_ge(sem, val))

    def _deps(self, eng, reads, writes, extra):
        deps = {}

        def add(d):
            for k, v in d.items():
                if deps.get(k, 0) < v:
                    deps[k] = v
        for r in reads:
            add(r.w)
        for w in writes:
            add(w.r)
            add(w.w)
        for h in extra:
            add(h)
        for k, v in deps.items():
            if k == eng and eng == "pe":
                continue
            self._wait(eng, k, v)

    def _post(self, reads, writes, h):
        for r in reads:
            for k, v in h.items():
                if r.r.get(k, 0) < v:
                    r.r[k] = v
        for w in writes:
            w.w = dict(h)
            w.r = {}

    def op(self, eng, fn, reads=(), writes=(), extra=()):
        self._deps(eng, reads, writes, extra)
        self.count[eng] += 1
        val = self.count[eng]
        sem = self.sems[eng]
        self.streams[eng].append(lambda e, fn=fn, sem=sem: fn(e).then_inc(sem, 1))
        h = {eng: val}
        self._post(reads, writes, h)
        self.n_instr += 1
        return h

    def ops(self, eng, fns, reads=(), writes=(), extra=()):
        self._deps(eng, reads, writes, extra)
        for fn in fns[:-1]:
            self.streams[eng].append(lambda e, fn=fn: fn(e))
        self.count[eng] += 1
        val = self.count[eng]
        sem = self.sems[eng]
        fn = fns[-1]
        self.streams[eng].append(lambda e, fn=fn, sem=sem: fn(e).then_inc(sem, 1))
        h = {eng: val}
        self._post(reads, writes, h)
        self.n_instr += len(fns)
        return h

    def dma(self, q, out, in_, reads=(), writes=(), extra=()):
        self._deps(q, reads, writes, extra)
        lst, i = self.dq[q]
        key = lst[i % len(lst)]
        self.dq[q][1] = i + 1
        self._wait(q, key, self.count[key])
        self.count[key] += 16
        val = self.count[key]
        sem = self.sems[key]
        self.streams[q].append(lambda e, out=out, in_=in_, sem=sem: e.dma_start(out=out, in_=in_).then_inc(sem, 16))
        h = {key: val}
        self._post(reads, writes, h)
        self.n_instr += 1
        return h

    def dma_fn(self, q, fn, reads=(), writes=(), extra=()):
        self._deps(q, reads, writes, extra)
        lst, i = self.dq[q]
        key = lst[i % len(lst)]
        self.dq[q][1] = i + 1
        self._wait(q, key, self.count[key])
        self.count[key] += 16
        val = self.count[key]
        sem = self.sems[key]
        self.streams[q].append(lambda e, fn=fn, sem=sem: fn(e).then_inc(sem, 16))
        h = {key: val}
        self._post(reads, writes, h)
        self.n_instr += 1
        return h

    def barrier(self):
        allh = {k: v for k, v in self.count.items() if v > 0}
        for e in self.ENG:
            for k, v in allh.items():
                if k != e:
                    self._wait(e, k, v)

    def final_wait(self, eng, handles):
        for h in handles:
            for k, v in h.items():
                self._wait(eng, k, v)

    def emit(self, block):
        nc = self.nc
        m = {"pe": block.tensor, "act": block.scalar, "dve": block.vector, "pool": block.gpsimd, "sp": block.sync}
        for name in self.ENG:
            stream = self.streams[name]

            def body(e, stream=stream):
                for f in stream:
                    f(e)
            m[name](body)


class Arena:
    def __init__(self, nc, limit=206 * 1024):
        self.nc = nc
        self.off = 0
        self.limit = limit
        self.base = nc.alloc_sbuf_tensor("arena", [128, limit], mybir.dt.uint8)

    def mark(self):
        return self.off

    def reset(self, m):
        self.off = m

    def alloc(self, shape, dtype, name=None):
        esz = 2 if dtype == BF16 else 4
        nbytes = esz
        for s in shape[1:]:
            nbytes *= s
        off = (self.off + 63) // 64 * 64
        assert off + nbytes <= self.limit, ("SBUF overflow", name, off, nbytes)
        self.off = off + nbytes
        v = self.base[0:shape[0], off:off + nbytes].bitcast(dtype)
        if len(shape) == 3:
            v = v.rearrange("p (a b) -> p a b", a=shape[1])
        elif len(shape) == 4:
            v = v.rearrange("p (a b c) -> p a b c", a=shape[1], b=shape[2])
        return v


def _meta_consts(S):
    nblk = (S * 4) // 512 + NEXP
    p = np.arange(128, dtype=np.float32)[:, None]
    thr16 = np.broadcast_to((512.0 * np.arange(17, dtype=np.float32))[None, :], (128, 17))
    blkthr = np.broadcast_to((512.0 * np.arange(nblk, dtype=np.float32))[None, :], (128, nblk))
    iota_e = np.broadcast_to(np.arange(NEXP, dtype=np.float32)[None, :], (128, NEXP))
    kcp = 128.0 * np.arange(8, dtype=np.float32)[None, :] + p
    tril = np.broadcast_to(np.tril(np.ones((NEXP, NEXP), np.float32)).reshape(1, NEXP * NEXP), (128, NEXP * NEXP))
    return np.ascontiguousarray(np.concatenate([thr16, blkthr, iota_e, kcp, p, tril], axis=1).astype(np.float32)), nblk


def _const_tables(S):
    pos = np.arange(S, dtype=np.float64)
    inv_a = 1.0 / (500000.0 ** (np.arange(8, dtype=np.float64) / 8))
    ang = pos[:, None] * inv_a[None, :]
    ca, sa = np.cos(ang), np.sin(ang)
    inv_r = 1.0 / (10000.0 ** (np.arange(32, dtype=np.float64) / 32))
    angr = pos[:, None] * inv_r[None, :]
    cr, sr = np.cos(angr), np.sin(angr)
    tab = np.concatenate([
        np.concatenate([ca, ca], 1) / 8.0, np.concatenate([-sa, sa], 1) / 8.0,
        np.concatenate([ca, ca], 1), np.concatenate([-sa, sa], 1),
        np.concatenate([cr, cr], 1), np.concatenate([-sr, sr], 1),
    ], axis=1).astype(np.float32)
    h = np.arange(8, dtype=np.float64)
    log_g = np.log1p(-(2.0 ** (-5.0 - h)))
    i = np.arange(128, dtype=np.float64)
    kfac = np.exp(-log_g[None, :] * (i[:, None] + 1.0)) / 8.0
    qdec = np.exp(log_g[None, :] * (i[:, None] + 1.0))
    cd = np.exp(log_g * 128.0)
    cdp = np.zeros((128, 4), np.float64)
    for p in range(4):
        cdp[:64, p] = cd[2 * p]
        cdp[64:, p] = cd[2 * p + 1]
    rtab = np.concatenate([kfac, qdec, cdp], axis=1).astype(np.float32)
    kk = np.arange(128)[:, None]
    qq = np.arange(128)[None, :]
    cm = (kk <= qq).astype(np.float32)
    am = np.zeros((128, NOFF, 128), np.float32)
    for j in range(NOFF):
        o = 16 - j
        dl = 128 * o + qq - kk
        m = ((dl >= 0) & (dl <= 128)).astype(np.float32)
        m += ((dl >= 0) & (dl % 4 == 0) & (dl <= 512)).astype(np.float32)
        m += ((dl >= 0) & (dl % 16 == 0) & (dl <= 2048)).astype(np.float32)
        am[:, j, :] = m
    return dict(
        tab=tab, rtab=rtab, cm=cm.astype(np.float32), am=am.astype(ml_dtypes.bfloat16),
        identb=np.eye(128, dtype=np.float32).astype(ml_dtypes.bfloat16),
        identf=np.eye(128, dtype=np.float32),
        ustrict=(kk < qq).astype(np.float32),
    )


def build_program(NT=64, stop_after="B", debug=False):
    S = NT * 128
    nc = bass.Bass("TRN2", target_bir_lowering=False)

    def din(name, shape, dt=F32):
        return nc.dram_tensor(name, list(shape), dt, kind="ExternalInput").ap()

    x_d = din("x", [S, D])
    mem_d = din("mem", [NMEM, D])
    w_in_d = din("w_in", [D, NIN])
    w_out_d = din("w_out", [D, D])
    wq_d = din("mem_wq", [D, D])
    wk_d = din("mem_wk", [D, D])
    wv_d = din("mem_wv", [D, D])
    wo_d = din("mem_wo", [D, D])
    rw_d = din("router_w", [D, NEXP])
    rb_d = din("router_b", [1, NEXP])
    wg_d = din("w_gate", [NEXP, D, D])
    wu_d = din("w_up", [NEXP, D, D])
    wd_d = din("w_down", [NEXP, D, D])
    bgT_d = din("b_gateT", [128, NEXP, 8])
    buT_d = din("b_upT", [128, NEXP, 8])
    bd_d = din("b_down", [NEXP, D])
    lnp_d = din("lnp", [128, 6, D])
    rng_d = din("rng", [128, 512])
    tab_d = din("tab", [S, 192])
    rtab_d = din("rtab", [128, 20])
    cm_d = din("cm", [128, 128])
    am_d = din("am", [128, NOFF, 128], BF16)
    identb_d = din("identb", [128, 128], BF16)
    identf_d = din("identf", [128, 128])
    ustrict_d = din("ustrict", [128, 128])
    NBLK = (S * 4) // 512 + NEXP
    NSLOT = NBLK * 512
    CMW = 17 + NBLK + NEXP + 8 + 1 + NEXP * NEXP
    cmeta_d = din("cmeta", [128, CMW])
    out_d = nc.dram_tensor("out", [S, D], F32, kind="ExternalOutput").ap()

    def dscr(name, shape, dt):
        if debug:
            return nc.dram_tensor(name, list(shape), dt, kind="ExternalOutput").ap()
        return nc.dram_tensor(name, list(shape), dt).ap()

    QAT = dscr("QAT", [128, 4, S], BF16)
    KAT = dscr("KAT", [128, 4, S], BF16)
    QRT = dscr("QRT", [128, 4, S], BF16)
    KRT = dscr("KRT", [128, 4, S], BF16)
    VA = dscr("VA", [S, 8 * VS], BF16)
    KR = dscr("KR", [S, 512], BF16)
    VR = dscr("VR", [S, 512], BF16)
    GS = dscr("GS", [S, 512], F32)
    X2 = dscr("X2", [S, D], F32)
    X2T = dscr("X2T", [128, 8, S], BF16)
    GT = dscr("GT", [S, NEXP], F32)
    RKD = dscr("RKD", [S, NEXP], F32)
    CNT = dscr("CNT", [128, NEXP], F32)
    X2B = dscr("X2B", [S, D], BF16)
    XS = dscr("XS", [NSLOT, D], BF16)
    YS = dscr("YS", [NSLOT, D], F32)

    dbg = {}
    if debug:
        def dout(name, shape, dt=F32):
            dbg[name] = nc.dram_tensor(name, list(shape), dt, kind="ExternalOutput").ap()
            return dbg[name]

    with ExitStack() as stack:
        sc = Sched(nc, stack)
        ar = Arena(nc)
        banks = [nc.alloc_psum_tensor("bank%d" % i, [128, 512], F32) for i in range(8)]
        bres = [Res("bank%d" % i) for i in range(8)]

        def bank_f32(i):
            return banks[i][:]

        def bank_bf16(i):
            return banks[i][:].bitcast(BF16)

        identb = ar.alloc([128, 128], BF16, "identb")
        identf = ar.alloc([128, 128], F32, "identf")
        r_const = Res("const")
        sc.dma("sp", identb[:], identb_d[:, :], writes=[r_const])
        sc.dma("sp", identf[:], identf_d[:, :], writes=[r_const])
        epsc = ar.alloc([128, 1], F32, "epsc")
        sc.op("pool", lambda e: e.memset(epsc[:], EPS), writes=[r_const])
        base_mark = ar.mark()
        out_handles = []

        w_in = ar.alloc([128, 8, NIN], BF16, "w_in")
        r_w_in = Res("w_in")
        w_in_view = w_in_d.rearrange("(k p) n -> p k n", p=128)
        for c in range(7):
            sc.dma("pool", w_in[:, :, c * 512:(c + 1) * 512], w_in_view[:, :, c * 512:(c + 1) * 512], writes=[r_w_in])
        rtab = ar.alloc([128, 20], F32, "rtab")
        sc.dma("sp", rtab[:], rtab_d[:, :], writes=[r_const])

        NB = 2
        xin = [ar.alloc([128, D], F32, "xin") for _ in range(NB)]
        r_xin = [Res("xin") for _ in range(NB)]
        tab = [ar.alloc([128, 192], F32, "tab") for _ in range(NB)]
        r_tab = [Res("tab") for _ in range(NB)]
        xb = [ar.alloc([128, D], BF16, "xb") for _ in range(NB)]
        r_xb = [Res("xb") for _ in range(NB)]
        xT = [ar.alloc([128, 8, 128], BF16, "xT") for _ in range(NB)]
        r_xT = [Res("xT") for _ in range(NB)]
        qa_b = [ar.alloc([128, 8, 64], BF16, "qa_b") for _ in range(NB)]
        ka_b = [ar.alloc([128, 8, 64], BF16, "ka_b") for _ in range(NB)]
        qr_b = [ar.alloc([128, 8, 64], BF16, "qr_b") for _ in range(NB)]
        kr_b = [ar.alloc([128, 8, 64], BF16, "kr_b") for _ in range(NB)]
        vr_b = [ar.alloc([128, 512], BF16, "vr_b") for _ in range(NB)]
        va_b = [ar.alloc([128, 8, VS], BF16, "va_b") for _ in range(NB)]
        gs_f = [ar.alloc([128, 512], F32, "gs_f") for _ in range(NB)]
        r_qa = [Res() for _ in range(NB)]
        r_ka = [Res() for _ in range(NB)]
        r_qr = [Res() for _ in range(NB)]
        r_kr = [Res() for _ in range(NB)]
        r_vr = [Res() for _ in range(NB)]
        r_va = [Res() for _ in range(NB)]
        r_gs = [Res() for _ in range(NB)]
        qaT = [ar.alloc([128, 4, 128], BF16, "qaT") for _ in range(NB)]
        kaT = [ar.alloc([128, 4, 128], BF16, "kaT") for _ in range(NB)]
        qrT = [ar.alloc([128, 4, 128], BF16, "qrT") for _ in range(NB)]
        krT = [ar.alloc([128, 4, 128], BF16, "krT") for _ in range(NB)]
        r_qaT = [Res() for _ in range(NB)]
        r_kaT = [Res() for _ in range(NB)]
        r_qrT = [Res() for _ in range(NB)]
        r_krT = [Res() for _ in range(NB)]
        rt_a = ar.alloc([128, 8, 64], F32, "rt_a")
        rt_b = ar.alloc([128, 8, 64], F32, "rt_b")
        r_rt = Res("rt")
        r_rtb = Res("rtb")
        for b in range(NB):
            sc.op("pool", lambda e, b=b: e.memset(va_b[b][:], 1.0), writes=[r_va[b]])

        def bc_heads(ap2d, n):
            return ap2d.unsqueeze(1).to_broadcast([128, 8, n])

        def rotary(pb, r_pb, dst, r_dst, tb, r_tb, c_off, n, post_scale=None):
            hn = n // 2
            p3 = pb.rearrange("p (h d) -> p h d", h=8)
            cc = bc_heads(tb[:, c_off:c_off + n], n)
            s1 = bc_heads(tb[:, c_off + n:c_off + n + hn], hn)
            s2 = bc_heads(tb[:, c_off + n + hn:c_off + 2 * n], hn)
            ta = rt_a[:, :, 0:n]
            tb_ = rt_b[:, :, 0:n]
            sc.op("dve", lambda e: e.tensor_tensor(out=ta, in0=p3[:, :, 0:n], in1=cc, op=ALU.mult),
                  reads=[r_tb, r_pb], writes=[r_rt])
            sc.op("dve", lambda e: e.tensor_tensor(out=rt_b[:, :, 0:hn], in0=p3[:, :, hn:n], in1=s1, op=ALU.mult),
                  reads=[r_tb, r_pb], writes=[r_rtb])
            sc.op("dve", lambda e: e.tensor_tensor(out=rt_b[:, :, hn:n], in0=p3[:, :, 0:hn], in1=s2, op=ALU.mult),
                  reads=[r_tb, r_pb, r_rtb], writes=[r_rtb])
            if post_scale is None:
                sc.op("dve", lambda e: e.tensor_tensor(out=dst[:, :, 0:n], in0=ta, in1=tb_, op=ALU.add),
                      reads=[r_rt, r_rtb], writes=[r_dst])
            else:
                sc.op("dve", lambda e: e.tensor_tensor(out=ta, in0=ta, in1=tb_, op=ALU.add),
                      reads=[r_rt, r_rtb], writes=[r_rt])
                ps = post_scale.unsqueeze(2).to_broadcast([128, 8, n])
                sc.op("dve", lambda e: e.tensor_tensor(out=dst[:, :, 0:n], in0=ta, in1=ps, op=ALU.mult),
                      reads=[r_rt, r_const], writes=[r_dst])

        import os
        CUT = int(os.environ.get('KCUT', '99'))
        def a1_post(t):
            b = t % NB
            tok = slice(t * 128, (t + 1) * 128)
            for (srcs, bk) in ((((qa_b, r_qa, qaT, r_qaT), (ka_b, r_ka, kaT, r_kaT)), 5), (((qr_b, r_qr, qrT, r_qrT), (kr_b, r_kr, krT, r_krT)), 6)):
                pv = bank_bf16(bk).rearrange("p (a c t) -> p a c t", a=2, c=4)
                fns = []
                rd = [r_const]
                for ai, (src, rsrc, dstT, rdst) in enumerate(srcs):
                    sflat = src[b][:].rearrange("p h d -> p (h d)")
                    rd.append(rsrc[b])
                    for c4 in range(4):
                        fns.append(lambda e, sflat=sflat, ai=ai, c4=c4, pv=pv: e.transpose(out=pv[:, ai, c4, :], in_=sflat[:, c4 * 128:(c4 + 1) * 128], identity=identb[:]))
                sc.ops("pe", fns, reads=rd, writes=[bres[bk]])
                if os.environ.get('KNOCOPY'):
                    continue
                for ai, (src, rsrc, dstT, rdst) in enumerate(srcs):
                    eng = "act" if bk == 5 else "dve"
                    if os.environ.get('KFORCE'):
                        eng = os.environ.get('KFORCE')
                    if os.environ.get('KONLY') and os.environ.get('KONLY') != eng:
                        continue
                    if eng == "act":
                        sc.op("act", lambda e, dstT=dstT, ai=ai, pv=pv, b=b: e.activation(out=dstT[b][:], in_=pv[:, ai, :, :], func=AF.Copy),
                              reads=[bres[bk]], writes=[rdst[b]])
                    else:
                        sc.op("dve", lambda e, dstT=dstT, ai=ai, pv=pv, b=b: e.tensor_copy(out=dstT[b][:], in_=pv[:, ai, :, :]),
                              reads=[bres[bk]], writes=[rdst[b]])
            sc.dma("sp", QAT[:, :, tok], qaT[b][:], reads=[r_qaT[b]])
            sc.dma("sp", KAT[:, :, tok], kaT[b][:], reads=[r_kaT[b]])
            sc.dma("sp", QRT[:, :, tok], qrT[b][:], reads=[r_qrT[b]])
            sc.dma("sp", KRT[:, :, tok], krT[b][:], reads=[r_krT[b]])
            sc.dma("sp", VA[tok, :], va_b[b][:].rearrange("p h d -> p (h d)"), reads=[r_va[b]])
            sc.dma("sp", KR[tok, :], kr_b[b][:].rearrange("p h d -> p (h d)"), reads=[r_kr[b]])
            sc.dma("sp", VR[tok, :], vr_b[b][:], reads=[r_vr[b]])
            sc.dma("sp", GS[tok, :], gs_f[b][:], reads=[r_gs[b]])

        zt = ar.alloc([128, 4, D], BF16, "zt"); r_zt = Res()
        sc.op("pool", lambda e: e.memset(zt[:], 0.0), writes=[r_zt])
        ZPT = -(-NBLK // NT)
        for t in range(NT if CUT > 0 else 0):
            b = t % NB
            tok = slice(t * 128, (t + 1) * 128)
            sc.dma("sp", xin[b][:], x_d[tok, :], writes=[r_xin[b]])
            sc.dma("sp", tab[b][:], tab_d[tok, :], writes=[r_tab[b]])
            if stop_after == "B" and SPARSE:
                for zb in range(t * ZPT, min(NBLK, (t + 1) * ZPT)):
                    sc.dma("sp", XS[zb * 512:(zb + 1) * 512, :].rearrange("(i p) d -> p i d", p=128), zt[:], reads=[r_zt])
            sc.op("act", lambda e, b=b: e.activation(out=xb[b][:], in_=xin[b][:], func=AF.Copy), reads=[r_xin[b]], writes=[r_xb[b]])
            if CUT < 2:
                continue
            pT = bank_bf16(0).rearrange("p (k t) -> p k t", k=8)
            sc.ops("pe", [lambda e, b=b, k=k: e.transpose(out=pT[:, k, :], in_=xb[b][:, k * 128:(k + 1) * 128], identity=identb[:])
                          for k in range(8)], reads=[r_xb[b], r_const], writes=[bres[0]])
            sc.op("act", lambda e, b=b: e.activation(out=xT[b][:], in_=pT, func=AF.Copy), reads=[bres[0]], writes=[r_xT[b]])
            if CUT < 3:
                continue
            for c in range(7 if CUT > 3 else 0):
                bk = 1 + (c % 4)
                pb = bank_f32(bk)
                sc.ops("pe", [lambda e, b=b, k=k, c=c, pb=pb: e.matmul(pb, lhsT=xT[b][:, k, :], rhs=w_in[:, k, c * 512:(c + 1) * 512],
                                                                    start=(k == 0), stop=(k == 7)) for k in range(8)],
                       reads=[r_xT[b], r_w_in], writes=[bres[bk]])
                p3 = pb.rearrange("p (h d) -> p h d", h=8)
                if c == 0:
                    rotary(pb, bres[bk], qa_b[b], r_qa[b], tab[b], r_tab[b], 0, 16)
                    sc.op("act", lambda e, b=b, p3=p3: e.activation(out=qa_b[b][:, :, 16:64], in_=p3[:, :, 16:64], func=AF.Copy, scale=0.125),
                          reads=[bres[bk]], writes=[r_qa[b]])
                elif c == 1:
                    rotary(pb, bres[bk], ka_b[b], r_ka[b], tab[b], r_tab[b], 32, 16)
                    sc.op("act", lambda e, b=b, p3=p3: e.activation(out=ka_b[b][:, :, 16:64], in_=p3[:, :, 16:64], func=AF.Copy),
                          reads=[bres[bk]], writes=[r_ka[b]])
                elif c == 2:
                    sc.op("act", lambda e, b=b, p3=p3: e.activation(out=va_b[b][:, :, 0:64], in_=p3, func=AF.Copy),
                          reads=[bres[bk]], writes=[r_va[b]])
                elif c == 3:
                    rotary(pb, bres[bk], qr_b[b], r_qr[b], tab[b], r_tab[b], 64, 64)
                elif c == 4:
                    rotary(pb, bres[bk], kr_b[b], r_kr[b], tab[b], r_tab[b], 64, 64, post_scale=rtab[:, 0:8])
                elif c == 5:
                    sc.op("act", lambda e, b=b, pb=pb: e.activation(out=vr_b[b][:], in_=pb, func=AF.Copy),
                          reads=[bres[bk]], writes=[r_vr[b]])
                else:
                    sc.op("act", lambda e, b=b, pb=pb: e.activation(out=gs_f[b][:], in_=pb, func=AF.Silu),
                          reads=[bres[bk]], writes=[r_gs[b]])
            if t > 0:
                a1_post(t - 1)

        a1_post(NT - 1)
        sc.barrier()
        def mm(out, lhsT, rhs, start=True, stop=True):
            return lambda e: e.matmul(out, lhsT=lhsT, rhs=rhs, start=start, stop=stop)

        def tr(out, in_, ident):
            return lambda e: e.transpose(out=out, in_=in_, identity=ident)

        def actf(out, in_, func, **kw):
            return lambda e: e.activation(out=out, in_=in_, func=func, **kw)

        def tt(out, a, b_, op):
            return lambda e: e.tensor_tensor(out=out, in0=a, in1=b_, op=op)

        def ts(out, a, s1, s2, op0, op1=None):
            if op1 is None:
                return lambda e: e.tensor_scalar(out=out, in0=a, scalar1=s1, scalar2=None, op0=op0)
            return lambda e: e.tensor_scalar(out=out, in0=a, scalar1=s1, scalar2=s2, op0=op0, op1=op1)

        def stt(out, a, s, b_, op0, op1):
            return lambda e: e.scalar_tensor_tensor(out=out, in0=a, scalar=s, in1=b_, op0=op0, op1=op1)

        def cp(out, in_):
            return lambda e: e.tensor_copy(out=out, in_=in_)

        def red(out, in_, op=ALU.add):
            return lambda e: e.tensor_reduce(out=out, in_=in_, axis=AX.X, op=op)

        def layer_norm(src, r_src, dst, r_dst, g_ap, b_ap, tmp):
            st6, mv, rstd, nmr = tmp
            r_t = Res()
            sc.ops("dve", [lambda e: e.bn_stats(out=st6[:, 0:6], in_=src[:, 0:512]),
                           lambda e: e.bn_stats(out=st6[:, 6:12], in_=src[:, 512:1024])], reads=[r_src], writes=[r_t])
            sc.op("dve", lambda e: e.bn_aggr(out=mv[:, 0:2], in_=st6[:, 0:12]), reads=[r_t], writes=[r_t])
            sc.op("act", actf(rstd[:, 0:1], mv[:, 1:2], AF.Ln, bias=epsc[:, 0:1]), reads=[r_t, r_const], writes=[r_t])
            sc.op("act", actf(rstd[:, 0:1], rstd[:, 0:1], AF.Exp, scale=-0.5), reads=[r_t], writes=[r_t])
            sc.op("dve", stt(nmr[:, 0:1], mv[:, 0:1], -1.0, rstd[:, 0:1], ALU.mult, ALU.mult), reads=[r_t], writes=[r_t])
            sc.op("act", actf(dst, src, AF.Identity, bias=nmr[:, 0:1], scale=rstd[:, 0:1]), reads=[r_src, r_t], writes=[r_dst])
            sc.op("dve", tt(dst, dst, g_ap, ALU.mult), reads=[r_dst, r_const], writes=[r_dst])
            sc.op("dve", tt(dst, dst, b_ap, ALU.add), reads=[r_dst, r_const], writes=[r_dst])

        if stop_after != "A1":
            ar.reset(base_mark)
            w_out_b = ar.alloc([128, 8, D], BF16, "w_out_b")
            wq_b = ar.alloc([128, 8, D], BF16, "wq_b")
            wo_b = ar.alloc([128, 8, D], BF16, "wo_b")
            kmT = ar.alloc([128, 8, NMEM], BF16, "kmT")
            vm_b = ar.alloc([128, 2, D], BF16, "vm_b")
            lnp = ar.alloc([128, 4, D], F32, "lnp")
            rng = ar.alloc([128, 512], F32, "rng")
            am = ar.alloc([128, NOFF, 128], BF16, "am")
            cm = ar.alloc([128, 128], F32, "cm")
            rtab2 = ar.alloc([128, 20], F32, "rtab2")
            rw_f = ar.alloc([128, 8, NEXP], F32, "rw_f")
            rb_f = ar.alloc([1, NEXP], F32, "rb_f")
            ones_f = ar.alloc([128, 128], F32, "ones_f")
            ustr = ar.alloc([128, 128], F32, "ustr")
            rbase = ar.alloc([128, NEXP], F32, "rbase"); r_rbase = Res()
            rkt = [ar.alloc([128, NEXP], F32, "rkt") for _ in range(2)]; r_rkt = [Res(), Res()]
            ones_b = ar.alloc([128, 8], BF16, "ones_b")
            r_w2 = Res("w2")
            for (dst, src) in ((w_out_b, w_out_d), (wq_b, wq_d), (wo_b, wo_d)):
                v = src.rearrange("(k p) n -> p k n", p=128)
                for hh in range(2):
                    sc.dma("pool", dst[:, :, hh * 512:(hh + 1) * 512], v[:, :, hh * 512:(hh + 1) * 512], writes=[r_w2])
            sc.dma("sp", lnp[:], lnp_d[:, 0:4, :], writes=[r_const])
            sc.dma("sp", rng[:], rng_d[:, :], writes=[r_const])
            sc.dma("sp", am[:], am_d[:, :, :], writes=[r_const])
            sc.dma("sp", cm[:], cm_d[:, :], writes=[r_const])
            sc.dma("sp", rtab2[:], rtab_d[:, :], writes=[r_const])
            sc.dma("sp", rw_f[:], rw_d.rearrange("(k p) n -> p k n", p=128), writes=[r_const])
            sc.dma("sp", rb_f[:], rb_d[:, :], writes=[r_const])
            sc.op("pool", lambda e: e.memset(ones_f[:], 1.0), writes=[r_const])
            sc.op("pool", lambda e: e.memset(rbase[:], 0.0), writes=[r_rbase])
            sc.dma("sp", ustr[:], ustrict_d[:, :], writes=[r_const])
            sc.op("pool", lambda e: e.memset(ones_b[:], 1.0), writes=[r_const])
            m2 = ar.mark()
            wk_b = ar.alloc([128, 8, D], BF16, "wk_b")
            wv_b = ar.alloc([128, 8, D], BF16, "wv_b")
            memf = ar.alloc([128, 2, D], F32, "memf")
            memb = ar.alloc([128, 2, D], BF16, "memb")
            memT = ar.alloc([128, 8, NMEM], BF16, "memT")
            r_wkv = Res()
            r_memf = Res(); r_memb = Res(); r_memT = Res(); r_km = Res(); r_vm = Res()
            for (dst, src) in ((wk_b, wk_d), (wv_b, wv_d)):
                v = src.rearrange("(k p) n -> p k n", p=128)
                for hh in range(2):
                    sc.dma("pool", dst[:, :, hh * 512:(hh + 1) * 512], v[:, :, hh * 512:(hh + 1) * 512], writes=[r_wkv])
            sc.dma("sp", memf[:], mem_d.rearrange("(c p) d -> p c d", p=128), writes=[r_memf])
            sc.op("pool", cp(memb[:], memf[:]), reads=[r_memf], writes=[r_memb])
            for mc in range(2):
                pTm = bank_bf16(mc).rearrange("p (k t) -> p k t", k=8)
                sc.ops("pe", [tr(pTm[:, k, :], memb[:, mc, k * 128:(k + 1) * 128], identb[:]) for k in range(8)],
                       reads=[r_memb, r_const], writes=[bres[mc]])
                sc.op("act", actf(memT[:, :, mc * 128:(mc + 1) * 128], pTm, AF.Copy), reads=[bres[mc]], writes=[r_memT])
            for fc in range(8):
                bk = 2 + fc % 2
                pk = bank_f32(bk)[:, 0:NMEM]
                sc.ops("pe", [mm(pk, wk_b[:, k, fc * 128:(fc + 1) * 128], memT[:, k, :], k == 0, k == 7) for k in range(8)],
                       reads=[r_wkv, r_memT], writes=[bres[bk]])
                sc.op("act", actf(kmT[:, fc, :], pk, AF.Copy), reads=[bres[bk]], writes=[r_km])
            for mc in range(2):
                for hh in range(2):
                    bk = 4 + (mc * 2 + hh) % 2
                    pvv = bank_f32(bk)
                    sc.ops("pe", [mm(pvv, memT[:, k, mc * 128:(mc + 1) * 128], wv_b[:, k, hh * 512:(hh + 1) * 512], k == 0, k == 7) for k in range(8)],
                           reads=[r_wkv, r_memT], writes=[bres[bk]])
                    sc.op("act", actf(vm_b[:, mc, hh * 512:(hh + 1) * 512], pvv, AF.Copy), reads=[bres[bk]], writes=[r_vm])
            sc.barrier()
            ar.reset(m2)
            R = 18
            kring = ar.alloc([128, 4, R * 128], BF16, "kring")
            vring = ar.alloc([128, R, 8 * VS], BF16, "vring")
            r_kring = [Res() for _ in range(R)]
            r_vring = [Res() for _ in range(R)]
            NB2 = 2
            def dbl(shape, dt, name):
                return [ar.alloc(shape, dt, name) for _ in range(NB2)], [Res(name) for _ in range(NB2)]
            qaT2, r_qaT2 = dbl([128, 4, 128], BF16, "qaT2")
            qrT2, r_qrT2 = dbl([128, 4, 128], BF16, "qrT2")
            krT2, r_krT2 = dbl([128, 4, 128], BF16, "krT2")
            kr2, r_kr2 = dbl([128, 512], BF16, "kr2")
            vr2, r_vr2 = dbl([128, 512], BF16, "vr2")
            gs2, r_gs2 = dbl([128, 512], F32, "gs2")
            xin2, r_xin2 = dbl([128, D], F32, "xin2")
            NSB = 3
            SBK = [0, 1, 4]
            Et = [ar.alloc([128, 512], BF16, "Et") for _ in range(NSB)]; r_Et = [Res() for _ in range(NSB)]
            Pt = [ar.alloc([128, 512], BF16, "Pt") for _ in range(NSB)]; r_Pt = [Res() for _ in range(NSB)]
            rec8 = ar.alloc([128, 8], F32, "rec8"); r_rec8 = Res()
            mixed_b = ar.alloc([128, D], BF16, "mixed_b"); r_mixa = Res(); r_mixr = Res()
            mixedT = ar.alloc([128, 8, 128], BF16, "mixedT"); r_mixedT = Res()
            Pr = ar.alloc([128, 8, 128], BF16, "Pr"); r_Pr = Res()
            state_f = ar.alloc([128, 4, 128], F32, "state_f"); r_state_f = Res()
            state_b = ar.alloc([128, 4, 128], BF16, "state_b"); r_state_b = Res()
            yv = ar.alloc([128, 8, 64], F32, "yv"); r_yv = Res()
            ysq = ar.alloc([128, 8, 64], F32, "ysq"); r_ysq = Res()
            st8 = ar.alloc([128, 32], F32, "st8"); r_st8 = Res()
            r1 = ar.alloc([128, D], F32, "r1"); r_r1 = Res()
            x1 = ar.alloc([128, D], F32, "x1"); r_x1 = Res()
            x1_b = ar.alloc([128, D], BF16, "x1_b"); r_x1b = Res()
            x1T = ar.alloc([128, 8, 128], BF16, "x1T"); r_x1T = Res()
            qcT = ar.alloc([128, 8, 128], BF16, "qcT"); r_qcT = Res()
            ET = ar.alloc([128, 8, 128], BF16, "ET"); r_ET = Res()
            oc_b = ar.alloc([128, D], BF16, "oc_b"); r_ocb = Res()
            ocT = ar.alloc([128, 8, 128], BF16, "ocT"); r_ocT = Res()
            rec4 = ar.alloc([128, 4], F32, "rec4"); r_rec4 = Res()
            r2 = r1; r_r2 = r_r1
            x2s, r_x2s = dbl([128, D], F32, "x2s")
            x2_b = ar.alloc([128, D], BF16, "x2_b"); r_x2b = Res()
            x2T_b, r_x2Tb = dbl([128, 8, 128], BF16, "x2T_b")
            x2T_f = ar.alloc([128, 8, 128], F32, "x2T_f"); r_x2Tf = Res()
            lg = ar.alloc([128, NEXP], F32, "lg"); r_lg = Res()
            mx8 = ar.alloc([128, 8], F32, "mx8")
            msk = ar.alloc([128, NEXP], F32, "msk")
            ex = ar.alloc([128, NEXP], F32, "ex")
            sm = ar.alloc([128, 4], F32, "sm")
            gat, r_gat = dbl([128, NEXP], F32, "gat")
            lntmp = (ar.alloc([128, 12], F32, "st6"), ar.alloc([128, 2], F32, "mv"), ar.alloc([128, 1], F32, "rstd"), ar.alloc([128, 1], F32, "nmr"))
            sc.op("pool", lambda e: e.memset(state_f[:], 0.0), writes=[r_state_f])
            sc.op("pool", lambda e: e.memset(state_b[:], 0.0), writes=[r_state_b])
            kfac_t = rtab2[:, 0:8]; qdec_t = rtab2[:, 8:16]; cdp_t = rtab2[:, 16:20]
            r_rt2 = Res()
            print("A2 arena top", ar.off, "kring off", kring.offset, "qaT2", qaT2[0].offset, qaT2[1].offset)

            CUT2 = int(os.environ.get('KCUT2', '99'))
            KATT = int(os.environ.get('KATT', '99'))
            KRET = int(os.environ.get('KRET', '99'))
            def a2_loads(t):
                b = t % NB2
                tok = slice(t * 128, (t + 1) * 128)
                slot = t % R
                sc.dma("sp", qaT2[b][:], QAT[:, :, tok], writes=[r_qaT2[b]])
                sc.dma("sp", kring[:, :, slot * 128:(slot + 1) * 128], KAT[:, :, tok], writes=[r_kring[slot]])
                sc.dma("sp", vring[:, slot, :], VA[tok, :], writes=[r_vring[slot]])
                sc.dma("sp", qrT2[b][:], QRT[:, :, tok], writes=[r_qrT2[b]])
                sc.dma("sp", krT2[b][:], KRT[:, :, tok], writes=[r_krT2[b]])
                sc.dma("sp", kr2[b][:], KR[tok, :], writes=[r_kr2[b]])
                sc.dma("sp", vr2[b][:], VR[tok, :], writes=[r_vr2[b]])
                sc.dma("sp", gs2[b][:], GS[tok, :], writes=[r_gs2[b]])
                sc.dma("sp", xin2[b][:], x_d[tok, :], writes=[r_xin2[b]])

            def a2_att_thunks(t):
                b = t % NB2
                tok = slice(t * 128, (t + 1) * 128)
                slot = t % R
                kbs = list(range(max(0, t - 16), t + 1))
                steps = []
                for h in range(0, 8, 2 if os.environ.get('KEVEN') else 1):
                    groups = [kbs[i:i + 4] for i in range(0, len(kbs), 4)]
                    for gi, g in enumerate(groups):
                        steps.append((h, g, gi == 0, gi == len(groups) - 1))
                Ob = [bank_f32(2)[:, 0:4 * VS].rearrange("p (h d) -> p h d", h=4), bank_f32(3)[:, 0:4 * VS].rearrange("p (h d) -> p h d", h=4)]

                def stageA(i, st):
                    h, g, first, last = st
                    si = i % NSB
                    sb = SBK[si]
                    p, hb = h // 2, 64 * (h % 2)
                    n = len(g)
                    Sb = bank_f32(sb)
                    if os.environ.get('KFULLK') == '2':
                        fns = [mm(Sb[:, j * 128:(j + 1) * 128], kmT[:, 0, 0:128], kmT[:, 1, 0:128])
                               for j, kb in enumerate(g)]
                    elif os.environ.get('KFULLK') == '3':
                        fns = [mm(Sb[:, j * 128:(j + 1) * 128], kmT[:, 0, 0:128], qaT2[b][:, p, :])
                               for j, kb in enumerate(g)]
                    elif os.environ.get('KFULLK'):
                        fns = [mm(Sb[:, j * 128:(j + 1) * 128], kring[:, p, (kb % R) * 128:(kb % R + 1) * 128], qaT2[b][:, p, :])
                               for j, kb in enumerate(g)]
                    else:
                        fns = [mm(Sb[:, j * 128:(j + 1) * 128], kring[hb:hb + 64, p, (kb % R) * 128:(kb % R + 1) * 128], qaT2[b][hb:hb + 64, p, :])
                               for j, kb in enumerate(g)]
                    if os.environ.get('KNOREADS'):
                        sc.ops("pe", fns, reads=[], writes=[bres[sb]])
                    else:
                        sc.ops("pe", fns, reads=[r_qaT2[b]] + [r_kring[kb % R] for kb in g], writes=[bres[sb]])
                    if KATT < 2:
                        return
                    sc.op("act", actf(Et[si][:, 0:n * 128], Sb[:, 0:n * 128], AF.Exp), reads=[bres[sb]], writes=[r_Et[si]])
                    if KATT < 3:
                        return
                    j0 = g[0] - t + 16
                    msk_ap = am[:, j0:j0 + n, :].rearrange("p j q -> p (j q)")
                    eng = "dve"
                    sc.op(eng, tt(Pt[si][:, 0:n * 128], Et[si][:, 0:n * 128], msk_ap, ALU.mult), reads=[r_Et[si], r_const], writes=[r_Pt[si]])

                def stageB(i, st):
                    if KATT < 4:
                        return
                    h, g, first, last = st
                    si = i % NSB
                    fns = []
                    for j, kb in enumerate(g):
                        fns.append(mm(Ob[h // 4][:, h % 4, :], Pt[si][:, j * 128:(j + 1) * 128], vring[:, kb % R, h * VS:(h + 1) * VS],
                                      first and j == 0, last and j == len(g) - 1))
                    sc.ops("pe", fns, reads=[r_Pt[si]] + [r_vring[kb % R] for kb in g], writes=[bres[2 + h // 4]])

                def att_final():
                    for i_ in range(max(0, len(steps) - (NSB - 1)), len(steps)):
                        stageB(i_, steps[i_])
                    mix3 = mixed_b[:, 0:512].rearrange("p (h d) -> p h d", h=8)
                    for hh in range(2):
                        sc.op("dve", lambda e, hh=hh: e.reciprocal(out=rec8[:, hh * 4:(hh + 1) * 4], in_=Ob[hh][:, :, 64]), reads=[bres[2 + hh]], writes=[r_rec8])
                        sc.op("dve", tt(mix3[:, hh * 4:(hh + 1) * 4, :], Ob[hh][:, :, 0:64], rec8[:, hh * 4:(hh + 1) * 4].unsqueeze(2).to_broadcast([128, 4, 64]), ALU.mult),
                              reads=[bres[2 + hh], r_rec8], writes=[r_mixa])

                thunks = []
                for i, st in enumerate(steps):
                    def th(i=i, st=st):
                        stageA(i, st)
                        if i >= NSB - 1:
                            stageB(i - (NSB - 1), steps[i - (NSB - 1)])
                    thunks.append(th)
                thunks.append(att_final)
                return thunks

            def a2_ret(t):
                b = t % NB2
                tok = slice(t * 128, (t + 1) * 128)
                slot = t % R
                Sr = [bank_f32(4).rearrange("p (h q) -> p h q", h=4), bank_f32(5).rearrange("p (h q) -> p h q", h=4)]
                for hh in range(2):
                    fns = []
                    for hl in range(4):
                        h = hh * 4 + hl
                        p, hb = h // 2, 64 * (h % 2)
                        fns.append(mm(Sr[hh][:, hl, :], krT2[b][hb:hb + 64, p, :], qrT2[b][hb:hb + 64, p, :]))
                        fns.append(mm(bank_f32(1)[:, 0:128], identb[:], identb[:]))
                    sc.ops("pe", fns, reads=[r_krT2[b], r_qrT2[b], r_const], writes=[bres[4 + hh], bres[1]])
                    sc.op("dve", tt(Pr[:, hh * 4:(hh + 1) * 4, :], Sr[hh], cm[:, :].unsqueeze(1).to_broadcast([128, 4, 128]), ALU.mult),
                          reads=[bres[4 + hh], r_const], writes=[r_Pr])
                Rb = bank_f32(6).rearrange("p (h d) -> p h d", h=8)
                Cb = bank_f32(0).rearrange("p (h d) -> p h d", h=8)
                fns = []
                for h in range(8):
                    fns.append(mm(Rb[:, h, :], Pr[:, h, :], vr2[b][:, h * 64:(h + 1) * 64], True, True))
                sc.ops("pe", fns, reads=[r_Pr, r_vr2[b]], writes=[bres[6]])
                fns = []
                for h in range(8):
                    p, hb = h // 2, 64 * (h % 2)
                    fns.append(mm(Cb[:, h, :], qrT2[b][hb:hb + 64, p, :], state_b[hb:hb + 64, p, hb:hb + 64], True, True))
                    fns.append(mm(bank_f32(1)[:, 0:128], identb[:], identb[:]))
                sc.ops("pe", fns, reads=[r_qrT2[b], r_state_b, r_const], writes=[bres[0], bres[1]])
                KVb = bank_f32(7).rearrange("p (a c) -> p a c", a=4)
                sc.ops("pe", [mm(KVb[:, p, :], kr2[b][:, p * 128:(p + 1) * 128], vr2[b][:, p * 128:(p + 1) * 128]) for p in range(4)],
                       reads=[r_kr2[b], r_vr2[b]], writes=[bres[7]])
                sc.op("dve", tt(state_f[:], state_f[:], KVb, ALU.add), reads=[bres[7], r_state_f], writes=[r_state_f])
                sc.op("dve", tt(state_f[:], state_f[:], cdp_t.unsqueeze(2).to_broadcast([128, 4, 128]), ALU.mult), reads=[r_state_f, r_const], writes=[r_state_f])
                sc.op("act", actf(state_b[:], state_f[:], AF.Copy), reads=[r_state_f], writes=[r_state_b])
                sc.op("dve", tt(yv[:], Rb, qdec_t.unsqueeze(2).to_broadcast([128, 8, 64]), ALU.mult), reads=[bres[6], r_const], writes=[r_yv])
                sc.op("dve", tt(ysq[:], Cb, qdec_t.unsqueeze(2).to_broadcast([128, 8, 64]), ALU.mult), reads=[bres[0], r_const], writes=[r_ysq])
                sc.op("dve", tt(yv[:], yv[:], ysq[:], ALU.add), reads=[r_yv, r_ysq], writes=[r_yv])
                sc.op("dve", red(st8[:, 0:8], yv[:]), reads=[r_yv], writes=[r_st8])
                sc.op("dve", ts(st8[:, 8:16], st8[:, 0:8], 1.0 / 64.0, None, ALU.mult), reads=[r_st8], writes=[r_st8])
                sc.op("dve", tt(yv[:], yv[:], st8[:, 8:16].unsqueeze(2).to_broadcast([128, 8, 64]), ALU.subtract), reads=[r_yv, r_st8], writes=[r_yv])
                sc.op("dve", tt(ysq[:], yv[:], yv[:], ALU.mult), reads=[r_yv], writes=[r_ysq])
                sc.op("dve", red(st8[:, 16:24], ysq[:]), reads=[r_ysq], writes=[r_st8])
                sc.op("act", actf(st8[:, 24:32], st8[:, 16:24], AF.Ln, bias=epsc[:, 0:1], scale=1.0 / 64.0), reads=[r_st8, r_const], writes=[r_st8])
                sc.op("act", actf(st8[:, 24:32], st8[:, 24:32], AF.Exp, scale=-0.5), reads=[r_st8], writes=[r_st8])
                sc.op("dve", tt(yv[:], yv[:], st8[:, 24:32].unsqueeze(2).to_broadcast([128, 8, 64]), ALU.mult), reads=[r_yv, r_st8], writes=[r_yv])
                yflat = yv[:].rearrange("p h d -> p (h d)")
                sc.op("dve", tt(yflat, yflat, rng[:], ALU.mult), reads=[r_yv, r_const], writes=[r_yv])
                sc.op("dve", tt(mixed_b[:, 512:1024], yflat, gs2[b][:], ALU.mult), reads=[r_yv, r_gs2[b]], writes=[r_mixr])

            def a2_tail(t):
                b = t % NB2
                tok = slice(t * 128, (t + 1) * 128)
                slot = t % R
                pTm = bank_bf16(7).rearrange("p (k t) -> p k t", k=8)
                sc.ops("pe", [tr(pTm[:, k, :], mixed_b[:, k * 128:(k + 1) * 128], identb[:]) for k in range(8)],
                       reads=[r_mixa, r_mixr, r_const], writes=[bres[7]])
                sc.op("act", actf(mixedT[:], pTm, AF.Copy), reads=[bres[7]], writes=[r_mixedT])
                yield
                for hh in range(2):
                    pb = bank_f32(5 + hh)
                    sc.ops("pe", [mm(pb, mixedT[:, k, :], w_out_b[:, k, hh * 512:(hh + 1) * 512], k == 0, k == 7) for k in range(8)],
                           reads=[r_mixedT, r_w2], writes=[bres[5 + hh]])
                    sc.op("dve", stt(r1[:, hh * 512:(hh + 1) * 512], xin2[b][:, hh * 512:(hh + 1) * 512], ALPHA, pb, ALU.mult, ALU.add),
                          reads=[bres[5 + hh], r_xin2[b]], writes=[r_r1])
                    yield
                layer_norm(r1[:], r_r1, x1[:], r_x1, lnp[:, 0, :], lnp[:, 1, :], lntmp)
                yield
                sc.op("act", actf(x1_b[:], x1[:], AF.Copy), reads=[r_x1], writes=[r_x1b])
                yield
                pTm = bank_bf16(7).rearrange("p (k t) -> p k t", k=8)
                sc.ops("pe", [tr(pTm[:, k, :], x1_b[:, k * 128:(k + 1) * 128], identb[:]) for k in range(8)], reads=[r_x1b, r_const], writes=[bres[7]])
                sc.op("act", actf(x1T[:], pTm, AF.Copy), reads=[bres[7]], writes=[r_x1T])
                yield
                for hh in range(2):
                    Qb = bank_f32([7, 5][hh]).rearrange("p (c t) -> p c t", c=4)
                    fns = []
                    for fl in range(4):
                        fc = hh * 4 + fl
                        fns += [mm(Qb[:, fl, :], wq_b[:, k, fc * 128:(fc + 1) * 128], x1T[:, k, :], k == 0, k == 7) for k in range(8)]
                    sc.ops("pe", fns, reads=[r_x1T, r_w2], writes=[bres[[7, 5][hh]]])
                    sc.op("act", actf(qcT[:, hh * 4:(hh + 1) * 4, :], Qb, AF.Copy, scale=1.0 / 16.0), reads=[bres[[7, 5][hh]]], writes=[r_qcT])
                    yield
                for hh in range(2):
                    Scb = bank_f32(6 + hh).rearrange("p (c t) -> p c t", c=4)
                    fns = []
                    for il in range(4):
                        idx = hh * 4 + il
                        hm, mc = idx // 2, idx % 2
                        for c in range(2):
                            fns.append(mm(Scb[:, il, :], kmT[:, 2 * hm + c, mc * 128:(mc + 1) * 128], qcT[:, 2 * hm + c, :], c == 0, c == 1))
                    sc.ops("pe", fns, reads=[r_qcT, r_km], writes=[bres[6 + hh]])
                    sc.op("act", actf(ET[:, hh * 4:(hh + 1) * 4, :], Scb, AF.Exp), reads=[bres[6 + hh]], writes=[r_ET])
                    yield
                denb = bank_f32(6)[:, 0:32].rearrange("p (h c) -> p h c", h=4)
                fns = []
                for hm in range(4):
                    for mc in range(2):
                        fns.append(mm(denb[:, hm, :], ET[:, hm * 2 + mc, :], ones_b[:, 0:8], mc == 0, mc == 1))
                sc.ops("pe", fns, reads=[r_ET, r_const], writes=[bres[6]])
                sc.op("dve", lambda e: e.reciprocal(out=rec4[:, :], in_=denb[:, :, 0]), reads=[bres[6]], writes=[r_rec4])
                yield
                for hh in range(2):
                    Ocb = bank_f32([7, 5][hh]).rearrange("p (c t) -> p c t", c=2)
                    fns = []
                    for hl in range(2):
                        hm = hh * 2 + hl
                        for mc in range(2):
                            fns.append(mm(Ocb[:, hl, :], ET[:, hm * 2 + mc, :], vm_b[:, mc, hm * 256:(hm + 1) * 256], mc == 0, mc == 1))
                    sc.ops("pe", fns, reads=[r_ET, r_vm], writes=[bres[[7, 5][hh]]])
                    sc.op("dve", tt(oc_b[:, hh * 512:(hh + 1) * 512].rearrange("p (c t) -> p c t", c=2), Ocb,
                                    rec4[:, hh * 2:(hh + 1) * 2].unsqueeze(2).to_broadcast([128, 2, 256]), ALU.mult),
                          reads=[bres[[7, 5][hh]], r_rec4], writes=[r_ocb])
                    yield
                pTm = bank_bf16(7).rearrange("p (k t) -> p k t", k=8)
                sc.ops("pe", [tr(pTm[:, k, :], oc_b[:, k * 128:(k + 1) * 128], identb[:]) for k in range(8)], reads=[r_ocb, r_const], writes=[bres[7]])
                sc.op("act", actf(ocT[:], pTm, AF.Copy), reads=[bres[7]], writes=[r_ocT])
                yield
                for hh in range(2):
                    pb = bank_f32(6 + hh)
                    sc.ops("pe", [mm(pb, ocT[:, k, :], wo_b[:, k, hh * 512:(hh + 1) * 512], k == 0, k == 7) for k in range(8)],
                           reads=[r_ocT, r_w2], writes=[bres[6 + hh]])
                    sc.op("dve", stt(r2[:, hh * 512:(hh + 1) * 512], x1[:, hh * 512:(hh + 1) * 512], ALPHA, pb, ALU.mult, ALU.add),
                          reads=[bres[6 + hh], r_x1], writes=[r_r2])
                    yield
                layer_norm(r2[:], r_r2, x2s[b][:], r_x2s[b], lnp[:, 2, :], lnp[:, 3, :], lntmp)
                yield
                sc.dma("sp", X2[tok, :], x2s[b][:], reads=[r_x2s[b]])
                sc.op("act", actf(x2_b[:], x2s[b][:], AF.Copy), reads=[r_x2s[b]], writes=[r_x2b])
                yield
                sc.dma("sp", X2B[tok, :], x2_b[:], reads=[r_x2b])
                pTm = bank_bf16(7).rearrange("p (k t) -> p k t", k=8)
                sc.ops("pe", [tr(pTm[:, k, :], x2_b[:, k * 128:(k + 1) * 128], identb[:]) for k in range(8)], reads=[r_x2b, r_const], writes=[bres[7]])
                sc.op("act", actf(x2T_b[b][:], pTm, AF.Copy), reads=[bres[7]], writes=[r_x2Tb[b]])
                yield
                sc.dma("sp", X2T[:, :, tok], x2T_b[b][:], reads=[r_x2Tb[b]])
                for hh in range(2):
                    pTf = bank_f32(5 + hh).rearrange("p (k t) -> p k t", k=4)
                    sc.ops("pe", [tr(pTf[:, k, :], x2s[b][:, (hh * 4 + k) * 128:(hh * 4 + k + 1) * 128], identf[:]) for k in range(4)],
                           reads=[r_x2s[b], r_const], writes=[bres[5 + hh]])
                    sc.op("dve", cp(x2T_f[:, hh * 4:(hh + 1) * 4, :], pTf), reads=[bres[5 + hh]], writes=[r_x2Tf])
                    yield
                lgb = bank_f32(7)[:, 0:NEXP]
                fns = [mm(lgb, x2T_f[:, k, :], rw_f[:, k, :], k == 0, False) for k in range(8)]
                fns.append(mm(lgb, ones_f[0:1, :], rb_f[0:1, :], False, True))
                sc.ops("pe", fns, reads=[r_x2Tf, r_const], writes=[bres[7]])
                sc.op("dve", cp(lg[:], lgb), reads=[bres[7]], writes=[r_lg])
                yield
                sc.op("dve", lambda e: e.max(out=mx8[:, 0:8], in_=lg[:, :]), reads=[r_lg], writes=[r_rt2])
                yield
                sc.op("dve", ts(msk[:], lg[:], mx8[:, 3:4], None, ALU.is_ge), reads=[r_lg, r_rt2], writes=[r_rt2])
                yield
                sc.op("dve", ts(sm[:, 0:1], mx8[:, 0:1], -1.0, None, ALU.mult), reads=[r_rt2], writes=[r_rt2])
                yield
                sc.op("act", actf(ex[:], lg[:], AF.Exp, bias=sm[:, 0:1]), reads=[r_lg, r_rt2], writes=[r_rt2])
                yield
                sc.op("dve", tt(ex[:], ex[:], msk[:], ALU.mult), reads=[r_rt2], writes=[r_rt2])
                yield
                sc.op("dve", red(sm[:, 1:2], ex[:]), reads=[r_rt2], writes=[r_rt2])
                yield
                sc.op("dve", lambda e: e.reciprocal(out=sm[:, 2:3], in_=sm[:, 1:2]), reads=[r_rt2], writes=[r_rt2])
                yield
                sc.op("dve", ts(gat[b][:], ex[:], sm[:, 2:3], None, ALU.mult), reads=[r_rt2], writes=[r_gat[b]])
                yield
                sc.dma("sp", GT[tok, :], gat[b][:], reads=[r_gat[b]])
                rkb = bank_f32(7)[:, 32:96]
                sc.ops("pe", [mm(rkb[:, 0:32], ustr[:], msk[:]), mm(rkb[:, 32:64], ones_f[:], msk[:])], reads=[r_rt2, r_const], writes=[bres[7]])
                sc.op("dve", tt(rkt[b][:], rbase[:], rkb[:, 0:32], ALU.add), reads=[bres[7], r_rbase], writes=[r_rkt[b]])
                yield
                sc.op("dve", tt(rbase[:], rbase[:], rkb[:, 32:64], ALU.add), reads=[bres[7], r_rbase], writes=[r_rbase])
                yield
                sc.dma("sp", RKD[tok, :], rkt[b][:], reads=[r_rkt[b]])
                yield

            a2_loads(0)
            for th in a2_att_thunks(0):
                th()
            a2_ret(0)
            LEAD = 6
            for t in range(NT):
                gen = a2_tail(t)
                alive = True
                if t + 1 < NT:
                    a2_loads(t + 1)
                    thunks = a2_att_thunks(t + 1)
                    for ti, th in enumerate(thunks):
                        if ti == len(thunks) - 1 and ti < LEAD:
                            next(gen)
                        th()
                        if alive and (ti >= LEAD - 1 or ti == len(thunks) - 1):
                            try:
                                next(gen)
                            except StopIteration:
                                alive = False
                for _ in gen:
                    pass
                if t + 1 < NT:
                    a2_ret(t + 1)
            sc.dma("sp", CNT[:, :], rbase[:], reads=[r_rbase])
            sc.barrier()

        if stop_after == "B" and SPARSE:
            I32 = mybir.dt.int32
            ar.reset(base_mark)
            slk_i = ar.alloc([128, NT, 4], I32, "slk_i"); r_slk = Res()
            wk = ar.alloc([128, NT, 4], F32, "wk"); r_wk = Res()
            widx_i = ar.alloc([128, NBLK, 8], I32, "widx_i"); r_widx = Res()
            ebf = ar.alloc([128, NBLK], F32, "ebf"); r_ebf = Res()
            widx2_i = ar.alloc([128, NBLK], I32, "widx2_i"); r_widx2 = Res()
            cmeta = ar.alloc([128, CMW], F32, "cmeta")
            bgT = ar.alloc([128, NEXP, 8], F32, "bgT"); buT = ar.alloc([128, NEXP, 8], F32, "buT")
            bd32 = ar.alloc([32, D], F32, "bd32"); bd16 = ar.alloc([32, D], BF16, "bd16")
            lnp3 = ar.alloc([128, 2, D], F32, "lnp3")
            c7 = ar.alloc([128, 512], F32, "c7")
            o_ = 0
            thr16 = cmeta[:, o_:o_ + 16]; o_ += 17
            blkthr = cmeta[:, o_:o_ + NBLK]; o_ += NBLK
            iota_e = cmeta[:, o_:o_ + NEXP]; o_ += NEXP
            kcp = cmeta[:, o_:o_ + 8]; o_ += 8
            iota_p = cmeta[:, o_:o_ + 1]; o_ += 1
            tril = cmeta[:, o_:o_ + NEXP * NEXP].rearrange("p (a b) -> p a b", a=NEXP)
            sc.dma("sp", cmeta[:], cmeta_d[:, :], writes=[r_const])
            sc.dma("sp", bgT[:], bgT_d[:, :, :], writes=[r_const])
            sc.dma("sp", buT[:], buT_d[:, :, :], writes=[r_const])
            sc.dma("sp", lnp3[:], lnp_d[:, 4:6, :], writes=[r_const])
            sc.dma("sp", bd32[:], bd_d[:, :], writes=[r_const])
            sc.op("pool", lambda e: e.memset(c7[:], 7.0), writes=[r_const])
            sc.op("dve", ts(buT[:], buT[:], 7.0, None, ALU.add), reads=[r_const], writes=[r_const])
            sc.op("dve", cp(bd16[:], bd32[:]), reads=[r_const], writes=[r_const])
            mB = ar.mark()
            oh = ar.alloc([128, NBLK, NEXP], F32, "oh"); r_oh = Res()
            rankA = ar.alloc([128, NT, NEXP], F32, "rankA"); r_rankA = Res()
            gatA = ar.alloc([128, NT, NEXP], F32, "gatA"); r_gatA = Res()
            maskA = ar.alloc([128, NT, NEXP], F32, "maskA"); r_maskA = Res()
            top8A = ar.alloc([128, NT, 8], F32, "top8A"); r_top8 = Res()
            cntt = ar.alloc([128, NEXP], F32, "cntt"); r_cnt = Res()
            cmp16 = ar.alloc([128, NEXP, 16], F32, "cmp16")
            t32 = ar.alloc([128, NEXP, NEXP], F32, "t32")
            nblk_t = ar.alloc([128, NEXP], F32, "nblk_t"); padded = ar.alloc([128, NEXP], F32, "padded")
            pend = ar.alloc([128, NEXP], F32, "pend"); pstart = ar.alloc([128, NEXP], F32, "pstart")
            eq = ar.alloc([128, 4, NEXP], F32, "eq"); r_eq = Res()
            xs_t = [ar.alloc([128, D], BF16, "xs_t") for _ in range(3)]; r_xs = [Res() for _ in range(3)]
            r_m = Res()
            sc.dma("sp", rankA[:], RKD.rearrange("(t p) e -> p t e", p=128), writes=[r_rankA])
            sc.dma("sp", gatA[:], GT.rearrange("(t p) e -> p t e", p=128), writes=[r_gatA])
            sc.dma("sp", cntt[:], CNT[:, :], writes=[r_cnt])
            sc.op("dve", tt(cmp16[:], cntt[:, :].unsqueeze(2).to_broadcast([128, NEXP, 16]), thr16.unsqueeze(1).to_broadcast([128, NEXP, 16]), ALU.is_gt),
                  reads=[r_cnt, r_const], writes=[r_m])
            sc.op("dve", red(nblk_t[:], cmp16[:]), reads=[r_m], writes=[r_m])
            sc.op("dve", ts(padded[:], nblk_t[:], 512.0, None, ALU.mult), reads=[r_m], writes=[r_m])
            sc.op("dve", tt(t32[:], tril, padded[:, :].unsqueeze(1).to_broadcast([128, NEXP, NEXP]), ALU.mult), reads=[r_m, r_const], writes=[r_m])
            sc.op("dve", red(pend[:], t32[:]), reads=[r_m], writes=[r_m])
            sc.op("dve", tt(pstart[:], pend[:], padded[:], ALU.subtract), reads=[r_m], writes=[r_m])
            sc.op("dve", tt(oh[:], pend[:, :].unsqueeze(1).to_broadcast([128, NBLK, NEXP]), blkthr.unsqueeze(2).to_broadcast([128, NBLK, NEXP]), ALU.is_le),
                  reads=[r_m, r_const], writes=[r_oh])
            sc.op("dve", red(ebf[:], oh[:]), reads=[r_oh], writes=[r_ebf])
            sc.op("dve", ts(ebf[:], ebf[:], float(NEXP - 1), None, ALU.min), reads=[r_ebf], writes=[r_ebf])
            sc.op("dve", stt(widx_i[:], ebf[:, :].unsqueeze(2).to_broadcast([128, NBLK, 8]), 1024.0, kcp.unsqueeze(1).to_broadcast([128, NBLK, 8]), ALU.mult, ALU.add),
                  reads=[r_ebf, r_const], writes=[r_widx])
            sc.op("dve", stt(widx2_i[:], ebf[:], 128.0, iota_p.to_broadcast([128, NBLK]), ALU.mult, ALU.add), reads=[r_ebf, r_const], writes=[r_widx2])
            sc.op("dve", tt(rankA[:], rankA[:], pstart[:, :].unsqueeze(1).to_broadcast([128, NT, NEXP]), ALU.add), reads=[r_rankA, r_m], writes=[r_rankA])
            sc.op("dve", ts(maskA[:], gatA[:], 0.0, None, ALU.is_gt), reads=[r_gatA], writes=[r_maskA])
            sc.op("dve", stt(rankA[:], rankA[:], 1.0, maskA[:], ALU.add, ALU.mult), reads=[r_rankA, r_maskA], writes=[r_rankA])
            for t in range(NT):
                sc.op("dve", lambda e, t=t: e.max(out=top8A[:, t, :], in_=rankA[:, t, :]), reads=[r_rankA], writes=[r_top8])
            sc.op("dve", ts(slk_i[:], top8A[:, :, 0:4], -1.0, None, ALU.add), reads=[r_top8], writes=[r_slk])
            for t in range(NT):
                sc.op("dve", tt(eq[:], rankA[:, t, :].unsqueeze(1).to_broadcast([128, 4, NEXP]), top8A[:, t, 0:4].unsqueeze(2).to_broadcast([128, 4, NEXP]), ALU.is_equal),
                      reads=[r_rankA, r_top8], writes=[r_eq])
                sc.op("dve", tt(eq[:], eq[:], gatA[:, t, :].unsqueeze(1).to_broadcast([128, 4, NEXP]), ALU.mult), reads=[r_eq, r_gatA], writes=[r_eq])
                sc.op("dve", red(wk[:, t, :], eq[:]), reads=[r_eq], writes=[r_wk])
            hz = {}
            for t in range(NT):
                xb_ = t % 3
                sc.dma("sp", xs_t[xb_][:], X2B[t * 128:(t + 1) * 128, :], writes=[r_xs[xb_]])
                for k4 in range(4):
                    sc.dma_fn("pool", lambda e, xb_=xb_, t=t, k4=k4: e.indirect_dma_start(
                        out=XS[:, :], out_offset=bass.IndirectOffsetOnAxis(ap=slk_i[:, t, k4:k4 + 1], axis=0),
                        in_=xs_t[xb_][:, :], in_offset=None),
                        reads=[r_xs[xb_], r_slk], extra=[hz])
            sc.barrier()
            ar.reset(mB)
            wsl = [[ar.alloc([128, 8, D], BF16, "w%d%d" % (i, j)) for j in range(3)] for i in range(2)]
            r_wsl = [[Res() for j in range(3)] for i in range(2)]
            xg = [ar.alloc([128, 4, D], BF16, "xg") for _ in range(2)]; r_xg = [Res(), Res()]
            xgT = [ar.alloc([128, 8, 512], BF16, "xgT") for _ in range(2)]; r_xgT = [Res(), Res()]
            hT = [ar.alloc([128, 8, 512], BF16, "hT") for _ in range(2)]; r_hT = [Res(), Res()]
            g1 = [ar.alloc([128, 512], F32, "g1") for _ in range(2)]; r_g1 = [Res(), Res()]
            sg = [ar.alloc([128, 512], F32, "sg") for _ in range(2)]; r_sg = [Res(), Res()]
            u1 = [ar.alloc([128, 512], F32, "u1") for _ in range(2)]; r_u1 = [Res(), Res()]
            yout = [ar.alloc([128, D], F32, "yout") for _ in range(2)]; r_yout = [Res(), Res()]
            bsel = [ar.alloc([128, 2, 8], F32, "bsel") for _ in range(2)]; r_bsel = [Res(), Res()]
            btmp = ar.alloc([128, 8, NEXP], F32, "btmp"); r_btmp = Res()
            ohb = ar.alloc([128, NEXP], F32, "ohb"); r_ohb = Res()
            oht = [ar.alloc([32, 128], BF16, "oht") for _ in range(2)]; r_oht = [Res(), Res()]
            wflat = [wg_d.rearrange("e r n -> (e r) n"), wu_d.rearrange("e r n -> (e r) n"), wd_d.rearrange("e r n -> (e r) n")]
            wpm = [wg_d.rearrange("e (p j) n -> (e p) (j n)", j=8), wu_d.rearrange("e (p j) n -> (e p) (j n)", j=8)]
            bgv = bgT[:].rearrange("p e f -> p f e")
            buv = buT[:].rearrange("p e f -> p f e")
            dcnt = 0

            def load_block(blk):
                sl = blk % 2
                if os.environ.get('KS_NOW') and blk >= 2:
                    return
                for j in range(2):
                    sc.dma_fn("pool", lambda e, sl=sl, j=j, blk=blk: e.indirect_dma_start(
                        out=wsl[sl][j][:, :, :].rearrange("p k n -> p (k n)"), out_offset=None,
                        in_=wpm[j][:, :],
                        in_offset=bass.IndirectOffsetOnAxis(ap=widx2_i[:, blk:blk + 1], axis=0)),
                        reads=[r_widx2], writes=[r_wsl[sl][j]])
                for j in (2,):
                    for kc in range(8):
                        sc.dma_fn("pool", lambda e, sl=sl, j=j, kc=kc, blk=blk: e.indirect_dma_start(
                            out=wsl[sl][j][:, kc, :], out_offset=None, in_=wflat[j][:, :],
                            in_offset=bass.IndirectOffsetOnAxis(ap=widx_i[:, blk, kc:kc + 1], axis=0)),
                            reads=[r_widx], writes=[r_wsl[sl][j]])

            def x_load(blk):
                sc.dma("sp", xg[blk % 2][:], XS[blk * 512:(blk + 1) * 512, :].rearrange("(i p) d -> p i d", p=128), writes=[r_xg[blk % 2]])

            def x_transposes(blk):
                xs_ = blk % 2
                for k2 in range(4):
                    bk = 6 + k2 % 2
                    pv = bank_bf16(bk).rearrange("p (a t) -> p a t", a=2)
                    fns = []
                    for a in range(2):
                        k = k2 * 2 + a
                        for i in range(4):
                            fns.append(tr(pv[:, a, i * 128:(i + 1) * 128], xg[xs_][:, i, :].rearrange("p (q j) -> p q j", j=8)[:, :, k], identb[:]))
                    sc.ops("pe", fns, reads=[r_xg[xs_], r_const], writes=[bres[bk]])
                    sc.op("act", actf(xgT[xs_][:, k2 * 2:k2 * 2 + 2, :], pv, AF.Copy), reads=[bres[bk]], writes=[r_xgT[xs_]])

            load_block(0)
            x_load(0)
            x_transposes(0)
            ycnt = 0
            for blk in range(NBLK):
                sl = blk % 2
                if blk + 1 < NBLK:
                    load_block(blk + 1)
                    x_load(blk + 1)
                sc.op("dve", ts(ohb[:], iota_e, ebf[:, blk:blk + 1], None, ALU.is_equal), reads=[r_ebf, r_const], writes=[r_ohb])
                for bi, bv in enumerate((bgv, buv)):
                    sc.op("dve", tt(btmp[:], bv, ohb[:, :].unsqueeze(1).to_broadcast([128, 8, NEXP]), ALU.mult), reads=[r_ohb, r_const], writes=[r_btmp])
                    sc.op("dve", red(bsel[sl][:, bi, :], btmp[:]), reads=[r_btmp], writes=[r_bsel[sl]])
                sc.op("dve", ts(oht[sl][:], ebf[0:32, blk:blk + 1].to_broadcast([32, 128]), iota_p[0:32, 0:1], None, ALU.is_equal), reads=[r_ebf, r_const], writes=[r_oht[sl]])
                for ffc in range(8):
                    fb = ffc % 2
                    pg = bank_f32(0 + fb); pu = bank_f32(2 + fb)
                    sc.ops("pe", [mm(pg, wsl[sl][0][:, k, ffc * 128:(ffc + 1) * 128], xgT[sl][:, k, :], k == 0, k == 7) for k in range(8)],
                           reads=[r_wsl[sl][0], r_xgT[sl]], writes=[bres[0 + fb]])
                    sc.ops("pe", [mm(pu, wsl[sl][1][:, k, ffc * 128:(ffc + 1) * 128], xgT[sl][:, k, :], k == 0, k == 7) for k in range(8)],
                           reads=[r_wsl[sl][1], r_xgT[sl]], writes=[bres[2 + fb]])
                    sc.op("dve", stt(g1[fb][:], pg, bsel[sl][:, 0, ffc:ffc + 1], c7[:], ALU.add, ALU.min), reads=[bres[0 + fb], r_bsel[sl], r_const], writes=[r_g1[fb]])
                    sc.op("act", actf(sg[fb][:], g1[fb][:], AF.Silu, scale=1.702), reads=[r_g1[fb]], writes=[r_sg[fb]])
                    sc.op("act", actf(u1[fb][:], pu, AF.Relu, bias=bsel[sl][:, 1, ffc:ffc + 1]), reads=[bres[2 + fb], r_bsel[sl]], writes=[r_u1[fb]])
                    sc.op("dve", ts(u1[fb][:], u1[fb][:], 14.0, -6.0, ALU.min, ALU.add), reads=[r_u1[fb]], writes=[r_u1[fb]])
                    sc.op("dve", stt(hT[sl][:, ffc, :], sg[fb][:], 1.0 / 1.702, u1[fb][:], ALU.mult, ALU.mult), reads=[r_sg[fb], r_u1[fb]], writes=[r_hT[sl]])
                if blk + 1 < NBLK:
                    x_transposes(blk + 1)
                for i in range(4):
                    yb_ = ycnt % 2
                    ycnt += 1
                    for colh in range(2):
                        bk = 4 + dcnt % 2
                        dcnt += 1
                        pd = bank_f32(bk)
                        fns = [mm(pd, hT[sl][:, k, i * 128:(i + 1) * 128], wsl[sl][2][:, k, colh * 512:(colh + 1) * 512], k == 0, False) for k in range(8)]
                        fns.append(mm(pd, oht[sl][0:32, :], bd16[0:32, colh * 512:(colh + 1) * 512], False, True))
                        sc.ops("pe", fns, reads=[r_hT[sl], r_wsl[sl][2], r_oht[sl], r_const], writes=[bres[bk]])
                        if bk == 4:
                            sc.op("act", actf(yout[yb_][:, colh * 512:(colh + 1) * 512], pd, AF.Copy), reads=[bres[bk]], writes=[r_yout[yb_]])
                        else:
                            sc.op("dve", cp(yout[yb_][:, colh * 512:(colh + 1) * 512], pd), reads=[bres[bk]], writes=[r_yout[yb_]])
                    sc.dma("sp", YS[blk * 512 + i * 128:blk * 512 + (i + 1) * 128, :], yout[yb_][:], reads=[r_yout[yb_]])
            sc.barrier()
            ar.reset(mB)
            acc = [ar.alloc([128, D], F32, "acc") for _ in range(2)]; r_acc = [Res(), Res()]
            gk = [[ar.alloc([128, D], F32, "gk") for _ in range(4)] for _ in range(2)]; r_gk = [[Res() for _ in range(4)] for _ in range(2)]
            outt = [ar.alloc([128, D], F32, "outt") for _ in range(2)]; r_outt = [Res(), Res()]
            lntmp3 = (ar.alloc([128, 12], F32, "st6b"), ar.alloc([128, 2], F32, "mvb"), ar.alloc([128, 1], F32, "rstdb"), ar.alloc([128, 1], F32, "nmrb"))
            for t in range(NT):
                b = t % 2
                tok = slice(t * 128, (t + 1) * 128)
                sc.dma("sp", acc[b][:], X2[tok, :], writes=[r_acc[b]])
                for k4 in range(4):
                    sc.dma_fn("pool", lambda e, b=b, t=t, k4=k4: e.indirect_dma_start(
                        out=gk[b][k4][:, :], out_offset=None, in_=YS[:, :],
                        in_offset=bass.IndirectOffsetOnAxis(ap=slk_i[:, t, k4:k4 + 1], axis=0)),
                        reads=[r_slk], writes=[r_gk[b][k4]])
                sc.op("dve", ts(acc[b][:], acc[b][:], ALPHA, None, ALU.mult), reads=[r_acc[b]], writes=[r_acc[b]])
                for k4 in range(4):
                    sc.op("dve", stt(acc[b][:], gk[b][k4][:], wk[:, t, k4:k4 + 1], acc[b][:], ALU.mult, ALU.add), reads=[r_gk[b][k4], r_wk, r_acc[b]], writes=[r_acc[b]])
                layer_norm(acc[b][:], r_acc[b], outt[b][:], r_outt[b], lnp3[:, 0, :], lnp3[:, 1, :], lntmp3)
                out_handles.append(sc.dma("sp", out_d[tok, :], outt[b][:], reads=[r_outt[b]]))
            sc.barrier()

        if stop_after == "B" and not SPARSE:
            ar.reset(base_mark)
            wsl = [[ar.alloc([128, 8, D], BF16, "w%d%d" % (i, j)) for j in range(3)] for i in range(2)]
            r_wsl = [[Res() for j in range(3)] for i in range(2)]
            x2T_c = ar.alloc([128, 8, TC], BF16, "x2T_c"); r_x2Tc = Res()
            Yacc = ar.alloc([128, 8, D], F32, "Yacc"); r_Y = [Res() for _ in range(8)]
            hT = [ar.alloc([128, 8, 512], BF16, "hT") for _ in range(2)]; r_hT = [Res(), Res()]
            g1 = [ar.alloc([128, 512], F32, "g1") for _ in range(2)]; r_g1 = [Res(), Res()]
            sg = [ar.alloc([128, 512], F32, "sg") for _ in range(2)]; r_sg = [Res(), Res()]
            u1 = [ar.alloc([128, 512], F32, "u1") for _ in range(2)]; r_u1 = [Res(), Res()]
            Gc = ar.alloc([128, 8, NEXP], F32, "Gc"); r_Gc = Res()
            GTs = ar.alloc([32, 128], F32, "GTs"); r_GTs = Res()
            bd32 = ar.alloc([32, D], F32, "bd32")
            bgT = ar.alloc([128, NEXP, 8], F32, "bgT"); buT = ar.alloc([128, NEXP, 8], F32, "buT")
            lnp3 = ar.alloc([128, 2, D], F32, "lnp3")
            c7 = ar.alloc([128, 512], F32, "c7")
            outt = [ar.alloc([128, D], F32, "outt") for _ in range(2)]; r_outt = [Res(), Res()]
            lntmp3 = (ar.alloc([128, 12], F32, "st6b"), ar.alloc([128, 2], F32, "mvb"), ar.alloc([128, 1], F32, "rstdb"), ar.alloc([128, 1], F32, "nmrb"))
            sc.dma("sp", bgT[:], bgT_d[:, :, :], writes=[r_const])
            sc.dma("sp", buT[:], buT_d[:, :, :], writes=[r_const])
            sc.dma("sp", lnp3[:], lnp_d[:, 4:6, :], writes=[r_const])
            sc.dma("sp", bd32[:], bd_d[:, :], writes=[r_const])
            sc.op("pool", lambda e: e.memset(c7[:], 7.0), writes=[r_const])
            sc.op("dve", ts(buT[:], buT[:], 7.0, None, ALU.add), reads=[r_const], writes=[r_const])
            wsrc = [wg_d, wu_d, wd_d]
            cnt = 0
            dcnt = 0
            ocnt = 0
            for c in range(S // TC):
                ctok = slice(c * TC, (c + 1) * TC)
                sc.dma("sp", x2T_c[:], X2T[:, :, ctok], writes=[r_x2Tc])
                sc.dma("sp", Gc[:], GT[ctok, :].rearrange("(t p) e -> p t e", p=128), writes=[r_Gc])
                for i in range(8):
                    sc.dma("sp", Yacc[:, i, :], X2[c * TC + i * 128:c * TC + (i + 1) * 128, :], writes=[r_Y[i]])
                    pgt = bank_f32(7)[0:32, 0:128]
                    sc.op("pe", tr(pgt, Gc[:, i, :], identf[:]), reads=[r_Gc, r_const], writes=[bres[7]])
                    sc.op("dve", cp(GTs[:], pgt), reads=[bres[7]], writes=[r_GTs])
                    for colh in range(2):
                        bk = 4 + dcnt % 3
                        dcnt += 1
                        pd = bank_f32(bk)
                        sc.op("pe", mm(pd, GTs[0:32, :], bd32[0:32, colh * 512:(colh + 1) * 512]), reads=[r_GTs, r_const], writes=[bres[bk]])
                        ysl = Yacc[:, i, colh * 512:(colh + 1) * 512]
                        sc.op("dve", stt(ysl, ysl, ALPHA, pd, ALU.mult, ALU.add), reads=[bres[bk], r_Y[i]], writes=[r_Y[i]])
                for ex_ in range(NEXP):
                    sl = cnt % 2
                    cnt += 1
                    for j in range(3):
                        v = wsrc[j][ex_].rearrange("(k p) n -> p k n", p=128)
                        for hh in range(2):
                            sc.dma("pool", wsl[sl][j][:, :, hh * 512:(hh + 1) * 512], v[:, :, hh * 512:(hh + 1) * 512], writes=[r_wsl[sl][j]])
                    for half in range(2):
                        hb_ = (cnt + half) % 2
                        for ffc in range(8):
                            fb = ffc % 2
                            pg = bank_f32(0 + fb); pu = bank_f32(2 + fb)
                            sc.ops("pe", [mm(pg, wsl[sl][0][:, k, ffc * 128:(ffc + 1) * 128], x2T_c[:, k, half * 512:(half + 1) * 512], k == 0, k == 7) for k in range(8)],
                                   reads=[r_wsl[sl][0], r_x2Tc], writes=[bres[0 + fb]])
                            sc.ops("pe", [mm(pu, wsl[sl][1][:, k, ffc * 128:(ffc + 1) * 128], x2T_c[:, k, half * 512:(half + 1) * 512], k == 0, k == 7) for k in range(8)],
                                   reads=[r_wsl[sl][1], r_x2Tc], writes=[bres[2 + fb]])
                            sc.op("dve", stt(g1[fb][:], pg, bgT[:, ex_, ffc:ffc + 1], c7[:], ALU.add, ALU.min), reads=[bres[0 + fb], r_const], writes=[r_g1[fb]])
                            sc.op("act", actf(sg[fb][:], g1[fb][:], AF.Silu, scale=1.702), reads=[r_g1[fb]], writes=[r_sg[fb]])
                            sc.op("act", actf(u1[fb][:], pu, AF.Relu, bias=buT[:, ex_, ffc:ffc + 1]), reads=[bres[2 + fb], r_const], writes=[r_u1[fb]])
                            sc.op("dve", ts(u1[fb][:], u1[fb][:], 14.0, -6.0, ALU.min, ALU.add), reads=[r_u1[fb]], writes=[r_u1[fb]])
                            sc.op("dve", stt(hT[hb_][:, ffc, :], sg[fb][:], 1.0 / 1.702, u1[fb][:], ALU.mult, ALU.mult), reads=[r_sg[fb], r_u1[fb]], writes=[r_hT[hb_]])
                        for i in range(4):
                            tl = half * 4 + i
                            for colh in range(2):
                                bk = 4 + dcnt % 3
                                dcnt += 1
                                pd = bank_f32(bk)
                                sc.ops("pe", [mm(pd, hT[hb_][:, k, i * 128:(i + 1) * 128], wsl[sl][2][:, k, colh * 512:(colh + 1) * 512], k == 0, k == 7) for k in range(8)],
                                       reads=[r_hT[hb_], r_wsl[sl][2]], writes=[bres[bk]])
                                ysl = Yacc[:, tl, colh * 512:(colh + 1) * 512]
                                sc.op("dve", stt(ysl, pd, Gc[:, tl, ex_:ex_ + 1], ysl, ALU.mult, ALU.add), reads=[bres[bk], r_Gc, r_Y[tl]], writes=[r_Y[tl]])
                for i in range(8):
                    ob = ocnt % 2
                    ocnt += 1
                    layer_norm(Yacc[:, i, :], r_Y[i], outt[ob][:], r_outt[ob], lnp3[:, 0, :], lnp3[:, 1, :], lntmp3)
                    out_handles.append(sc.dma("sp", out_d[c * TC + i * 128:c * TC + (i + 1) * 128, :], outt[ob][:], reads=[r_outt[ob]]))
            sc.barrier()

        sc.barrier()
        blk = stack.enter_context(nc.Block())
        sc.emit(blk)
    return nc, dbg


def _prep_shared(inputs, S):
    f = lambda a: np.ascontiguousarray(np.asarray(a, dtype=np.float32))
    sh = {}
    sh["w_in"] = f(inputs["w_in"][0])
    sh["w_out"] = f(inputs["w_out"][0])
    for k in ("mem_wq", "mem_wk", "mem_wv", "mem_wo"):
        sh[k] = f(inputs[k][0])
    sh["router_w"] = f(inputs["router_w"][0])
    sh["router_b"] = f(inputs["router_b"][0]).reshape(1, NEXP)
    sh["w_gate"] = f(inputs["w_gate"][0])
    sh["w_up"] = f(inputs["w_up"][0])
    sh["w_down"] = f(inputs["w_down"][0])
    sh["b_gateT"] = f(np.asarray(inputs["b_gate"][0]).reshape(NEXP, 8, 128).transpose(2, 0, 1))
    sh["b_upT"] = f(np.asarray(inputs["b_up"][0]).reshape(NEXP, 8, 128).transpose(2, 0, 1))
    sh["b_down"] = f(inputs["b_down"][0])
    lnp = np.stack([np.asarray(inputs[k][0], dtype=np.float32) for k in ("ln1_g", "ln1_b", "ln2_g", "ln2_b", "ln3_g", "ln3_b")], 0)
    sh["lnp"] = f(np.broadcast_to(lnp[None], (128, 6, D)))
    sh["rng"] = f(np.broadcast_to(np.asarray(inputs["ret_norm_g"][0], dtype=np.float32).reshape(1, 512), (128, 512)))
    sh.update(_const_tables(S))
    sh["cmeta"] = _meta_consts(S)[0]
    return sh


def kernel(**inputs):
    x = np.asarray(inputs["x"], dtype=np.float32)
    mem = np.asarray(inputs["mem"], dtype=np.float32)
    B, S, _ = x.shape
    sh = _prep_shared(inputs, S)
    nc, _ = build_program(NT=S // 128)
    in_maps = []
    for b in range(B):
        m = dict(sh)
        m["x"] = np.ascontiguousarray(x[b])
        m["mem"] = np.ascontiguousarray(mem[b])
        in_maps.append(m)
    res = run_bass_kernel_spmd(nc, in_maps, core_ids=list(range(B)))
    return np.stack([np.asarray(r["out"], dtype=np.float32) for r in res.results], axis=0)
```
